# Optimizing a Trainium2 kernel written in Bass

```python
import math
import jax, jax.numpy as jnp
from jax import lax
import numpy as np

D_MODEL = 1024
BATCH = 4
SEQ = 8192
DEPTH = 2

GRID_W = 64
CTX_LEN = 256
N_MIXERS = 2
N_MOD = 6
NORM_EPS = 1e-6
POS_BASE = 10000.0
RNN_WIDTH = 1280
RNN_HEADS = 5
RNN_BLOCK = RNN_WIDTH // RNN_HEADS
CONV_WIDTH = 4
CONV_PAD_LEFT = 2
CONV_PAD_RIGHT = CONV_WIDTH - 1 - CONV_PAD_LEFT
LRU_C = 8.0
LAMBDA_MIN = 0.9
LAMBDA_MAX = 0.999
SGU_WIDTH = 2048
SGU_HEADS = 8
SGU_GROUP = SGU_WIDTH // SGU_HEADS
CHUNK = 128
N_EXPERTS = 64
TOP_K = 8
N_GROUPS = 8
TOPK_GROUPS = 4
EXPERTS_PER_GROUP = N_EXPERTS // N_GROUPS
EXPERT_FF = 256
SHARED_FF = 256
ROUTED_SCALE = 2.5
MOE_BLOCK = 128

kernel_name = 'hybrid_rglru_sgu_moe_dit'


def rmsnorm(x, g):
    x32 = x.astype(jnp.float32)
    y = x32 * lax.rsqrt(jnp.mean(x32 * x32, axis=-1, keepdims=True) + NORM_EPS)
    return (y * g.astype(jnp.float32)).astype(x.dtype)


def layernorm(x, g, b):
    x32 = x.astype(jnp.float32)
    mu = jnp.mean(x32, axis=-1, keepdims=True)
    xc = x32 - mu
    y = xc * lax.rsqrt(jnp.mean(xc * xc, axis=-1, keepdims=True) + NORM_EPS)
    return (y * g.astype(jnp.float32) + b.astype(jnp.float32)).astype(x.dtype)


def sincos_2d(rows, d):
    quarter = d // 4
    omega = 1.0 / (POS_BASE ** (jnp.arange(quarter, dtype=jnp.float32) / quarter))
    def emb(n):
        p = jnp.arange(n, dtype=jnp.float32)[:, None] * omega[None, :]
        return jnp.concatenate([jnp.sin(p), jnp.cos(p)], axis=-1)
    er, ec = emb(rows), emb(GRID_W)
    pe = jnp.concatenate([jnp.broadcast_to(er[:, None, :], (rows, GRID_W, d // 2)),
                          jnp.broadcast_to(ec[None, :, :], (rows, GRID_W, d // 2))], axis=-1)
    return pe.reshape(rows * GRID_W, d)


def dwconv_centred(u, w, b):
    t = u.shape[1]
    up = jnp.pad(u, ((0, 0), (CONV_PAD_LEFT, CONV_PAD_RIGHT), (0, 0)))
    return sum(up[:, k:k + t] * w[k] for k in range(CONV_WIDTH)) + b


def lru_coeffs(u, r_w, r_b, i_w, i_b, lam, reset_first):
    u = u.astype(jnp.float32)
    ub = u.reshape(u.shape[:2] + (RNN_HEADS, RNN_BLOCK))
    r = jax.nn.sigmoid(jnp.einsum('bthi,hij->bthj', ub, r_w.astype(jnp.float32)).reshape(u.shape) + r_b.astype(jnp.float32))
    ig = jax.nn.sigmoid(jnp.einsum('bthi,hij->bthj', ub, i_w.astype(jnp.float32)).reshape(u.shape) + i_b.astype(jnp.float32))
    log_a = LRU_C * r * jax.nn.log_sigmoid(lam.astype(jnp.float32))
    a = jnp.exp(log_a)
    mult = jnp.sqrt(-jnp.expm1(2.0 * log_a))
    if reset_first:
        mult = mult.at[:, 0].set(1.0)
    return a, mult * ig * u


def linear_scan(a, b):
    def combine(l, r):
        return l[0] * r[0], r[0] * l[1] + r[1]
    _, h = lax.associative_scan(combine, (a, b), axis=1)
    return h


def rglru_mixer(hx, hc, w_in, conv_w, conv_b, r_w, r_b, i_w, i_b, lam, w_out, ctx_out):
    gate_x, ux = jnp.split(hx @ w_in, 2, axis=-1)
    ux = dwconv_centred(ux, conv_w, conv_b)
    uc = dwconv_centred(hc @ w_in[:, RNN_WIDTH:], conv_w, conv_b)
    yx = 0.0
    yc = 0.0
    for d in range(2):
        flip = (lambda t: jnp.flip(t, axis=1)) if d == 1 else (lambda t: t)
        a_c, b_c = lru_coeffs(flip(uc), r_w[d], r_b[d], i_w[d], i_b[d], lam[d], True)
        h_c = linear_scan(a_c, b_c)
        a_x, b_x = lru_coeffs(flip(ux), r_w[d], r_b[d], i_w[d], i_b[d], lam[d], False)
        b_x = b_x.at[:, 0].add(a_x[:, 0] * h_c[:, -1])
        yx = yx + flip(linear_scan(a_x, b_x))
        if ctx_out:
            yc = yc + flip(h_c)
    out_x = (jax.nn.gelu(gate_x) * yx.astype(hx.dtype)) @ w_out
    if not ctx_out:
        return out_x, None
    gate_c = hc @ w_in[:, :RNN_WIDTH]
    out_c = (jax.nn.gelu(gate_c) * yc.astype(hc.dtype)) @ w_out
    return out_x, out_c


def sgu_mixer(h, w_in, ln_g, ln_b, w_s, b_s, w_out):
    bsz, t, _ = h.shape
    u, v = jnp.split(jax.nn.gelu(h @ w_in), 2, axis=-1)
    v = layernorm(v, ln_g, ln_b)
    vc = v.reshape(bsz, t // CHUNK, CHUNK, SGU_HEADS, SGU_GROUP)
    sv = jnp.einsum('gpq,bnqgd->bnpgd', w_s, vc) + b_s.T[:, :, None]
    return (u * sv.reshape(bsz, t, SGU_WIDTH)) @ w_out


def swiglu(x, w_gu, w_down):
    g, u = jnp.split(x @ w_gu, 2, axis=-1)
    return (jax.nn.silu(g) * u) @ w_down


def moe_ffn(xn, router_w, router_b, w_gu, w_down, ws_gu, ws_down):
    n_tok, d = xn.shape
    scores = jax.nn.sigmoid(jnp.dot(xn.astype(jnp.float32), router_w.astype(jnp.float32)))
    sel = scores + router_b.astype(jnp.float32)
    grp_score = lax.top_k(sel.reshape(n_tok, N_GROUPS, EXPERTS_PER_GROUP), 2)[0].sum(-1)
    _, top_grp = lax.top_k(grp_score, TOPK_GROUPS)
    grp_keep = jnp.any(top_grp[:, :, None] == jnp.arange(N_GROUPS)[None, None, :], axis=1)
    keep = jnp.repeat(grp_keep, EXPERTS_PER_GROUP, axis=1)
    _, eidx = lax.top_k(jnp.where(keep, sel, -jnp.inf), TOP_K)
    gate = jnp.take_along_axis(scores, eidx, axis=1)
    gate = (gate / jnp.sum(gate, axis=-1, keepdims=True) * ROUTED_SCALE).astype(xn.dtype)
    n_assign = n_tok * TOP_K
    n_blocks = (n_assign + N_EXPERTS * (MOE_BLOCK - 1) + MOE_BLOCK - 1) // MOE_BLOCK
    n_pad = n_blocks * MOE_BLOCK
    flat_e = eidx.reshape(-1)
    flat_tok = jnp.repeat(jnp.arange(n_tok, dtype=jnp.int32), TOP_K)
    order = jnp.argsort(flat_e)
    se, stok, sw = flat_e[order], flat_tok[order], gate.reshape(-1)[order]
    counts = jax.ops.segment_sum(jnp.ones_like(flat_e), flat_e, num_segments=N_EXPERTS)
    padded = (counts + MOE_BLOCK - 1) // MOE_BLOCK * MOE_BLOCK
    pad_end = jnp.cumsum(padded)
    pad_start = pad_end - padded
    start = jnp.cumsum(counts) - counts
    dest = pad_start[se] + jnp.arange(n_assign, dtype=se.dtype) - start[se]
    buf_tok = jnp.full((n_pad,), n_tok, jnp.int32).at[dest].set(stok)
    buf_w = jnp.zeros((n_pad,), xn.dtype).at[dest].set(sw)
    block_e = jnp.minimum(jnp.searchsorted(pad_end, jnp.arange(n_blocks, dtype=pad_end.dtype) * MOE_BLOCK, side='right'), N_EXPERTS - 1)
    x_pad = jnp.concatenate([xn, jnp.zeros((1, d), xn.dtype)], axis=0)
    def expert_block(args):
        tok, w, e = args
        return swiglu(x_pad[tok], w_gu[e], w_down[e]) * w[:, None]
    y = lax.map(expert_block, (buf_tok.reshape(n_blocks, MOE_BLOCK), buf_w.reshape(n_blocks, MOE_BLOCK), block_e))
    routed = jax.ops.segment_sum(y.reshape(n_pad, d), buf_tok, num_segments=n_tok + 1)[:n_tok]
    return routed + swiglu(xn, ws_gu, ws_down)


def setup_inputs(seed: int = 0) -> dict:
    key = jax.random.key(seed)
    ks = iter(jax.random.split(key, 40))
    def nrm(shape, scale):
        return jax.random.normal(next(ks), shape, jnp.float32) * scale
    d = D_MODEL
    n_a = (DEPTH + N_MIXERS - 1) // N_MIXERS
    n_b = DEPTH // N_MIXERS
    lam_u = jax.random.uniform(next(ks), (n_a, 2, RNN_WIDTH), jnp.float32, LAMBDA_MIN, LAMBDA_MAX)
    return {
        'x': nrm((BATCH, SEQ, d), 1.0),
        'c': nrm((BATCH, d), 1.0),
        'ctx': nrm((BATCH, CTX_LEN, d), 1.0),
        'c_ctx': nrm((d,), 1.0),
        'ada_w': nrm((DEPTH, d, N_MOD * d), 0.5 * d ** -0.5),
        'ada_b': nrm((DEPTH, N_MOD * d), 0.02),
        'mix_norm_g': 1.0 + nrm((DEPTH, d), 0.02),
        'ffn_norm_g': 1.0 + nrm((DEPTH, d), 0.02),
        'a_w_in': nrm((n_a, d, 2 * RNN_WIDTH), d ** -0.5),
        'a_conv_w': nrm((n_a, CONV_WIDTH, RNN_WIDTH), CONV_WIDTH ** -0.5),
        'a_conv_b': nrm((n_a, RNN_WIDTH), 0.02),
        'a_gate_r_w': nrm((n_a, 2, RNN_HEADS, RNN_BLOCK, RNN_BLOCK), RNN_BLOCK ** -0.5),
        'a_gate_r_b': nrm((n_a, 2, RNN_WIDTH), 0.02),
        'a_gate_i_w': nrm((n_a, 2, RNN_HEADS, RNN_BLOCK, RNN_BLOCK), RNN_BLOCK ** -0.5),
        'a_gate_i_b': nrm((n_a, 2, RNN_WIDTH), 0.02),
        'a_lambda': jnp.log(lam_u) - jnp.log1p(-lam_u),
        'a_w_out': nrm((n_a, RNN_WIDTH, d), RNN_WIDTH ** -0.5),
        'b_w_in': nrm((n_b, d, 2 * SGU_WIDTH), d ** -0.5),
        'b_ln_g': 1.0 + nrm((n_b, SGU_WIDTH), 0.02),
        'b_ln_b': nrm((n_b, SGU_WIDTH), 0.02),
        'b_w_s': nrm((n_b, SGU_HEADS, CHUNK, CHUNK), CHUNK ** -0.5),
        'b_b_s': 1.0 + nrm((n_b, SGU_HEADS, CHUNK), 0.02),
        'b_w_out': nrm((n_b, SGU_WIDTH, d), SGU_WIDTH ** -0.5),
        'router_w': nrm((DEPTH, d, N_EXPERTS), d ** -0.5),
        'router_b': nrm((DEPTH, N_EXPERTS), 0.01),
        'moe_w_gu': nrm((DEPTH, N_EXPERTS, d, 2 * EXPERT_FF), d ** -0.5),
        'moe_w_down': nrm((DEPTH, N_EXPERTS, EXPERT_FF, d), EXPERT_FF ** -0.5),
        'shared_w_gu': nrm((DEPTH, d, 2 * SHARED_FF), d ** -0.5),
        'shared_w_down': nrm((DEPTH, SHARED_FF, d), SHARED_FF ** -0.5),
        'final_norm_g': 1.0 + nrm((d,), 0.02),
    }


def reference(x, c, ctx, c_ctx, ada_w, ada_b, mix_norm_g, ffn_norm_g,
              a_w_in, a_conv_w, a_conv_b, a_gate_r_w, a_gate_r_b, a_gate_i_w, a_gate_i_b, a_lambda, a_w_out,
              b_w_in, b_ln_g, b_ln_b, b_w_s, b_b_s, b_w_out,
              router_w, router_b, moe_w_gu, moe_w_down, shared_w_gu, shared_w_down, final_norm_g):
    bsz, s, d = x.shape
    rows = s // GRID_W
    x = x + sincos_2d(rows, d).astype(x.dtype)[None]
    cx = ctx
    silu_c = jax.nn.silu(c)
    silu_cc = jax.nn.silu(c_ctx)
    for i in range(DEPTH):
        kind, j = i % N_MIXERS, i // N_MIXERS
        ctx_live = any(l % N_MIXERS == 0 for l in range(i + 1, DEPTH))
        mod = silu_c @ ada_w[i] + ada_b[i]
        mod_c = silu_cc @ ada_w[i] + ada_b[i]
        sh1, sc1, g1, sh2, sc2, g2 = jnp.split(mod[:, None, :], N_MOD, axis=-1)
        csh1, csc1, cg1, csh2, csc2, cg2 = jnp.split(mod_c, N_MOD)
        hx = rmsnorm(x, mix_norm_g[i]) * (1.0 + sc1) + sh1
        if kind == 0:
            hc = rmsnorm(cx, mix_norm_g[i]) * (1.0 + csc1) + csh1
            yx, yc = rglru_mixer(hx, hc, a_w_in[j], a_conv_w[j], a_conv_b[j], a_gate_r_w[j], a_gate_r_b[j],
                                 a_gate_i_w[j], a_gate_i_b[j], a_lambda[j], a_w_out[j], ctx_live)
        else:
            yx = sgu_mixer(hx, b_w_in[j], b_ln_g[j], b_ln_b[j], b_w_s[j], b_b_s[j], b_w_out[j])
            if ctx_live:
                hc = rmsnorm(cx, mix_norm_g[i]) * (1.0 + csc1) + csh1
                yc = sgu_mixer(hc, b_w_in[j], b_ln_g[j], b_ln_b[j], b_w_s[j], b_b_s[j], b_w_out[j])
        x = x + g1 * yx
        if ctx_live:
            cx = cx + cg1 * yc
        hx = rmsnorm(x, ffn_norm_g[i]) * (1.0 + sc2) + sh2
        if ctx_live:
            hc = rmsnorm(cx, ffn_norm_g[i]) * (1.0 + csc2) + csh2
            n_c = hc.shape[0] * hc.shape[1]
            out = moe_ffn(jnp.concatenate([hc.reshape(-1, d), hx.reshape(-1, d)], axis=0), router_w[i], router_b[i],
                          moe_w_gu[i], moe_w_down[i], shared_w_gu[i], shared_w_down[i])
            cx = cx + cg2 * out[:n_c].reshape(cx.shape)
            x = x + g2 * out[n_c:].reshape(x.shape)
        else:
            out = moe_ffn(hx.reshape(-1, d), router_w[i], router_b[i], moe_w_gu[i], moe_w_down[i],
                          shared_w_gu[i], shared_w_down[i])
            x = x + g2 * out.reshape(x.shape)
    return rmsnorm(x, final_norm_g)
```

```python
import os
import numpy as np
import concourse.bass as bass
import concourse.mybir as mybir
from concourse.bass_utils import run_bass_kernel_spmd
from contextlib import ExitStack

F32 = mybir.dt.float32
BF16 = mybir.dt.bfloat16
AF = mybir.ActivationFunctionType
ALU = mybir.AluOpType
AX = mybir.AxisListType

D = 1024
KC = 8
SEQ = 8192
HALF = 4096
CTX = 256
RW = 1280
NJ = 10
NH = 5
NE = 64
EPS = 1e-6
NTOT = CTX + 2 * HALF

COMPUTE = ("pe", "act", "dve", "pool")
QUEUES = ("sp", "q2")
NSEM = 8
SAME_ENGINE_SYNC = True


class Buf:
    __slots__ = ("name", "w", "r")

    def __init__(self, name=""):
        self.name = name
        self.w = None
        self.r = []


class Op:
    __slots__ = ("eng", "fn", "deps", "signal", "val", "idx", "is_dma", "qn")

    def __init__(self, eng, fn, is_dma=False):
        self.eng = eng
        self.fn = fn
        self.deps = []
        self.signal = False
        self.val = None
        self.is_dma = is_dma
        self.qn = 0


class Prog:
    def __init__(self):
        self.streams = {"pe": [], "act": [], "dve": [], "pool": [], "sp": []}
        self.ndma = {"sp": 0, "q2": 0}
        self.ring = {"sp": [None] * NSEM, "q2": [None] * NSEM}

    @staticmethod
    def _stream_of(eng):
        return "pool" if eng == "q2" else eng

    def op(self, eng, fn, reads=(), writes=(), extra=()):
        is_dma = eng in QUEUES
        o = Op(eng, fn, is_dma)
        if is_dma:
            o.qn = self.ndma[eng]
            self.ndma[eng] += 1
            self.ring[eng][o.qn % NSEM] = o
        st = self._stream_of(eng)
        cand = []
        for b in reads:
            if b.w is not None:
                cand.append((b.w, True))
        for b in writes:
            if b.w is not None:
                cand.append((b.w, True))
            for r in b.r:
                cand.append((r, False))
        for d in extra:
            cand.append((d, True))
        seen = set()
        for d, strong in cand:
            if d is o or id(d) in seen:
                continue
            same = (not d.is_dma) and (not is_dma) and self._stream_of(d.eng) == st
            if same:
                if d.eng == "pe" or not SAME_ENGINE_SYNC:
                    continue
            seen.add(id(d))
            o.deps.append(d)
            d.signal = True
        for b in reads:
            b.r.append(o)
        for b in writes:
            b.w = o
            b.r = []
        o.idx = len(self.streams[st])
        self.streams[st].append(o)
        return o

    def barrier(self):
        last = []
        for st, ops in self.streams.items():
            for o in reversed(ops):
                if not o.is_dma:
                    last.append(o)
                    break
        for q in QUEUES:
            for o in self.ring[q]:
                if o is not None:
                    last.append(o)
        for st in ("pe", "act", "dve", "pool", "sp"):
            self.op(st, (lambda e: e.nop()), extra=[d for d in last])

    def assign(self):
        for st, ops in self.streams.items():
            cnt = 0
            for o in ops:
                if o.is_dma:
                    o.val = 16 * (o.qn // NSEM + 1)
                elif o.signal:
                    cnt += 1
                    o.val = cnt

    def emit_stream(self, st, engobj, sem_eng, sem_dma):
        waited = {}

        def wait(key, sem, val):
            if waited.get(key, 0) >= val:
                return
            engobj.wait_ge(sem, val)
            waited[key] = val

        for o in self.streams[st]:
            for d in o.deps:
                if d.is_dma:
                    wait((d.eng, d.qn % NSEM), sem_dma[d.eng][d.qn % NSEM], d.val)
                else:
                    dst = self._stream_of(d.eng)
                    wait(dst, sem_eng[dst], d.val)
            if o.is_dma:
                if o.qn >= NSEM:
                    wait((o.eng, o.qn % NSEM), sem_dma[o.eng][o.qn % NSEM], o.val - 16)
                ins = o.fn(engobj)
                ins.then_inc(sem_dma[o.eng][o.qn % NSEM], 16)
            else:
                ins = o.fn(engobj)
                if o.signal:
                    ins.then_inc(sem_eng[st], 1)

    def final_waits(self, engobj, sem_eng, sem_dma, ops):
        for d in ops:
            if d.is_dma:
                engobj.wait_ge(sem_dma[d.eng][d.qn % NSEM], d.val)
            else:
                engobj.wait_ge(sem_eng[self._stream_of(d.eng)], d.val)


def run_prog(nc, P, final_ops):
    P.assign()
    with ExitStack() as es:
        sem_eng = {s: es.enter_context(nc.semaphore("sem_" + s)) for s in COMPUTE}
        sem_dma = {q: [es.enter_context(nc.semaphore(f"semd_{q}_{i}")) for i in range(NSEM)]
                   for q in QUEUES}
        block = es.enter_context(nc.Block())

        @block.tensor
        def _(e):
            P.emit_stream("pe", e, sem_eng, sem_dma)

        @block.scalar
        def _(e):
            P.emit_stream("act", e, sem_eng, sem_dma)

        @block.vector
        def _(e):
            P.emit_stream("dve", e, sem_eng, sem_dma)

        @block.gpsimd
        def _(e):
            P.emit_stream("pool", e, sem_eng, sem_dma)

        @block.sync
        def _(e):
            P.emit_stream("sp", e, sem_eng, sem_dma)
            P.final_waits(e, sem_eng, sem_dma, final_ops)


class T:
    __slots__ = ("ap", "b")

    def __init__(self, ap, name=""):
        self.ap = ap
        self.b = Buf(name)


class Ring:
    def __init__(self, items):
        self.items = items
        self.i = 0

    def next(self):
        t = self.items[self.i % len(self.items)]
        self.i += 1
        return t


ARENA_COLS = 52992


class Builder:
    def __init__(self, stage=99, debug=False):
        self.stage = stage
        self.debug = debug
        self.nc = bass.Bass("TRN2", target_bir_lowering=False)
        self.P = Prog()
        self.outs = []
        self.es = ExitStack()

    def din(self, name, shape, dt=F32):
        return self.nc.dram_tensor(name, list(shape), dt, kind="ExternalInput").ap()

    def dscr(self, name, shape, dt=F32, out=False):
        kind = "ExternalOutput" if out else "Internal"
        return self.nc.dram_tensor(name, list(shape), dt, kind=kind).ap()

    def alloc(self, ncols, dt=F32, name=""):
        n32 = ncols if dt == F32 else (ncols + 1) // 2
        assert self.top + n32 <= ARENA_COLS, f"arena overflow {self.top}+{n32} ({name})"
        ap = self.arena[:, self.top:self.top + n32]
        self.top += n32
        if dt != F32:
            ap = ap.bitcast(dt)
        return ap

    def tile(self, ncols, dt=F32, name=""):
        return T(self.alloc(ncols, dt, name), name)

    def dma(self, q, out, in_, reads=(), writes=(), slow=False):
        if slow:
            return self.P.op(q, lambda e: e.dma_start(out=out, in_=in_, allow_slow_non_contiguous=True), reads, writes)
        return self.P.op(q, lambda e: e.dma_start(out=out, in_=in_), reads, writes)

    def act(self, out, in_, func, reads=(), writes=(), **kw):
        return self.P.op("act", lambda e: e.activation(out=out, in_=in_, func=func, **kw), reads, writes)

    def ts(self, eng, out, in0, s1, op0, s2=None, op1=None, reads=(), writes=()):
        if op1 is None:
            return self.P.op(eng, lambda e: e.tensor_scalar(out=out, in0=in0, scalar1=s1, scalar2=None, op0=op0),
                             reads, writes)
        return self.P.op(eng, lambda e: e.tensor_scalar(out=out, in0=in0, scalar1=s1, scalar2=s2, op0=op0, op1=op1),
                         reads, writes)

    def tt(self, eng, out, in0, in1, op, reads=(), writes=()):
        return self.P.op(eng, lambda e: e.tensor_tensor(out=out, in0=in0, in1=in1, op=op), reads, writes)

    def stt(self, out, in0, scalar, in1, op0, op1, reads=(), writes=()):
        return self.P.op("dve", lambda e: e.scalar_tensor_tensor(out=out, in0=in0, scalar=scalar, in1=in1,
                                                                 op0=op0, op1=op1), reads, writes)

    def mm(self, out, lhsT, rhs, start, stop, reads=(), writes=()):
        return self.P.op("pe", lambda e: e.matmul(out, lhsT, rhs, start=start, stop=stop), reads, writes)

    def tr(self, out, in_, ident, reads=(), writes=()):
        return self.P.op("pe", lambda e: e.transpose(out=out, in_=in_, identity=ident), reads, writes)

    def copy(self, eng, out, in_, reads=(), writes=()):
        if eng == "act":
            return self.P.op("act", lambda e: e.copy(out=out, in_=in_), reads, writes)
        return self.P.op(eng, lambda e: e.tensor_copy(out=out, in_=in_), reads, writes)

    def memset(self, eng, ap, val, writes=()):
        return self.P.op(eng, lambda e: e.memset(ap, val), (), writes)

    def declare(self):
        dbg = self.debug
        I = {}
        I["x_own"] = self.din("x_own", [HALF, D])
        I["x_oth"] = self.din("x_oth", [HALF, D])
        I["ctxb"] = self.din("ctxb", [CTX, D])
        I["pe_own"] = self.din("pe_own", [HALF, D])
        I["pe_oth"] = self.din("pe_oth", [HALF, D])
        I["cvec"] = self.din("cvec", [128, 16])
        I["flags"] = self.din("flags", [128, 2])
        I["ident"] = self.din("ident", [128, 128])
        I["ada_w"] = self.din("ada_w", [2, D, 6 * D])
        I["adab_fm"] = self.din("adab_fm", [128, 2 * 96])
        I["adab_row"] = self.din("adab_row", [1, 2 * 6 * D])
        I["ng"] = self.din("ng", [128, 4 * KC])
        I["fin_row"] = self.din("fin_row", [1, D])
        I["a_w_in"] = self.din("a_w_in", [D, 2 * RW])
        I["a_small"] = self.din("a_small", [128, 110])
        I["a_rw"] = self.din("a_rw", [2, NH, 256, 256])
        I["a_iw"] = self.din("a_iw", [2, NH, 256, 256])
        I["a_w_out"] = self.din("a_w_out", [RW, D])
        I["b_w_in"] = self.din("b_w_in", [D, 4096])
        I["b_ln_row"] = self.din("b_ln_row", [1, 4096])
        I["b_lng_fm"] = self.din("b_lng_fm", [128, 32])
        I["b_w_s"] = self.din("b_w_s", [8, 128, 128])
        I["b_bs_row"] = self.din("b_bs_row", [1, 1024])
        I["b_w_out"] = self.din("b_w_out", [2048, D])
        I["router_w"] = self.din("router_w", [2, D, NE])
        I["rb_row"] = self.din("rb_row", [1, 2 * NE])
        if self.stage >= 3:
            I["moe_w_gu"] = self.din("moe_w_gu", [2, NE, D, 512])
            I["moe_w_down"] = self.din("moe_w_down", [2, NE, 256, D])
        I["sh_w_gu"] = self.din("sh_w_gu", [2, D, 512])
        I["sh_w_down"] = self.din("sh_w_down", [2, 256, D])
        self.I = I
        st = self.stage
        self.out = self.nc.dram_tensor("out", [HALF, D], F32, kind="ExternalOutput").ap()
        self.uT = self.dscr("uT", [NJ, 128, NTOT], F32, out=(dbg and st == 1))
        self.gg = self.dscr("gg", [NJ, 128, HALF], F32, out=(dbg and st == 1))
        self.ygT = self.dscr("ygT", [NJ, 128, HALF], BF16, out=(dbg and st == 2))
        self.XS = self.dscr("XS", [HALF, D], F32)
        self.dbgX = self.dscr("dbgX", [HALF, D], F32, out=True) if dbg else None
        self.GBC = self.dscr("GBC", [4, 128, D], F32, out=(dbg and st == 1))
        self.modd = self.dscr("modd", [128, 192], F32, out=(dbg and st == 1))
        self.uT_b = {}
        self.gg_b = {}
        self.yg_b = {}
        self.XS_b = [Buf(f"XS{t}") for t in range(32)]
        self.GBC_b = [Buf(f"GBC{i}") for i in range(4)]

    def build(self):
        nc = self.nc
        self.declare()
        es = self.es
        self.arena = es.enter_context(nc.sbuf_tensor("arena", [128, ARENA_COLS], F32))
        self.ps = es.enter_context(nc.psum_tensor("ps", [128, 4096], F32))
        self.psb = [Buf(f"bank{i}") for i in range(8)]
        self.top = 0
        self.phase_pro()
        self.const_top = self.top
        if self.stage >= 1:
            self.phase_a0()
        dbgskip = os.environ.get('SKIPSTAGES', '')
        if self.stage >= 2 and 'b0' not in dbgskip:
            self.P.barrier()
            self.top = self.const_top
            self.phase_b0()
        if self.stage >= 3 and 'm0' not in dbgskip:
            for hh in range(2):
                self.P.barrier()
                self.top = self.const_top
                self.phase_m(0, hh)
        if self.stage >= 4:
            self.P.barrier()
            self.top = self.const_top
            self.phase_s1()
        if self.stage >= 5:
            for hh in range(2):
                self.P.barrier()
                self.top = self.const_top
                self.phase_m(1, hh)
        if self.stage < 5:
            self.P.barrier()
            self.top = self.const_top
            z = self.tile(D, F32, "zero")
            self.memset("dve", z.ap, 0.0, writes=[z.b])
            for t in range(32):
                self.outs.append(self.dma("sp", self.out[t * 128:(t + 1) * 128, :], z.ap, reads=[z.b]))
        self.P.barrier()
        run_prog(nc, self.P, self.outs)
        es.close()
        return nc

    def bank(self, b, n=1):
        return self.ps[:, b * 512:(b + n) * 512]

    def phase_pro(self):
        I = self.I
        c = {}
        for name, n in [("ident", 128), ("cvec", 16), ("flags", 2), ("adab_fm", 192), ("ng", 32),
                        ("a_small", 110), ("b_lng_fm", 32)]:
            c[name] = self.tile(n, F32, name)
            self.dma("sp", c[name].ap, I[name], writes=[c[name].b])
        ones = self.tile(128, F32, "ones")
        self.memset("dve", ones.ap, 1.0, writes=[ones.b])
        self.ident, self.ones, self.flags = c["ident"], ones, c["flags"]
        silu = self.tile(16, F32, "silu_c")
        self.act(silu.ap, c["cvec"].ap, AF.Silu, reads=[c["cvec"].b], writes=[silu.b])
        mod = self.tile(192, F32, "mod")
        self.mod = mod
        fin_bc = self.tile(D, F32, "fin_bc")
        self.fin_bc = fin_bc
        rb_bc = self.tile(2 * NE, F32, "rb_bc")
        self.rb_bc = rb_bc
        self.A1 = [self.tile(KC, F32, f"A1_{i}") for i in range(2)]
        self.B1 = [self.tile(KC, F32, f"B1_{i}") for i in range(2)]
        self.A2 = [self.tile(KC, F32, f"A2_{i}") for i in range(2)]
        self.B2 = [self.tile(KC, F32, f"B2_{i}") for i in range(2)]
        self.A1c = self.tile(KC, F32, "A1c")
        self.B1c = self.tile(KC, F32, "B1c")
        self.asm = c["a_small"]
        self.nrb = self.tile(20, F32, "nrb")
        self.nib = self.tile(20, F32, "nib")
        self.c8 = self.tile(20, F32, "c8")
        self.c16 = self.tile(20, F32, "c16")
        self.neghalf = self.tile(8, F32, "neghalf")
        self.memset("dve", self.neghalf.ap, -0.5, writes=[self.neghalf.b])
        self.lng_fm = c["b_lng_fm"]
        mark = self.top
        srep = self.tile(KC * 128, F32, "silu_rep")
        for kc in range(KC):
            self.ts("dve", srep.ap[:, kc * 128:(kc + 1) * 128], ones.ap, silu.ap[:, 2 * kc:2 * kc + 1], ALU.mult,
                    reads=[ones.b, silu.b], writes=[srep.b])
        rows = self.tile(2 * 6 * D, F32, "adab_row")
        self.dma("sp", rows.ap[0:1, :], I["adab_row"], writes=[rows.b])
        frow = self.tile(D + 2 * NE, F32, "fin_row")
        self.dma("sp", frow.ap[0:1, 0:D], I["fin_row"], writes=[frow.b])
        self.dma("sp", frow.ap[0:1, D:D + 2 * NE], I["rb_row"], writes=[frow.b])
        adat = Ring([self.tile(KC * 512, F32, f"adat{i}") for i in range(2)])
        gst = Ring([self.tile(512, F32, f"gst{i}") for i in range(2)])
        for n in range(2):
            self.mm(self.bank(3)[:, 0:512], ones.ap[0:1, :], frow.ap[0:1, n * 512:(n + 1) * 512], True, True,
                    reads=[ones.b, frow.b], writes=[self.psb[3]])
            self.copy("dve", fin_bc.ap[:, n * 512:(n + 1) * 512], self.bank(3), reads=[self.psb[3]], writes=[fin_bc.b])
        self.mm(self.bank(3)[:, 0:2 * NE], ones.ap[0:1, :], frow.ap[0:1, D:D + 2 * NE], True, True,
                reads=[ones.b, frow.b], writes=[self.psb[3]])
        self.copy("dve", rb_bc.ap, self.bank(3)[:, 0:2 * NE], reads=[self.psb[3]], writes=[rb_bc.b])
        for i in range(2):
            psA = self.bank(0)[:, 0:96]
            for g in range(12):
                at = adat.next()
                src = I["ada_w"][i][:, g * 512:(g + 1) * 512].rearrange("(kc p) n -> p kc n", p=128)
                self.dma("sp" if g % 2 == 0 else "q2", at.ap.rearrange("p (kc n) -> p kc n", kc=KC), src, writes=[at.b])
                for fc in range(4):
                    col = (g * 4 + fc) * 2
                    for kc in range(KC):
                        self.mm(psA[:, col:col + 2], at.ap[:, kc * 512 + fc * 128: kc * 512 + (fc + 1) * 128],
                                silu.ap[:, 2 * kc:2 * kc + 2], kc == 0, kc == KC - 1,
                                reads=[at.b, silu.b], writes=[self.psb[0]])
                if g in (4, 5, 10, 11):
                    bk = 1 + (g % 2)
                    for kc in range(KC):
                        self.mm(self.bank(bk), srep.ap[:, kc * 128:(kc + 1) * 128], at.ap[:, kc * 512:(kc + 1) * 512],
                                kc == 0, False, reads=[srep.b, at.b], writes=[self.psb[bk]])
                    self.mm(self.bank(bk), ones.ap[0:1, :], rows.ap[0:1, i * 6 * D + g * 512: i * 6 * D + (g + 1) * 512],
                            False, True, reads=[ones.b, rows.b], writes=[self.psb[bk]])
                    s = gst.next()
                    self.copy("dve", s.ap, self.bank(bk), reads=[self.psb[bk]], writes=[s.b])
                    idx = i * 2 + (0 if g < 6 else 1)
                    self.dma("sp", self.GBC[idx][:, (g % 2) * 512:(g % 2 + 1) * 512], s.ap, reads=[s.b],
                             writes=[self.GBC_b[idx]])
            self.tt("dve", mod.ap[:, i * 96:(i + 1) * 96], psA, c["adab_fm"].ap[:, i * 96:(i + 1) * 96], ALU.add,
                    reads=[self.psb[0], c["adab_fm"].b], writes=[mod.b])
        if self.debug and self.stage == 1:
            self.dma("sp", self.modd, mod.ap, reads=[mod.b])

        def mcol(i, chunk0, which):
            base = i * 96 + chunk0 * 2 + which
            return mod.ap[:, base: base + 2 * KC: 2]
        ng = c["ng"].ap
        tmp = self.tile(KC, F32, "tmpk")
        for i in range(2):
            for (A, Bv, gi, sc_chunk, sh_chunk, which) in [
                    (self.A1[i], self.B1[i], 2 * i, 8, 0, 0), (self.A2[i], self.B2[i], 2 * i + 1, 32, 24, 0)]:
                self.ts("dve", tmp.ap, mcol(i, sc_chunk, which), 1.0, ALU.add, reads=[mod.b], writes=[tmp.b])
                self.tt("dve", A.ap, tmp.ap, ng[:, gi * KC:(gi + 1) * KC], ALU.mult, reads=[tmp.b, c["ng"].b], writes=[A.b])
                self.copy("dve", Bv.ap, mcol(i, sh_chunk, which), reads=[mod.b], writes=[Bv.b])
        self.ts("dve", tmp.ap, mcol(0, 8, 1), 1.0, ALU.add, reads=[mod.b], writes=[tmp.b])
        self.tt("dve", self.A1c.ap, tmp.ap, ng[:, 0:KC], ALU.mult, reads=[tmp.b, c["ng"].b], writes=[self.A1c.b])
        self.copy("dve", self.B1c.ap, mcol(0, 0, 1), reads=[mod.b], writes=[self.B1c.b])
        asm = self.asm
        self.ts("dve", self.nrb.ap, asm.ap[:, 50:70], -1.0, ALU.mult, reads=[asm.b], writes=[self.nrb.b])
        self.ts("dve", self.nib.ap, asm.ap[:, 70:90], -1.0, ALU.mult, reads=[asm.b], writes=[self.nib.b])
        t20 = self.tile(20, F32, "t20")
        self.act(t20.ap, asm.ap[:, 90:110], AF.Exp, reads=[asm.b], writes=[t20.b], scale=-1.0)
        self.act(t20.ap, t20.ap, AF.Ln, reads=[t20.b], writes=[t20.b], bias=1.0)
        self.ts("dve", self.c8.ap, t20.ap, -8.0, ALU.mult, reads=[t20.b], writes=[self.c8.b])
        self.ts("dve", self.c16.ap, t20.ap, -16.0, ALU.mult, reads=[t20.b], writes=[self.c16.b])
        self.P.barrier()
        self.top = mark

    def norm_tile(self, xt, A, Bv, dst, nb, dst32=None):
        st = self.norm_a(xt, nb)
        self.norm_b(st, A, Bv, dst, nb, dst32)

    def norm_a(self, xt, nb):
        junk, ss, rstd, xs = nb["junkr"].next(), nb["ss"].next(), nb["rstd"].next(), nb["xs"].next()
        self.act(junk.ap, xt.ap, AF.Square, reads=[xt.b], writes=[ss.b, junk.b], accum_out=ss.ap)
        self.ts("pool", rstd.ap, ss.ap, 1.0 / D, ALU.mult, EPS, ALU.add, reads=[ss.b], writes=[rstd.b])
        self.tt("pool", rstd.ap, rstd.ap, self.neghalf.ap[:, 0:1], ALU.pow, reads=[rstd.b, self.neghalf.b],
                writes=[rstd.b])
        self.ts("dve", xs.ap, xt.ap, rstd.ap, ALU.mult, reads=[xt.b, rstd.b], writes=[xs.b])
        return xs

    def norm_b(self, xs, A, Bv, dst, nb, dst32=None):
        bk = nb["bk"].next()
        for c in range(KC):
            b = bk + c // 4
            self.tr(self.bank(b)[:, (c % 4) * 128:(c % 4 + 1) * 128], xs.ap[:, c * 128:(c + 1) * 128],
                    self.ident.ap, reads=[xs.b, self.ident.b], writes=[self.psb[b]])
        for c in range(KC):
            b = bk + c // 4
            src = self.bank(b)[:, (c % 4) * 128:(c % 4 + 1) * 128]
            if dst32 is not None:
                ap32, b32 = dst32(c)
                self.ts("dve", ap32, src, A.ap[:, c:c + 1], ALU.mult, Bv.ap[:, c:c + 1], ALU.add,
                        reads=[self.psb[b], A.b, Bv.b], writes=[b32])
                continue
            ap, bb = dst(c)
            self.act(ap, src, AF.Identity, reads=[self.psb[b], A.b, Bv.b], writes=[bb],
                     scale=A.ap[:, c:c + 1], bias=Bv.ap[:, c:c + 1])

    def norm_bufs(self, banks=((0, 2)), nxs=2):
        nb = {}
        nb["junkr"] = Ring([self.tile(D, BF16, f"junk{i}") for i in range(2)])
        nb["junk"] = nb["junkr"].items[0]
        nb["ss"] = Ring([self.tile(1, F32, f"ss{i}") for i in range(8)])
        nb["rstd"] = Ring([self.tile(1, F32, f"rstd{i}") for i in range(8)])
        nb["xs"] = Ring([self.tile(D, F32, f"xs{i}") for i in range(nxs)])
        nb["bk"] = Ring(list(banks))
        return nb

    def phase_a0(self):
        I = self.I
        winb = self.tile(KC * 2 * RW, BF16, "a_w_in_b")
        self.dma("q2", winb.ap.rearrange("p (kc n) -> p kc n", kc=KC),
                 I["a_w_in"].rearrange("(kc p) n -> p kc n", p=128), writes=[winb.b])
        SKEW = 3
        xts = Ring([self.tile(D, F32, f"xt{i}") for i in range(SKEW + 1)])
        pets = Ring([self.tile(D, F32, f"pet{i}") for i in range(SKEW + 1)])
        nb = self.norm_bufs(banks=(0, 2), nxs=SKEW + 1)
        pend = []
        hx = [[[T(None, f"hx{s}_{c}_{t}") for t in range(4)] for c in range(KC)] for s in range(2)]
        hx_ap = [self.alloc(KC * 512, BF16, f"hxblk{s}") for s in range(2)]
        ust = Ring([self.tile(NJ * 512, F32, f"ust{i}") for i in range(2)])
        gst = Ring([self.tile(NJ * 512, F32, f"gst{i}") for i in range(2)])
        pbank = Ring([4, 5, 6, 7])
        segs = [("ctx", I["ctxb"], None, 0, CTX, self.A1c, self.B1c),
                ("oth", I["x_oth"], I["pe_oth"], CTX, HALF, self.A1[0], self.B1[0]),
                ("own", I["x_own"], I["pe_own"], CTX + HALF, HALF, self.A1[0], self.B1[0])]
        blk_i = 0
        for (sname, xsrc, pesrc, col0, ntok, A, Bv) in segs:
            nblk = (ntok + 511) // 512
            for blk in range(nblk):
                ncol = min(512, ntok - blk * 512)
                s = blk_i % 2
                blk_i += 1
                hap = hx_ap[s].rearrange("p (kc n) -> p kc n", kc=KC)
                for tt_ in range(ncol // 128):
                    t = blk * 4 + tt_
                    xt = xts.next()
                    self.dma("sp", xt.ap, xsrc[t * 128:(t + 1) * 128, :], writes=[xt.b])
                    if pesrc is not None:
                        pet = pets.next()
                        self.dma("sp", pet.ap, pesrc[t * 128:(t + 1) * 128, :], writes=[pet.b])
                        self.tt("pool", xt.ap, xt.ap, pet.ap, ALU.add, reads=[xt.b, pet.b], writes=[xt.b])
                    xs_ = self.norm_a(xt, nb)
                    pend.append(("norm", xs_, A, Bv,
                                 (lambda c, tt_=tt_, hap=hap, s=s: (hap[:, c, tt_ * 128:(tt_ + 1) * 128], hx[s][c][tt_].b)),
                                 (xt, t) if sname == "own" else None))
                    if tt_ == ncol // 128 - 1:
                        pend.append(("proj", sname, blk, ncol, s, hap, col0))
                    while len([p for p in pend if p[0] == "norm"]) > SKEW:
                        self._a0_drain(pend, nb, winb, hx, ust, gst, pbank)
        while pend:
            self._a0_drain(pend, nb, winb, hx, ust, gst, pbank)

    def _a0_drain(self, pend, nb, winb, hx, ust, gst, pbank):
        it = pend.pop(0)
        if it[0] == "norm":
            _, xs_, A, Bv, dst, st = it
            if st is not None:
                xt, t = st
                self.dma("sp", self.XS[t * 128:(t + 1) * 128, :], xt.ap, reads=[xt.b], writes=[self.XS_b[t]])
                if self.debug and self.stage == 1:
                    self.dma("sp", self.dbgX[t * 128:(t + 1) * 128, :], xt.ap, reads=[xt.b])
            self.norm_b(xs_, A, Bv, dst, nb)
            return
        _, sname, blk, ncol, s, hap, col0 = it
        if True:
            if True:
                nt = ncol // 128
                us = ust.next()
                usv = us.ap.rearrange("p (j n) -> p j n", j=NJ)
                for j in range(NJ):
                    bk = pbank.next()
                    for kc in range(KC):
                        self.mm(self.bank(bk)[:, 0:ncol], winb.ap[:, kc * 2 * RW + RW + j * 128: kc * 2 * RW + RW + (j + 1) * 128],
                                hap[:, kc, 0:ncol], kc == 0, kc == KC - 1,
                                reads=[winb.b] + [hx[s][kc][q].b for q in range(nt)], writes=[self.psb[bk]])
                    self.copy("dve", usv[:, j, 0:ncol], self.bank(bk)[:, 0:ncol], reads=[self.psb[bk]], writes=[us.b])
                key = (sname, blk)
                self.uT_b[key] = Buf(f"uT{key}")
                self.dma("sp", self.uT.rearrange("j p n -> p j n")[:, :, col0 + blk * 512: col0 + blk * 512 + ncol],
                         usv[:, :, 0:ncol], reads=[us.b], writes=[self.uT_b[key]])
                if sname == "own":
                    gs = gst.next()
                    gsv = gs.ap.rearrange("p (j n) -> p j n", j=NJ)
                    for j in range(NJ):
                        bk = pbank.next()
                        for kc in range(KC):
                            self.mm(self.bank(bk), winb.ap[:, kc * 2 * RW + j * 128: kc * 2 * RW + (j + 1) * 128],
                                    hap[:, kc, :], kc == 0, kc == KC - 1,
                                    reads=[winb.b] + [hx[s][kc][q].b for q in range(4)], writes=[self.psb[bk]])
                        self.act(gsv[:, j, :], self.bank(bk), AF.Gelu_apprx_tanh, reads=[self.psb[bk]], writes=[gs.b])
                    self.gg_b[blk] = Buf(f"gg{blk}")
                    self.dma("sp", self.gg.rearrange("j p n -> p j n")[:, :, blk * 512:(blk + 1) * 512], gsv,
                             reads=[gs.b], writes=[self.gg_b[blk]])

    def phase_b0(self):
        I = self.I
        asm = self.asm
        rwb = self.tile(2 * NH * 2 * 256, BF16, "rwb")
        iwb = self.tile(2 * NH * 2 * 256, BF16, "iwb")
        for (wt, src) in ((rwb, I["a_rw"]), (iwb, I["a_iw"])):
            for d in range(2):
                self.dma("q2", wt.ap[:, d * NH * 512:(d + 1) * NH * 512].rearrange("p (h kc n) -> p h kc n", h=NH, kc=2),
                         src[d].rearrange("h (kc p) n -> p h kc n", p=128), writes=[wt.b])

        def gw(wt, d, h, kc, co):
            base = ((d * NH + h) * 2 + kc) * 256 + co * 128
            return wt.ap[:, base:base + 128]
        Ust = [self.tile(HALF + 3, F32, "Ust0")]
        UCs = [self.tile(2 * HALF, F32, f"UC{i}") for i in range(2)]
        UCbs = [self.tile(2 * HALF, BF16, f"UCb{i}") for i in range(2)]
        UC_b = [[[Buf(f"UC{p}_{c}_{b}") for b in range(8)] for c in range(2)] for p in range(2)]
        UCb_b = [[[Buf(f"UCb{p}_{c}_{b}") for b in range(8)] for c in range(2)] for p in range(2)]
        cbuf = []
        for co_ in range(2):
            cbuf.append({
                "er": Ring([self.tile(512, F32, f"er{co_}{i}") for i in range(1)]),
                "ei": Ring([self.tile(512, F32, f"ei{co_}{i}") for i in range(1)]),
                "a": Ring([self.tile(512, F32, f"a{co_}{i}") for i in range(1)]),
                "m": Ring([self.tile(512, F32, f"m{co_}{i}") for i in range(1)]),
                "yd": Ring([self.tile(512, F32, f"yd{co_}{i}") for i in range(2)]),
                "stmp": self.tile(1, F32, f"stmp{co_}"),
                "stmp2": self.tile(1, F32, f"stmp2{co_}"),
                "pairs": Ring([(2 * co_, 2 * co_ + 1)]),
            })
        Yf = [self.alloc(HALF, F32, f"Yf{c}") for c in range(2)]
        Yf_b = [[Buf(f"Yf{c}_{i}") for i in range(8)] for c in range(2)]
        ggt_r = Ring([self.tile(512, F32, f"ggt{i}") for i in range(2)])
        ygb_r = Ring([self.tile(512, BF16, f"ygb{i}") for i in range(2)])
        s_ctx = [[self.tile(1, F32, f"sctx{c}{d}") for d in range(2)] for c in range(2)]
        s_oth = [[self.tile(1, F32, f"soth{c}{d}") for d in range(2)] for c in range(2)]
        s_ini = [[self.tile(1, F32, f"sini{c}{d}") for d in range(2)] for c in range(2)]
        s_tmp = self.tile(1, F32, "stmp")
        pairs = Ring([(0, 1), (2, 3), (4, 5), (6, 7)])
        fl = self.flags
        own0 = CTX + HALF
        oth0 = CTX
        segs = [("ctx", 0, CTX), ("oth", CTX, HALF), ("own", CTX + HALF, HALF)]
        items = [(h, sg) for h in range(NH) for sg in segs]

        dg = self.tile(8 * 128, F32, "convdiag")
        cvb = Ring([4, 5, 6, 7])

        def conv_gen(idx):
            h, (sname, col0, n) = items[idx]
            par = idx % 2
            nblk = (n + 511) // 512
            UCv = UCs[par].ap.rearrange("p (c n) -> p c n", c=2)
            UCbv = UCbs[par].ap.rearrange("p (c n) -> p c n", c=2)
            if sname == "ctx":
                for cc in range(2):
                    for k in range(4):
                        jj = 2 * h + cc
                        self.ts("dve", dg.ap[:, (cc * 4 + k) * 128:(cc * 4 + k + 1) * 128], self.ident.ap,
                                asm.ap[:, jj * 4 + k: jj * 4 + k + 1], ALU.mult, reads=[self.ident.b, asm.b], writes=[dg.b])
            for cc in range(2):
                j = 2 * h + cc
                U = Ust[0]
                srcb = [self.uT_b[(sname, b)] for b in range(nblk)]
                self.dma("sp", U.ap[:, 2:2 + n], self.uT[j][:, col0:col0 + n], reads=srcb, writes=[U.b])
                if sname == "ctx":
                    self.memset("dve", U.ap[:, 0:2], 0.0, writes=[U.b])
                    self.memset("dve", U.ap[:, n + 2:n + 3], 0.0, writes=[U.b])
                else:
                    if sname == "oth":
                        lsrc, lfl, rsrc, rfl = own0 + HALF - 2, 0, own0, 1
                        lb, rb = self.uT_b[("own", 7)], self.uT_b[("own", 0)]
                    else:
                        lsrc, lfl, rsrc, rfl = oth0 + HALF - 2, 1, oth0, 0
                        lb, rb = self.uT_b[("oth", 7)], self.uT_b[("oth", 0)]
                    self.dma("sp", U.ap[:, 0:2], self.uT[j][:, lsrc:lsrc + 2], reads=[lb], writes=[U.b])
                    self.dma("sp", U.ap[:, n + 2:n + 3], self.uT[j][:, rsrc:rsrc + 1], reads=[rb], writes=[U.b], slow=True)
                    self.ts("dve", U.ap[:, 0:2], U.ap[:, 0:2], fl.ap[:, lfl:lfl + 1], ALU.mult,
                            reads=[U.b, fl.b], writes=[U.b])
                    self.ts("dve", U.ap[:, n + 2:n + 3], U.ap[:, n + 2:n + 3], fl.ap[:, rfl:rfl + 1], ALU.mult,
                            reads=[U.b, fl.b], writes=[U.b])
                w = lambda k: asm.ap[:, j * 4 + k: j * 4 + k + 1]
                for blk in range(nblk):
                    ncol = min(512, n - blk * 512)
                    c0 = blk * 512
                    ucv = UCv[:, cc, c0:c0 + ncol]
                    ub = UC_b[par][cc][blk]
                    bk = cvb.next()
                    for k in range(4):
                        self.mm(self.bank(bk)[:, 0:ncol], dg.ap[:, (cc * 4 + k) * 128:(cc * 4 + k + 1) * 128],
                                U.ap[:, c0 + k:c0 + k + ncol], k == 0, k == 3, reads=[dg.b, U.b], writes=[self.psb[bk]])
                    self.ts("dve", ucv, self.bank(bk)[:, 0:ncol], asm.ap[:, 40 + j:41 + j], ALU.add,
                            reads=[self.psb[bk], asm.b], writes=[ub])
                    self.copy("pool", UCbv[:, cc, c0:c0 + ncol], ucv, reads=[ub], writes=[UCb_b[par][cc][blk]])
                    yield

        def run_all(g):
            for _ in g:
                pass

        run_all(conv_gen(0))
        for idx in range(len(items)):
            if True:
                h, (sname, col0, n) = items[idx]
                par = idx % 2
                nblk = (n + 511) // 512
                UCv = UCs[par].ap.rearrange("p (c n) -> p c n", c=2)
                UCbv = UCbs[par].ap.rearrange("p (c n) -> p c n", c=2)
                cgen = conv_gen(idx + 1) if idx + 1 < len(items) else iter(())
                def chain(co, d, sname=sname, n=n, nblk=nblk, h=h, par=par, UCv=UCv, UCbv=UCbv):
                    j = 2 * h + co
                    bcol = d * NJ + j
                    cb = cbuf[co]
                    if sname == "ctx":
                        init = (0.0, None)
                    elif sname == "oth":
                        init = (s_ctx[co][d].ap, s_ctx[co][d].b)
                    else:
                        f = fl.ap[:, 1:2] if d == 0 else fl.ap[:, 0:1]
                        st_ = cb["stmp"]
                        self.tt("dve", st_.ap, s_oth[co][d].ap, s_ctx[co][d].ap, ALU.subtract,
                                reads=[s_oth[co][d].b, s_ctx[co][d].b], writes=[st_.b])
                        self.stt(s_ini[co][d].ap, st_.ap, f, s_ctx[co][d].ap, ALU.mult, ALU.add,
                                 reads=[st_.b, fl.b, s_ctx[co][d].b], writes=[s_ini[co][d].b])
                        init = (s_ini[co][d].ap, s_ini[co][d].b)
                    order = list(range(nblk)) if d == 0 else list(range(nblk - 1, -1, -1))
                    for bi, blk in enumerate(order):
                        ncol = min(512, n - blk * 512)
                        cols = slice(blk * 512, blk * 512 + ncol)
                        pr, pi = cb["pairs"].next()
                        for (pb, wt) in ((pr, rwb), (pi, iwb)):
                            for kc in range(2):
                                self.mm(self.bank(pb)[:, 0:ncol], gw(wt, d, h, kc, co), UCbv[:, kc, cols], kc == 0, kc == 1,
                                        reads=[wt.b, UCb_b[par][0][blk], UCb_b[par][1][blk]], writes=[self.psb[pb]])
                        yield
                        er, ei, a, m = cb["er"].next(), cb["ei"].next(), cb["a"].next(), cb["m"].next()
                        erv, eiv, av, mv = er.ap[:, 0:ncol], ei.ap[:, 0:ncol], a.ap[:, 0:ncol], m.ap[:, 0:ncol]
                        self.act(erv, self.bank(pr)[:, 0:ncol], AF.Exp, reads=[self.psb[pr], self.nrb.b], writes=[er.b],
                                 scale=-1.0, bias=self.nrb.ap[:, bcol:bcol + 1])
                        yield
                        self.act(erv, erv, AF.Ln, reads=[er.b], writes=[er.b], bias=1.0)
                        yield
                        self.act(erv, erv, AF.Exp, reads=[er.b], writes=[er.b], scale=-1.0)
                        yield
                        self.act(av, erv, AF.Exp, reads=[er.b, self.c8.b], writes=[a.b], scale=self.c8.ap[:, bcol:bcol + 1])
                        self.tt("pool", mv, av, av, ALU.mult, reads=[a.b], writes=[m.b])
                        yield
                        self.act(eiv, self.bank(pi)[:, 0:ncol], AF.Exp, reads=[self.psb[pi], self.nib.b], writes=[ei.b],
                                 scale=-1.0, bias=self.nib.ap[:, bcol:bcol + 1])
                        yield
                        self.act(eiv, eiv, AF.Ln, reads=[ei.b], writes=[ei.b], bias=1.0)
                        yield
                        first_ctx = (sname == "ctx" and bi == 0)
                        if first_ctx:
                            c0 = 0 if d == 0 else ncol - 1
                            ig0 = cb["stmp2"]
                            self.act(ig0.ap, ei.ap[:, c0:c0 + 1], AF.Exp, reads=[ei.b], writes=[ig0.b], scale=-1.0)
                        self.act(mv, mv, AF.Ln, reads=[m.b], writes=[m.b], scale=-1.0, bias=1.0)
                        yield
                        self.stt(eiv, mv, 0.5, eiv, ALU.mult, ALU.subtract, reads=[m.b, ei.b], writes=[ei.b])
                        self.act(eiv, eiv, AF.Exp, reads=[ei.b], writes=[ei.b])
                        yield
                        self.tt("dve", mv, eiv, UCv[:, co, cols], ALU.mult, reads=[ei.b, UC_b[par][co][blk]], writes=[m.b])
                        if first_ctx:
                            self.tt("dve", m.ap[:, c0:c0 + 1], ig0.ap, UCv[:, co, blk * 512 + c0: blk * 512 + c0 + 1], ALU.mult,
                                    reads=[ig0.b, UC_b[par][co][blk], m.b], writes=[m.b])
                        if sname == "own" and d == 0:
                            yap, yb = Yf[co][:, cols], Yf_b[co][blk]
                        else:
                            yd = cb["yd"].next()
                            yap, yb = yd.ap[:, 0:ncol], yd.b
                        ini_ap, ini_b = init
                        rd = [a.b, m.b] + ([ini_b] if ini_b is not None else [])
                        if d == 0:
                            self.P.op("dve", lambda e, o=yap, d0=av, d1=mv, ii=ini_ap: e.tensor_tensor_scan(
                                out=o, data0=d0, data1=d1, initial=ii, op0=ALU.mult, op1=ALU.add), rd, [yb])
                            init = (yap[:, ncol - 1:ncol], yb)
                        else:
                            self.P.op("dve", lambda e, o=yap, d0=av, d1=mv, ii=ini_ap: e.tensor_tensor_scan(
                                out=o[:, ::-1], data0=d0[:, ::-1], data1=d1[:, ::-1], initial=ii, op0=ALU.mult, op1=ALU.add),
                                rd, [yb])
                            init = (yap[:, 0:1], yb)
                        if sname == "own" and d == 1:
                            self.tt("pool", Yf[co][:, cols], Yf[co][:, cols], yap, ALU.add, reads=[Yf_b[co][blk], yb],
                                    writes=[Yf_b[co][blk]])
                        yield
                    if sname == "ctx":
                        self.copy("dve", s_ctx[co][d].ap, init[0], reads=[init[1]], writes=[s_ctx[co][d].b])
                    elif sname == "oth":
                        self.copy("dve", s_oth[co][d].ap, init[0], reads=[init[1]], writes=[s_oth[co][d].b])

                stepn = 0
                for d in range(2):
                    gens = [chain(0, d), chain(1, d)]
                    while gens:
                        for g_ in list(gens):
                            try:
                                next(g_)
                            except StopIteration:
                                gens.remove(g_)
                            stepn += 1
                            if stepn % 24 == 0:
                                next(cgen, None)
                run_all(cgen)
                if sname == "own":
                    for co in range(2):
                        j = 2 * h + co
                        for blk in range(8):
                            cols = slice(blk * 512, (blk + 1) * 512)
                            ggt, ygb = ggt_r.next(), ygb_r.next()
                            self.dma("sp", ggt.ap, self.gg[j][:, cols], reads=[self.gg_b[blk]], writes=[ggt.b])
                            self.tt("pool", ygb.ap, ggt.ap, Yf[co][:, cols], ALU.mult, reads=[ggt.b, Yf_b[co][blk]], writes=[ygb.b])
                            key = (j, blk)
                            self.yg_b[key] = Buf(f"yg{key}")
                            self.dma("sp", self.ygT[j][:, cols], ygb.ap, reads=[ygb.b], writes=[self.yg_b[key]])

    def phase_m(self, L, hh):
        I = self.I
        NT = 16
        xacc = self.alloc(NT * D, F32, "xacc")
        xa = [T(xacc[:, t * D:(t + 1) * D], f"xacc{t}") for t in range(NT)]
        hxT = self.alloc(KC * 2048, BF16, "hxT").rearrange("p (kc n) -> p kc n", kc=KC)
        hx_b = [[Buf(f"hx{kc}_{t}") for t in range(NT)] for kc in range(KC)]
        gates = self.alloc(NT * NE, F32, "gates")
        gate_b = [Buf(f"gate{t}") for t in range(NT)]
        G2 = self.tile(D, F32, "G2bc")
        self.dma("sp", G2.ap, self.GBC[2 * L + 1], reads=[self.GBC_b[2 * L + 1]], writes=[G2.b])
        rw = self.tile(KC * NE, F32, "rw")
        self.dma("sp", rw.ap.rearrange("p (kc n) -> p kc n", kc=KC), I["router_w"][L].rearrange("(kc p) n -> p kc n", p=128),
                 writes=[rw.b])
        ot_r = Ring([self.tile(D, F32, f"ot{i}") for i in range(2)])
        ep_junk = self.tile(D, BF16, "ep_junk")
        ep_ss = Ring([self.tile(1, F32, f"ep_ss{i}") for i in range(4)])
        ep_rstd = Ring([self.tile(1, F32, f"ep_rstd{i}") for i in range(4)])
        mark = self.top
        MSKEW = 2
        nb = self.norm_bufs(banks=(0, 2), nxs=MSKEW + 1)
        pendn = []
        hx32_r = Ring([self.tile(KC * 128, F32, f"hx32_{i}") for i in range(2)])
        hx32_b = [[Buf(f"hx32_{i}_{c}") for c in range(KC)] for i in range(2)]
        sm = {n: self.tile(w, F32, n) for n, w in [("th", 64), ("sc", 64), ("sel", 64), ("mx", 64), ("gs", 8), ("g8", 8),
                                                    ("pen", 8), ("selm", 64), ("t8", 8), ("mask", 64), ("den", 1)]}
        obanks = Ring([(4, 5), (6, 7)])
        skipP = 'prologue' in os.environ.get('MSKIP', '')
        if L == 0 and not skipP:
            G1 = self.tile(D, F32, "G1bc")
            self.dma("sp", G1.ap, self.GBC[0], reads=[self.GBC_b[0]], writes=[G1.b])
            woutb = self.tile(NJ * D, BF16, "woutb")
            wst = Ring([self.tile(D, F32, f"wst{i}") for i in range(2)])
            for jc in range(NJ):
                st = wst.next()
                self.dma("sp", st.ap, I["a_w_out"][jc * 128:(jc + 1) * 128, :], writes=[st.b])
                self.tt("pool", woutb.ap[:, jc * D:(jc + 1) * D], st.ap, G1.ap, ALU.mult, reads=[st.b, G1.b], writes=[woutb.b])
            ygt_r = Ring([self.tile(NJ * 128, BF16, f"ygt{i}") for i in range(2)])
        stage_b = lambda t_, xs_: self._m_router(L, t_, xs_, nb, hxT, hx_b, hx32_r, hx32_b, rw, sm, obanks, gates, gate_b)
        for t in range(NT):
            tg = hh * NT + t
            self.dma("sp", xa[t].ap, self.XS[tg * 128:(tg + 1) * 128, :], reads=[self.XS_b[tg]], writes=[xa[t].b])
            if L == 0 and not skipP:
                ygt = ygt_r.next()
                self.dma("sp", ygt.ap.rearrange("p (j n) -> p j n", j=NJ),
                         self.ygT.rearrange("j p n -> p j n")[:, :, tg * 128:(tg + 1) * 128],
                         reads=[self.yg_b[(j, tg // 4)] for j in range(NJ)], writes=[ygt.b])
                ob = obanks.next()
                for n in range(2):
                    for jc in range(NJ):
                        self.mm(self.bank(ob[n]), ygt.ap[:, jc * 128:(jc + 1) * 128],
                                woutb.ap[:, jc * D + n * 512: jc * D + (n + 1) * 512], jc == 0, jc == NJ - 1,
                                reads=[ygt.b, woutb.b], writes=[self.psb[ob[n]]])
                self.tt("dve", xa[t].ap, self.bank(ob[0], 2), xa[t].ap, ALU.add,
                        reads=[self.psb[ob[0]], self.psb[ob[1]], xa[t].b], writes=[xa[t].b])
            xs_ = self.norm_a(xa[t], nb)
            pendn.append((t, xs_))
            if len(pendn) > MSKEW:
                stage_b(*pendn.pop(0))
        while pendn:
            stage_b(*pendn.pop(0))
        self.P.barrier()
        self.top = mark
        self._m_experts(L, hh, xa, hxT, hx_b, gates, gate_b, G2, obanks, ot_r, ep_junk, ep_ss, ep_rstd)

    def _m_router(self, L, t, xs_, nb, hxT, hx_b, hx32_r, hx32_b, rw, sm, obanks, gates, gate_b):
        if True:
            si = t % 2
            h32 = hx32_r.next()
            h32v = h32.ap.rearrange("p (kc n) -> p kc n", kc=KC)
            self.norm_b(xs_, self.A2[L], self.B2[L],
                        lambda c, t=t: (hxT[:, c, t * 128:(t + 1) * 128], hx_b[c][t]), nb,
                        dst32=lambda c, h32v=h32v, si=si: (h32v[:, c, :], hx32_b[si][c]))
            self.copy("act", hxT[:, :, t * 128:(t + 1) * 128], h32v, reads=[hx32_b[si][c] for c in range(KC)],
                      writes=[hx_b[c][t] for c in range(KC)])
            ob = obanks.next()
            lg = self.bank(ob[0])[:, 0:NE]
            for kc in range(KC):
                self.mm(lg, h32v[:, kc, :], rw.ap[:, kc * NE:(kc + 1) * NE], kc == 0, kc == KC - 1,
                        reads=[hx32_b[si][kc], rw.b], writes=[self.psb[ob[0]]])
            th, sc, sel, mx, gs, g8, pen, selm, t8, mask, den = [sm[k] for k in
                                                                 ("th", "sc", "sel", "mx", "gs", "g8", "pen", "selm", "t8", "mask", "den")]
            self.act(th.ap, lg, AF.Tanh, reads=[self.psb[ob[0]]], writes=[th.b], scale=0.5)
            self.ts("dve", sc.ap, th.ap, 0.5, ALU.mult, 0.5, ALU.add, reads=[th.b], writes=[sc.b])
            self.tt("dve", sel.ap, sc.ap, self.rb_bc.ap[:, L * NE:(L + 1) * NE], ALU.add, reads=[sc.b, self.rb_bc.b], writes=[sel.b])
            for g in range(8):
                self.P.op("dve", lambda e, o=mx.ap[:, g * 8:(g + 1) * 8], i_=sel.ap[:, g * 8:(g + 1) * 8]: e.max(out=o, in_=i_),
                          [sel.b], [mx.b])
            mxv = mx.ap.rearrange("p (g k) -> p g k", k=8)
            self.tt("dve", gs.ap, mxv[:, :, 0], mxv[:, :, 1], ALU.add, reads=[mx.b], writes=[gs.b])
            self.P.op("dve", lambda e, o=g8.ap, i_=gs.ap: e.max(out=o, in_=i_), [gs.b], [g8.b])
            self.ts("dve", pen.ap, gs.ap, g8.ap[:, 3:4], ALU.is_ge, reads=[gs.b, g8.b], writes=[pen.b])
            self.ts("dve", pen.ap, pen.ap, -1.0, ALU.add, 1e30, ALU.mult, reads=[pen.b], writes=[pen.b])
            for g in range(8):
                self.ts("dve", selm.ap[:, g * 8:(g + 1) * 8], sel.ap[:, g * 8:(g + 1) * 8], pen.ap[:, g:g + 1], ALU.add,
                        reads=[sel.b, pen.b], writes=[selm.b])
            self.P.op("dve", lambda e, o=t8.ap, i_=selm.ap: e.max(out=o, in_=i_), [selm.b], [t8.b])
            self.ts("dve", mask.ap, selm.ap, t8.ap[:, 7:8], ALU.is_ge, reads=[selm.b, t8.b], writes=[mask.b])
            self.tt("dve", mask.ap, mask.ap, sc.ap, ALU.mult, reads=[mask.b, sc.b], writes=[mask.b])
            self.P.op("dve", lambda e, o=den.ap, i_=mask.ap: e.tensor_reduce(out=o, in_=i_, axis=AX.X, op=ALU.add),
                      [mask.b], [den.b])
            self.P.op("dve", lambda e, o=den.ap: e.reciprocal(out=o, in_=o), [den.b], [den.b])
            self.ts("dve", gates[:, t * NE:(t + 1) * NE], mask.ap, den.ap, ALU.mult, 2.5, ALU.mult,
                    reads=[mask.b, den.b], writes=[gate_b[t]])

    def _m_experts(self, L, hh, xa, hxT, hx_b, gates, gate_b, G2, obanks, ot_r, ep_junk, ep_ss, ep_rstd):
        I = self.I
        NT = 16
        wgu_r = Ring([self.tile(KC * 512, BF16, f"wgu{i}") for i in range(2)])
        wdf_r = Ring([self.tile(2 * D, F32, f"wdf{i}") for i in range(2)])
        wdb_r = Ring([self.tile(2 * D, BF16, f"wdb{i}") for i in range(2)])
        s_r = Ring([self.tile(512, F32, f"s{i}") for i in range(2)])
        actT_r = Ring([[self.tile(512, BF16, f"actT{i}_{f}") for f in range(2)] for i in range(3)])
        gubanks = Ring([(0, 1), (2, 3)])
        elist = list(range(NE + 1)) if 'NE_DBG' not in os.environ else list(range(int(os.environ['NE_DBG']))) + [NE]
        if 'expert' in os.environ.get('MSKIP', ''):
            elist = []
        pend_down = None
        for e in elist:
            wgu, wdf, wdb = wgu_r.next(), wdf_r.next(), wdb_r.next()
            src_gu = I["moe_w_gu"][L, e] if e < NE else I["sh_w_gu"][L]
            src_dn = I["moe_w_down"][L, e] if e < NE else I["sh_w_down"][L]
            wguv = wgu.ap.rearrange("p (kc n) -> p kc n", kc=KC)
            self.dma("q2", wguv, src_gu.rearrange("(kc p) n -> p kc n", p=128), writes=[wgu.b])
            self.dma("sp", wdf.ap.rearrange("p (f n) -> p f n", f=2), src_dn.rearrange("(f p) n -> p f n", p=128), writes=[wdf.b])
            for fh in range(2):
                self.tt("pool", wdb.ap[:, fh * D:(fh + 1) * D], wdf.ap[:, fh * D:(fh + 1) * D], G2.ap, ALU.mult,
                        reads=[wdf.b, G2.b], writes=[wdb.b])
            for blk in range(4):
                cols = slice(blk * 512, (blk + 1) * 512)
                actT = actT_r.next()
                for fh in range(2):
                    bg, bu = gubanks.next()
                    rd = [wgu.b]
                    for kc in range(KC):
                        rdk = rd + [hx_b[kc][blk * 4 + q] for q in range(4)]
                        self.mm(self.bank(bg), wguv[:, kc, fh * 128:(fh + 1) * 128], hxT[:, kc, cols], kc == 0, kc == KC - 1,
                                reads=rdk, writes=[self.psb[bg]])
                    for kc in range(KC):
                        rdk = rd + [hx_b[kc][blk * 4 + q] for q in range(4)]
                        self.mm(self.bank(bu), wguv[:, kc, 256 + fh * 128:256 + (fh + 1) * 128], hxT[:, kc, cols], kc == 0,
                                kc == KC - 1, reads=rdk, writes=[self.psb[bu]])
                    s = s_r.next()
                    self.act(s.ap, self.bank(bg), AF.Silu, reads=[self.psb[bg]], writes=[s.b])
                    self.tt("dve", actT[fh].ap, self.bank(bu), s.ap, ALU.mult, reads=[self.psb[bu], s.b], writes=[actT[fh].b])
                def down(blk=blk, actT=actT, wdb=wdb, e=e):
                    for q in range(4):
                        t = blk * 4 + q
                        ob = obanks.next()
                        for n in range(2):
                            for fh in range(2):
                                self.mm(self.bank(ob[n]), actT[fh].ap[:, q * 128:(q + 1) * 128],
                                        wdb.ap[:, fh * D + n * 512: fh * D + (n + 1) * 512], fh == 0, fh == 1,
                                        reads=[actT[fh].b, wdb.b], writes=[self.psb[ob[n]]])
                        gsc = gates[:, t * NE + e: t * NE + e + 1] if e < NE else 1.0
                        self.stt(xa[t].ap, self.bank(ob[0], 2), gsc, xa[t].ap, ALU.mult, ALU.add,
                                 reads=[self.psb[ob[0]], self.psb[ob[1]], xa[t].b] + ([gate_b[t]] if e < NE else []),
                                 writes=[xa[t].b])
                if pend_down is not None:
                    pend_down()
                pend_down = down
        if pend_down is not None:
            pend_down()
        for t in range(NT):
            tg = hh * NT + t
            if L == 0:
                self.dma("sp", self.XS[tg * 128:(tg + 1) * 128, :], xa[t].ap, reads=[xa[t].b], writes=[self.XS_b[tg]])
                if self.debug and self.stage == 3:
                    self.dma("sp", self.dbgX[tg * 128:(tg + 1) * 128, :], xa[t].ap, reads=[xa[t].b])
            else:
                ss, rstd = ep_ss.next(), ep_rstd.next()
                self.act(ep_junk.ap, xa[t].ap, AF.Square, reads=[xa[t].b], writes=[ss.b, ep_junk.b], accum_out=ss.ap)
                self.ts("pool", rstd.ap, ss.ap, 1.0 / D, ALU.mult, EPS, ALU.add, reads=[ss.b], writes=[rstd.b])
                self.tt("pool", rstd.ap, rstd.ap, self.neghalf.ap[:, 0:1], ALU.pow, reads=[rstd.b, self.neghalf.b], writes=[rstd.b])
                ot = ot_r.next()
                self.stt(ot.ap, xa[t].ap, rstd.ap, self.fin_bc.ap, ALU.mult, ALU.mult,
                         reads=[xa[t].b, rstd.b, self.fin_bc.b], writes=[ot.b])
                self.outs.append(self.dma("sp", self.out[tg * 128:(tg + 1) * 128, :], ot.ap, reads=[ot.b]))

    def phase_s1(self):
        I = self.I
        NF = 16
        winu = self.tile(KC * 2048, BF16, "winu")
        winv = self.tile(KC * 2048, BF16, "winv")
        winuv = winu.ap.rearrange("p (kc n) -> p kc n", kc=KC)
        winvv = winv.ap.rearrange("p (kc n) -> p kc n", kc=KC)
        src = I["b_w_in"].rearrange("(kc p) n -> p kc n", p=128)
        self.dma("q2", winuv, src[:, :, 0:2048], writes=[winu.b])
        self.dma("q2", winvv, src[:, :, 2048:4096], writes=[winv.b])
        woutb = self.tile(NF * D, BF16, "s_woutb")
        wsTb = self.tile(8 * 128, BF16, "wsTb")
        Cm = self.tile(NF * 128, F32, "Cmat")
        Gbc = self.tile(2048, BF16, "s_Gbc")
        lnG = self.lng_fm.ap[:, 0:16]
        lnB = self.lng_fm.ap[:, 16:32]
        mark = self.top
        G1 = self.tile(D, F32, "s_G1")
        self.dma("sp", G1.ap, self.GBC[2], reads=[self.GBC_b[2]], writes=[G1.b])
        wst = Ring([self.tile(D, F32, f"s_wst{i}") for i in range(2)])
        for fc in range(NF):
            st = wst.next()
            self.dma("sp", st.ap, I["b_w_out"][fc * 128:(fc + 1) * 128, :], writes=[st.b])
            self.tt("pool", woutb.ap[:, fc * D:(fc + 1) * D], st.ap, G1.ap, ALU.mult, reads=[st.b, G1.b], writes=[woutb.b])
        ws32 = self.tile(8 * 128, F32, "ws32")
        self.dma("sp", ws32.ap.rearrange("p (g q) -> p g q", g=8), I["b_w_s"].rearrange("g p q -> p g q"), writes=[ws32.b])
        wsT32 = self.tile(8 * 128, F32, "wsT32")
        rs_bc = self.tile(8 * 128, F32, "rs_bc")
        bs_bc = self.tile(8 * 128, F32, "bs_bc")
        bsrow = self.tile(D, F32, "bsrow")
        self.dma("sp", bsrow.ap[0:1, :], I["b_bs_row"], writes=[bsrow.b])
        for hb in range(2):
            for g4 in range(4):
                g = hb * 4 + g4
                self.tr(self.bank(hb)[:, g4 * 128:(g4 + 1) * 128], ws32.ap[:, g * 128:(g + 1) * 128], self.ident.ap,
                        reads=[ws32.b, self.ident.b], writes=[self.psb[hb]])
            self.copy("dve", wsT32.ap[:, hb * 512:(hb + 1) * 512], self.bank(hb), reads=[self.psb[hb]], writes=[wsT32.b])
        self.copy("pool", wsTb.ap, wsT32.ap, reads=[wsT32.b], writes=[wsTb.b])
        for hb in range(2):
            self.mm(self.bank(2 + hb), self.ones.ap, wsT32.ap[:, hb * 512:(hb + 1) * 512], True, True,
                    reads=[self.ones.b, wsT32.b], writes=[self.psb[2 + hb]])
            self.copy("dve", rs_bc.ap[:, hb * 512:(hb + 1) * 512], self.bank(2 + hb), reads=[self.psb[2 + hb]], writes=[rs_bc.b])
            self.mm(self.bank(4 + hb), self.ones.ap[0:1, :], bsrow.ap[0:1, hb * 512:(hb + 1) * 512], True, True,
                    reads=[self.ones.b, bsrow.b], writes=[self.psb[4 + hb]])
            self.copy("dve", bs_bc.ap[:, hb * 512:(hb + 1) * 512], self.bank(4 + hb), reads=[self.psb[4 + hb]], writes=[bs_bc.b])
        lnrow = self.tile(2048, F32, "lnrow")
        self.dma("sp", lnrow.ap[0:1, :], I["b_ln_row"][:, 0:2048], writes=[lnrow.b])
        for n4 in range(4):
            bk = 6 + n4 % 2
            self.mm(self.bank(bk), self.ones.ap[0:1, :], lnrow.ap[0:1, n4 * 512:(n4 + 1) * 512], True, True,
                    reads=[self.ones.b, lnrow.b], writes=[self.psb[bk]])
            self.copy("dve", Gbc.ap[:, n4 * 512:(n4 + 1) * 512], self.bank(bk), reads=[self.psb[bk]], writes=[Gbc.b])
        for fc in range(NF):
            g = fc // 2
            self.stt(Cm.ap[:, fc * 128:(fc + 1) * 128], rs_bc.ap[:, g * 128:(g + 1) * 128], lnB[:, fc:fc + 1],
                     bs_bc.ap[:, g * 128:(g + 1) * 128], ALU.mult, ALU.add,
                     reads=[rs_bc.b, bs_bc.b, self.lng_fm.b], writes=[Cm.b])
        self.P.barrier()
        self.top = mark
        SK = os.environ.get('SSKIP', '')
        if 'main' in SK:
            return
        xts = Ring([self.tile(D, F32, f"s_xt{i}") for i in range(2)])
        nb = self.norm_bufs(banks=(0,), nxs=1)
        hx_ap = [self.alloc(KC * 512, BF16, f"s_hx{i}").rearrange("p (kc n) -> p kc n", kc=KC) for i in range(2)]
        hx_b = [[[Buf(f"shx{i}_{c}_{q}") for q in range(4)] for c in range(KC)] for i in range(2)]
        uT = self.tile(NF * 512, BF16, "s_uT")
        uTv = uT.ap.rearrange("p (f n) -> p f n", f=NF)
        uT_b = [Buf(f"s_uT{f}") for f in range(NF)]
        v = self.tile(2048, F32, "s_v")
        v_b = [Buf(f"s_v{i}") for i in range(4)]
        vn_r = Ring([self.tile(2048, BF16, f"s_vn{i}") for i in range(2)])
        th_r = Ring([self.tile(1024, F32, f"s_th{i}") for i in range(1)])
        vn0 = self.tile(2048, BF16, "s_vn0")
        zT_r = Ring([self.tile(NF * 128, BF16, f"s_zT{i}") for i in range(2)])
        st_ = {n: self.tile(w, F32, "s_" + n) for n, w in [("s1", 4), ("s2", 2), ("mean", 1), ("ex2", 1), ("msq", 1),
                                                            ("rstd", 1), ("nmr", 1), ("t2", 2)]}
        uvb = Ring([2, 3])
        xres_r = Ring([self.tile(D, F32, f"s_xres{i}") for i in range(2)])

        def stage_n(blk):
            si = blk % 2
            hap = hx_ap[si]
            for q in range(4):
                tg = blk * 4 + q
                xt = xts.next()
                self.dma("sp", xt.ap, self.XS[tg * 128:(tg + 1) * 128, :], reads=[self.XS_b[tg]], writes=[xt.b])
                self.norm_tile(xt, self.A1[1], self.B1[1],
                               lambda c, q=q, hap=hap, si=si: (hap[:, c, q * 128:(q + 1) * 128], hx_b[si][c][q]), nb)

        def stage_u(blk):
            si = blk % 2
            hap = hx_ap[si]
            for fc in range(NF):
                bk = uvb.next()
                for kc in range(KC):
                    self.mm(self.bank(bk), winuv[:, kc, fc * 128:(fc + 1) * 128], hap[:, kc, :], kc == 0, kc == KC - 1,
                            reads=[winu.b] + [hx_b[si][kc][q] for q in range(4)], writes=[self.psb[bk]])
                self.act(uTv[:, fc, :], self.bank(bk), AF.Gelu_apprx_tanh, reads=[self.psb[bk]], writes=[uT_b[fc]])

        def stage_a(blk, q):
            si = blk % 2
            hap = hx_ap[si]
            s1, s2 = st_["s1"], st_["s2"]
            for cg in range(4):
                bk = uvb.next()
                for kc in range(KC):
                    self.mm(self.bank(bk), hap[:, kc, q * 128:(q + 1) * 128], winvv[:, kc, cg * 512:(cg + 1) * 512],
                            kc == 0, kc == KC - 1, reads=[winv.b, hx_b[si][kc][q]], writes=[self.psb[bk]])
                self.act(v.ap[:, cg * 512:(cg + 1) * 512], self.bank(bk), AF.Gelu_apprx_tanh,
                         reads=[self.psb[bk]], writes=[v_b[cg], s1.b], accum_out=s1.ap[:, cg:cg + 1])
            for hv in range(2):
                jk = nb["junkr"].next()
                self.act(jk.ap, v.ap[:, hv * 1024:(hv + 1) * 1024], AF.Square, reads=[v_b[2 * hv], v_b[2 * hv + 1]],
                         writes=[s2.b, jk.b], accum_out=s2.ap[:, hv:hv + 1])
            mean, ex2, msq, rstd, nmr, t2 = [st_[k] for k in ("mean", "ex2", "msq", "rstd", "nmr", "t2")]
            self.tt("pool", t2.ap, s1.ap[:, 0:2], s1.ap[:, 2:4], ALU.add, reads=[s1.b], writes=[t2.b])
            self.tt("pool", mean.ap, t2.ap[:, 0:1], t2.ap[:, 1:2], ALU.add, reads=[t2.b], writes=[mean.b])
            self.tt("pool", ex2.ap, s2.ap[:, 0:1], s2.ap[:, 1:2], ALU.add, reads=[s2.b], writes=[ex2.b])
            self.ts("pool", msq.ap, mean.ap, mean.ap, ALU.mult, 1.0 / (2048.0 * 2048.0), ALU.mult, reads=[mean.b], writes=[msq.b])
            self.ts("pool", ex2.ap, ex2.ap, 1.0 / 2048, ALU.mult, EPS, ALU.add, reads=[ex2.b], writes=[ex2.b])
            self.tt("pool", rstd.ap, ex2.ap, msq.ap, ALU.subtract, reads=[ex2.b, msq.b], writes=[rstd.b])
            self.tt("pool", rstd.ap, rstd.ap, self.neghalf.ap[:, 0:1], ALU.pow, reads=[rstd.b, self.neghalf.b], writes=[rstd.b])
            self.ts("pool", nmr.ap, mean.ap, rstd.ap, ALU.mult, -1.0 / 2048, ALU.mult, reads=[mean.b, rstd.b], writes=[nmr.b])
            vn = vn_r.next()
            self.ts("dve", vn0.ap, v.ap, rstd.ap, ALU.mult, nmr.ap, ALU.add, reads=v_b + [rstd.b, nmr.b], writes=[vn0.b])
            self.tt("dve", vn.ap, vn0.ap, Gbc.ap, ALU.mult, reads=[vn0.b, Gbc.b], writes=[vn.b])
            return vn

        def stage_s(blk, q, vn):
            for fc in range(NF):
                bk = 4 + fc // 4
                self.mm(self.bank(bk)[:, (fc % 4) * 128:(fc % 4 + 1) * 128], vn.ap[:, fc * 128:(fc + 1) * 128],
                        wsTb.ap[:, (fc // 2) * 128:(fc // 2 + 1) * 128], True, True,
                        reads=[vn.b, wsTb.b], writes=[self.psb[bk]])
            zT = zT_r.next()
            for hf in range(2):
                th = th_r.next()
                self.tt("dve", th.ap, self.bank(4 + 2 * hf, 2), Cm.ap[:, hf * 1024:(hf + 1) * 1024], ALU.add,
                        reads=[self.psb[4 + 2 * hf], self.psb[5 + 2 * hf], Cm.b], writes=[th.b])
                self.tt("dve", zT.ap[:, hf * 1024:(hf + 1) * 1024].rearrange("p (f n) -> p f n", f=8),
                        th.ap.rearrange("p (f n) -> p f n", f=8), uTv[:, hf * 8:(hf + 1) * 8, q * 128:(q + 1) * 128], ALU.mult,
                        reads=[th.b] + uT_b[hf * 8:(hf + 1) * 8], writes=[zT.b])
            return zT

        def stage_o(blk, q, zT):
            tg = blk * 4 + q
            xr = xres_r.next()
            self.dma("sp", xr.ap, self.XS[tg * 128:(tg + 1) * 128, :], reads=[self.XS_b[tg]], writes=[xr.b])
            for n in range(2):
                for fc in range(NF):
                    self.mm(self.bank(2 + n), zT.ap[:, fc * 128:(fc + 1) * 128], woutb.ap[:, fc * D + n * 512: fc * D + (n + 1) * 512],
                            fc == 0, fc == NF - 1, reads=[zT.b, woutb.b], writes=[self.psb[2 + n]])
            self.tt("dve", xr.ap, self.bank(2, 2), xr.ap, ALU.add, reads=[self.psb[2], self.psb[3], xr.b], writes=[xr.b])
            self.dma("sp", self.XS[tg * 128:(tg + 1) * 128, :], xr.ap, reads=[xr.b], writes=[self.XS_b[tg]])
            if self.debug and self.stage == 4:
                self.dma("sp", self.dbgX[tg * 128:(tg + 1) * 128, :], xr.ap, reads=[xr.b])

        pendS, pendO = [], []

        def step_s():
            if pendS:
                b_, q_, vn_ = pendS.pop(0)
                pendO.append((b_, q_, stage_s(b_, q_, vn_)))

        def step_o():
            if pendO:
                stage_o(*pendO.pop(0))

        stage_n(0)
        for blk in range(8):
            while pendS:
                step_o()
                step_s()
            stage_u(blk)
            for q in range(4):
                vn = stage_a(blk, q)
                step_o()
                step_s()
                pendS.append((blk, q, vn))
                if q == 1 and blk + 1 < 8:
                    stage_n(blk + 1)
        while pendS or pendO:
            step_o()
            step_s()


def _fm(v, nchunk):
    return np.ascontiguousarray(np.asarray(v, np.float32).reshape(nchunk, 128).T)


def _sincos_table():
    quarter = D // 4
    omega = (1.0 / (np.float32(10000.0) ** (np.arange(quarter, dtype=np.float32) / np.float32(quarter)))).astype(np.float32)

    def emb(n):
        p = np.arange(n, dtype=np.float32)[:, None] * omega[None, :]
        return np.concatenate([np.sin(p), np.cos(p)], axis=-1).astype(np.float32)
    rows = SEQ // 64
    er, ec = emb(rows), emb(64)
    pe = np.concatenate([np.broadcast_to(er[:, None, :], (rows, 64, D // 2)),
                         np.broadcast_to(ec[None, :, :], (rows, 64, D // 2))], axis=-1)
    return np.ascontiguousarray(pe.reshape(SEQ, D).astype(np.float32))


def make_in_maps(inp):
    f = lambda a: np.ascontiguousarray(np.asarray(a, np.float32))
    pe = _sincos_table()
    shared = {}
    shared["ident"] = np.eye(128, dtype=np.float32)
    shared["ada_w"] = f(inp["ada_w"])
    ab = f(inp["ada_b"])
    abf = np.stack([_fm(ab[i], 48) for i in range(2)], 0)
    shared["adab_fm"] = np.ascontiguousarray(np.repeat(abf.transpose(1, 0, 2)[:, :, :, None], 2, axis=3).reshape(128, 192))
    shared["adab_row"] = f(ab.reshape(1, -1))
    shared["ng"] = np.ascontiguousarray(np.concatenate(
        [_fm(inp["mix_norm_g"][0], 8), _fm(inp["ffn_norm_g"][0], 8), _fm(inp["mix_norm_g"][1], 8), _fm(inp["ffn_norm_g"][1], 8)], axis=1))
    shared["fin_row"] = f(inp["final_norm_g"]).reshape(1, D)
    shared["a_w_in"] = f(inp["a_w_in"][0])
    cw = f(inp["a_conv_w"][0])
    cwf = np.stack([_fm(cw[k], 10) for k in range(4)], axis=2).reshape(128, 40)
    parts = [cwf, _fm(inp["a_conv_b"][0], 10)]
    for nm in ("a_gate_r_b", "a_gate_i_b", "a_lambda"):
        v = f(inp[nm][0])
        parts.append(np.concatenate([_fm(v[0], 10), _fm(v[1], 10)], axis=1))
    shared["a_small"] = np.ascontiguousarray(np.concatenate(parts, axis=1))
    shared["a_rw"] = f(inp["a_gate_r_w"][0])
    shared["a_iw"] = f(inp["a_gate_i_w"][0])
    shared["a_w_out"] = f(inp["a_w_out"][0])
    shared["b_w_in"] = f(inp["b_w_in"][0])
    shared["b_ln_row"] = np.ascontiguousarray(np.concatenate([f(inp["b_ln_g"][0]), f(inp["b_ln_b"][0])]).reshape(1, 4096))
    shared["b_lng_fm"] = np.ascontiguousarray(np.concatenate([_fm(inp["b_ln_g"][0], 16), _fm(inp["b_ln_b"][0], 16)], axis=1))
    shared["b_w_s"] = f(inp["b_w_s"][0])
    shared["b_bs_row"] = f(inp["b_b_s"][0]).reshape(1, 1024)
    shared["b_w_out"] = f(inp["b_w_out"][0])
    shared["router_w"] = f(inp["router_w"])
    shared["rb_row"] = f(inp["router_b"]).reshape(1, 2 * NE)
    shared["moe_w_gu"] = f(inp["moe_w_gu"])
    shared["moe_w_down"] = f(inp["moe_w_down"])
    shared["sh_w_gu"] = f(inp["shared_w_gu"])
    shared["sh_w_down"] = f(inp["shared_w_down"])
    x = f(inp["x"])
    ctx = f(inp["ctx"])
    c = f(inp["c"])
    cc = f(inp["c_ctx"])
    maps = []
    for k in range(8):
        b, h = k // 2, k % 2
        m = dict(shared)
        m["x_own"] = np.ascontiguousarray(x[b, h * HALF:(h + 1) * HALF])
        m["x_oth"] = np.ascontiguousarray(x[b, (1 - h) * HALF:(2 - h) * HALF])
        m["pe_own"] = np.ascontiguousarray(pe[h * HALF:(h + 1) * HALF])
        m["pe_oth"] = np.ascontiguousarray(pe[(1 - h) * HALF:(2 - h) * HALF])
        m["ctxb"] = np.ascontiguousarray(ctx[b])
        cv = np.stack([_fm(c[b], 8), _fm(cc, 8)], axis=2).reshape(128, 16)
        m["cvec"] = np.ascontiguousarray(cv)
        fl = np.zeros((128, 2), np.float32)
        fl[:, h] = 1.0
        m["flags"] = fl
        maps.append(m)
    return maps


_NC_CACHE = {}


def kernel(**inputs):
    if "nc" not in _NC_CACHE:
        _NC_CACHE["nc"] = Builder(stage=99).build()
    nc = _NC_CACHE["nc"]
    maps = make_in_maps(inputs)
    res = run_bass_kernel_spmd(nc, maps, core_ids=list(range(8)))
    out = np.empty((4, SEQ, D), np.float32)
    for k in range(8):
        b, h = k // 2, k % 2
        out[b, h * HALF:(h + 1) * HALF] = res.results[k]["out"]
    return out
```

```python
import os
import numpy as np
import concourse.bass as bass
import concourse.mybir as mybir
from concourse.bass_utils import run_bass_kernel_spmd
from contextlib import ExitStack

F32 = mybir.dt.float32
BF16 = mybir.dt.bfloat16
AF = mybir.ActivationFunctionType
ALU = mybir.AluOpType
AX = mybir.AxisListType

D = 1024
KC = 8
SEQ = 8192
HALF = 4096
CTX = 256
RW = 1280
NJ = 10
NH = 5
NE = 64
EPS = 1e-6
NTOT = CTX + 2 * HALF

COMPUTE = ("pe", "act", "dve", "pool")
QUEUES = ("sp", "q2")
NSEM = 8
SAME_ENGINE_SYNC = True


class Buf:
    __slots__ = ("name", "w", "r")

    def __init__(self, name=""):
        self.name = name
        self.w = None
        self.r = []


class Op:
    __slots__ = ("eng", "fn", "deps", "signal", "val", "idx", "is_dma", "qn")

    def __init__(self, eng, fn, is_dma=False):
        self.eng = eng
        self.fn = fn
        self.deps = []
        self.signal = False
        self.val = None
        self.is_dma = is_dma
        self.qn = 0


class Prog:
    def __init__(self):
        self.streams = {"pe": [], "act": [], "dve": [], "pool": [], "sp": []}
        self.ndma = {"sp": 0, "q2": 0}
        self.ring = {"sp": [None] * NSEM, "q2": [None] * NSEM}

    @staticmethod
    def _stream_of(eng):
        return "pool" if eng == "q2" else eng

    def op(self, eng, fn, reads=(), writes=(), extra=()):
        is_dma = eng in QUEUES
        o = Op(eng, fn, is_dma)
        if is_dma:
            o.qn = self.ndma[eng]
            self.ndma[eng] += 1
            self.ring[eng][o.qn % NSEM] = o
        st = self._stream_of(eng)
        cand = []
        for b in reads:
            if b.w is not None:
                cand.append((b.w, True))
        for b in writes:
            if b.w is not None:
                cand.append((b.w, True))
            for r in b.r:
                cand.append((r, False))
        for d in extra:
            cand.append((d, True))
        seen = set()
        for d, strong in cand:
            if d is o or id(d) in seen:
                continue
            same = (not d.is_dma) and (not is_dma) and self._stream_of(d.eng) == st
            if same:
                if d.eng == "pe" or not SAME_ENGINE_SYNC:
                    continue
            seen.add(id(d))
            o.deps.append(d)
            d.signal = True
        for b in reads:
            b.r.append(o)
        for b in writes:
            b.w = o
            b.r = []
        o.idx = len(self.streams[st])
        self.streams[st].append(o)
        return o

    def barrier(self):
        last = []
        for st, ops in self.streams.items():
            for o in reversed(ops):
                if not o.is_dma:
                    last.append(o)
                    break
        for q in QUEUES:
            for o in self.ring[q]:
                if o is not None:
                    last.append(o)
        for st in ("pe", "act", "dve", "pool", "sp"):
            self.op(st, (lambda e: e.nop()), extra=[d for d in last])

    def assign(self):
        for st, ops in self.streams.items():
            cnt = 0
            for o in ops:
                if o.is_dma:
                    o.val = 16 * (o.qn // NSEM + 1)
                elif o.signal:
                    cnt += 1
                    o.val = cnt

    def emit_stream(self, st, engobj, sem_eng, sem_dma):
        waited = {}

        def wait(key, sem, val):
            if waited.get(key, 0) >= val:
                return
            engobj.wait_ge(sem, val)
            waited[key] = val

        for o in self.streams[st]:
            for d in o.deps:
                if d.is_dma:
                    wait((d.eng, d.qn % NSEM), sem_dma[d.eng][d.qn % NSEM], d.val)
                else:
                    dst = self._stream_of(d.eng)
                    wait(dst, sem_eng[dst], d.val)
            if o.is_dma:
                if o.qn >= NSEM:
                    wait((o.eng, o.qn % NSEM), sem_dma[o.eng][o.qn % NSEM], o.val - 16)
                ins = o.fn(engobj)
                ins.then_inc(sem_dma[o.eng][o.qn % NSEM], 16)
            else:
                ins = o.fn(engobj)
                if o.signal:
                    ins.then_inc(sem_eng[st], 1)

    def final_waits(self, engobj, sem_eng, sem_dma, ops):
        for d in ops:
            if d.is_dma:
                engobj.wait_ge(sem_dma[d.eng][d.qn % NSEM], d.val)
            else:
                engobj.wait_ge(sem_eng[self._stream_of(d.eng)], d.val)


def run_prog(nc, P, final_ops):
    P.assign()
    with ExitStack() as es:
        sem_eng = {s: es.enter_context(nc.semaphore("sem_" + s)) for s in COMPUTE}
        sem_dma = {q: [es.enter_context(nc.semaphore(f"semd_{q}_{i}")) for i in range(NSEM)]
                   for q in QUEUES}
        block = es.enter_context(nc.Block())

        @block.tensor
        def _(e):
            P.emit_stream("pe", e, sem_eng, sem_dma)

        @block.scalar
        def _(e):
            P.emit_stream("act", e, sem_eng, sem_dma)

        @block.vector
        def _(e):
            P.emit_stream("dve", e, sem_eng, sem_dma)

        @block.gpsimd
        def _(e):
            P.emit_stream("pool", e, sem_eng, sem_dma)

        @block.sync
        def _(e):
            P.emit_stream("sp", e, sem_eng, sem_dma)
            P.final_waits(e, sem_eng, sem_dma, final_ops)


class T:
    __slots__ = ("ap", "b")

    def __init__(self, ap, name=""):
        self.ap = ap
        self.b = Buf(name)


class Ring:
    def __init__(self, items):
        self.items = items
        self.i = 0

    def next(self):
        t = self.items[self.i % len(self.items)]
        self.i += 1
        return t


ARENA_COLS = 52992


class Builder:
    def __init__(self, stage=99, debug=False):
        self.stage = stage
        self.debug = debug
        self.nc = bass.Bass("TRN2", target_bir_lowering=False)
        self.P = Prog()
        self.outs = []
        self.es = ExitStack()

    def din(self, name, shape, dt=F32):
        return self.nc.dram_tensor(name, list(shape), dt, kind="ExternalInput").ap()

    def dscr(self, name, shape, dt=F32, out=False):
        kind = "ExternalOutput" if out else "Internal"
        return self.nc.dram_tensor(name, list(shape), dt, kind=kind).ap()

    def alloc(self, ncols, dt=F32, name=""):
        n32 = ncols if dt == F32 else (ncols + 1) // 2
        assert self.top + n32 <= ARENA_COLS, f"arena overflow {self.top}+{n32} ({name})"
        ap = self.arena[:, self.top:self.top + n32]
        self.top += n32
        if dt != F32:
            ap = ap.bitcast(dt)
        return ap

    def tile(self, ncols, dt=F32, name=""):
        return T(self.alloc(ncols, dt, name), name)

    def dma(self, q, out, in_, reads=(), writes=(), slow=False):
        if slow:
            return self.P.op(q, lambda e: e.dma_start(out=out, in_=in_, allow_slow_non_contiguous=True), reads, writes)
        return self.P.op(q, lambda e: e.dma_start(out=out, in_=in_), reads, writes)

    def act(self, out, in_, func, reads=(), writes=(), **kw):
        return self.P.op("act", lambda e: e.activation(out=out, in_=in_, func=func, **kw), reads, writes)

    def ts(self, eng, out, in0, s1, op0, s2=None, op1=None, reads=(), writes=()):
        if op1 is None:
            return self.P.op(eng, lambda e: e.tensor_scalar(out=out, in0=in0, scalar1=s1, scalar2=None, op0=op0),
                             reads, writes)
        return self.P.op(eng, lambda e: e.tensor_scalar(out=out, in0=in0, scalar1=s1, scalar2=s2, op0=op0, op1=op1),
                         reads, writes)

    def tt(self, eng, out, in0, in1, op, reads=(), writes=()):
        return self.P.op(eng, lambda e: e.tensor_tensor(out=out, in0=in0, in1=in1, op=op), reads, writes)

    def stt(self, out, in0, scalar, in1, op0, op1, reads=(), writes=()):
        return self.P.op("dve", lambda e: e.scalar_tensor_tensor(out=out, in0=in0, scalar=scalar, in1=in1,
                                                                 op0=op0, op1=op1), reads, writes)

    def mm(self, out, lhsT, rhs, start, stop, reads=(), writes=()):
        return self.P.op("pe", lambda e: e.matmul(out, lhsT, rhs, start=start, stop=stop), reads, writes)

    def tr(self, out, in_, ident, reads=(), writes=()):
        return self.P.op("pe", lambda e: e.transpose(out=out, in_=in_, identity=ident), reads, writes)

    def copy(self, eng, out, in_, reads=(), writes=()):
        if eng == "act":
            return self.P.op("act", lambda e: e.copy(out=out, in_=in_), reads, writes)
        return self.P.op(eng, lambda e: e.tensor_copy(out=out, in_=in_), reads, writes)

    def memset(self, eng, ap, val, writes=()):
        return self.P.op(eng, lambda e: e.memset(ap, val), (), writes)

    def declare(self):
        dbg = self.debug
        I = {}
        I["x_own"] = self.din("x_own", [HALF, D])
        I["x_oth"] = self.din("x_oth", [HALF, D])
        I["ctxb"] = self.din("ctxb", [CTX, D])
        I["pe_own"] = self.din("pe_own", [HALF, D])
        I["pe_oth"] = self.din("pe_oth", [HALF, D])
        I["cvec"] = self.din("cvec", [128, 16])
        I["flags"] = self.din("flags", [128, 2])
        I["ident"] = self.din("ident", [128, 128])
        I["ada_w"] = self.din("ada_w", [2, D, 6 * D])
        I["adab_fm"] = self.din("adab_fm", [128, 2 * 96])
        I["adab_row"] = self.din("adab_row", [1, 2 * 6 * D])
        I["ng"] = self.din("ng", [128, 4 * KC])
        I["fin_row"] = self.din("fin_row", [1, D])
        I["a_w_in"] = self.din("a_w_in", [D, 2 * RW])
        I["a_small"] = self.din("a_small", [128, 110])
        I["a_rw"] = self.din("a_rw", [2, NH, 256, 256])
        I["a_iw"] = self.din("a_iw", [2, NH, 256, 256])
        I["a_w_out"] = self.din("a_w_out", [RW, D])
        I["b_w_in"] = self.din("b_w_in", [D, 4096])
        I["b_ln_row"] = self.din("b_ln_row", [1, 4096])
        I["b_lng_fm"] = self.din("b_lng_fm", [128, 32])
        I["b_w_s"] = self.din("b_w_s", [8, 128, 128])
        I["b_bs_row"] = self.din("b_bs_row", [1, 1024])
        I["b_w_out"] = self.din("b_w_out", [2048, D])
        I["router_w"] = self.din("router_w", [2, D, NE])
        I["rb_row"] = self.din("rb_row", [1, 2 * NE])
        if self.stage >= 3:
            I["moe_w_gu"] = self.din("moe_w_gu", [2, NE, D, 512])
            I["moe_w_down"] = self.din("moe_w_down", [2, NE, 256, D])
        I["sh_w_gu"] = self.din("sh_w_gu", [2, D, 512])
        I["sh_w_down"] = self.din("sh_w_down", [2, 256, D])
        self.I = I
        st = self.stage
        self.out = self.nc.dram_tensor("out", [HALF, D], F32, kind="ExternalOutput").ap()
        self.uT = self.dscr("uT", [NJ, 128, NTOT], F32, out=(dbg and st == 1))
        self.gg = self.dscr("gg", [NJ, 128, HALF], F32, out=(dbg and st == 1))
        self.ygT = self.dscr("ygT", [NJ, 128, HALF], BF16, out=(dbg and st == 2))
        self.XS = self.dscr("XS", [HALF, D], F32)
        self.dbgX = self.dscr("dbgX", [HALF, D], F32, out=True) if dbg else None
        self.GBC = self.dscr("GBC", [4, 128, D], F32, out=(dbg and st == 1))
        self.modd = self.dscr("modd", [128, 192], F32, out=(dbg and st == 1))
        self.uT_b = {}
        self.gg_b = {}
        self.yg_b = {}
        self.XS_b = [Buf(f"XS{t}") for t in range(32)]
        self.GBC_b = [Buf(f"GBC{i}") for i in range(4)]

    def build(self):
        nc = self.nc
        self.declare()
        es = self.es
        self.arena = es.enter_context(nc.sbuf_tensor("arena", [128, ARENA_COLS], F32))
        self.ps = es.enter_context(nc.psum_tensor("ps", [128, 4096], F32))
        self.psb = [Buf(f"bank{i}") for i in range(8)]
        self.top = 0
        self.phase_pro()
        self.const_top = self.top
        if self.stage >= 1:
            self.phase_a0()
        dbgskip = os.environ.get('SKIPSTAGES', '')
        if self.stage >= 2 and 'b0' not in dbgskip:
            self.P.barrier()
            self.top = self.const_top
            self.phase_b0()
        if self.stage >= 3 and 'm0' not in dbgskip:
            for hh in range(2):
                self.P.barrier()
                self.top = self.const_top
                self.phase_m(0, hh)
        if self.stage >= 4:
            self.P.barrier()
            self.top = self.const_top
            self.phase_s1()
        if self.stage >= 5:
            for hh in range(2):
                self.P.barrier()
                self.top = self.const_top
                self.phase_m(1, hh)
        if self.stage < 5:
            self.P.barrier()
            self.top = self.const_top
            z = self.tile(D, F32, "zero")
            self.memset("dve", z.ap, 0.0, writes=[z.b])
            for t in range(32):
                self.outs.append(self.dma("sp", self.out[t * 128:(t + 1) * 128, :], z.ap, reads=[z.b]))
        self.P.barrier()
        run_prog(nc, self.P, self.outs)
        es.close()
        return nc

    def bank(self, b, n=1):
        return self.ps[:, b * 512:(b + n) * 512]

    def phase_pro(self):
        I = self.I
        c = {}
        for name, n in [("ident", 128), ("cvec", 16), ("flags", 2), ("adab_fm", 192), ("ng", 32),
                        ("a_small", 110), ("b_lng_fm", 32)]:
            c[name] = self.tile(n, F32, name)
            self.dma("sp", c[name].ap, I[name], writes=[c[name].b])
        ones = self.tile(128, F32, "ones")
        self.memset("dve", ones.ap, 1.0, writes=[ones.b])
        self.ident, self.ones, self.flags = c["ident"], ones, c["flags"]
        silu = self.tile(16, F32, "silu_c")
        self.act(silu.ap, c["cvec"].ap, AF.Silu, reads=[c["cvec"].b], writes=[silu.b])
        mod = self.tile(192, F32, "mod")
        self.mod = mod
        fin_bc = self.tile(D, F32, "fin_bc")
        self.fin_bc = fin_bc
        rb_bc = self.tile(2 * NE, F32, "rb_bc")
        self.rb_bc = rb_bc
        self.A1 = [self.tile(KC, F32, f"A1_{i}") for i in range(2)]
        self.B1 = [self.tile(KC, F32, f"B1_{i}") for i in range(2)]
        self.A2 = [self.tile(KC, F32, f"A2_{i}") for i in range(2)]
        self.B2 = [self.tile(KC, F32, f"B2_{i}") for i in range(2)]
        self.A1c = self.tile(KC, F32, "A1c")
        self.B1c = self.tile(KC, F32, "B1c")
        self.asm = c["a_small"]
        self.nrb = self.tile(20, F32, "nrb")
        self.nib = self.tile(20, F32, "nib")
        self.c8 = self.tile(20, F32, "c8")
        self.c16 = self.tile(20, F32, "c16")
        self.neghalf = self.tile(8, F32, "neghalf")
        self.memset("dve", self.neghalf.ap, -0.5, writes=[self.neghalf.b])
        self.lng_fm = c["b_lng_fm"]
        mark = self.top
        srep = self.tile(KC * 128, F32, "silu_rep")
        for kc in range(KC):
            self.ts("dve", srep.ap[:, kc * 128:(kc + 1) * 128], ones.ap, silu.ap[:, 2 * kc:2 * kc + 1], ALU.mult,
                    reads=[ones.b, silu.b], writes=[srep.b])
        rows = self.tile(2 * 6 * D, F32, "adab_row")
        self.dma("sp", rows.ap[0:1, :], I["adab_row"], writes=[rows.b])
        frow = self.tile(D + 2 * NE, F32, "fin_row")
        self.dma("sp", frow.ap[0:1, 0:D], I["fin_row"], writes=[frow.b])
        self.dma("sp", frow.ap[0:1, D:D + 2 * NE], I["rb_row"], writes=[frow.b])
        adat = Ring([self.tile(KC * 512, F32, f"adat{i}") for i in range(2)])
        gst = Ring([self.tile(512, F32, f"gst{i}") for i in range(2)])
        for n in range(2):
            self.mm(self.bank(3)[:, 0:512], ones.ap[0:1, :], frow.ap[0:1, n * 512:(n + 1) * 512], True, True,
                    reads=[ones.b, frow.b], writes=[self.psb[3]])
            self.copy("dve", fin_bc.ap[:, n * 512:(n + 1) * 512], self.bank(3), reads=[self.psb[3]], writes=[fin_bc.b])
        self.mm(self.bank(3)[:, 0:2 * NE], ones.ap[0:1, :], frow.ap[0:1, D:D + 2 * NE], True, True,
                reads=[ones.b, frow.b], writes=[self.psb[3]])
        self.copy("dve", rb_bc.ap, self.bank(3)[:, 0:2 * NE], reads=[self.psb[3]], writes=[rb_bc.b])
        for i in range(2):
            psA = self.bank(0)[:, 0:96]
            for g in range(12):
                at = adat.next()
                src = I["ada_w"][i][:, g * 512:(g + 1) * 512].rearrange("(kc p) n -> p kc n", p=128)
                self.dma("sp" if g % 2 == 0 else "q2", at.ap.rearrange("p (kc n) -> p kc n", kc=KC), src, writes=[at.b])
                for fc in range(4):
                    col = (g * 4 + fc) * 2
                    for kc in range(KC):
                        self.mm(psA[:, col:col + 2], at.ap[:, kc * 512 + fc * 128: kc * 512 + (fc + 1) * 128],
                                silu.ap[:, 2 * kc:2 * kc + 2], kc == 0, kc == KC - 1,
                                reads=[at.b, silu.b], writes=[self.psb[0]])
                if g in (4, 5, 10, 11):
                    bk = 1 + (g % 2)
                    for kc in range(KC):
                        self.mm(self.bank(bk), srep.ap[:, kc * 128:(kc + 1) * 128], at.ap[:, kc * 512:(kc + 1) * 512],
                                kc == 0, False, reads=[srep.b, at.b], writes=[self.psb[bk]])
                    self.mm(self.bank(bk), ones.ap[0:1, :], rows.ap[0:1, i * 6 * D + g * 512: i * 6 * D + (g + 1) * 512],
                            False, True, reads=[ones.b, rows.b], writes=[self.psb[bk]])
                    s = gst.next()
                    self.copy("dve", s.ap, self.bank(bk), reads=[self.psb[bk]], writes=[s.b])
                    idx = i * 2 + (0 if g < 6 else 1)
                    self.dma("sp", self.GBC[idx][:, (g % 2) * 512:(g % 2 + 1) * 512], s.ap, reads=[s.b],
                             writes=[self.GBC_b[idx]])
            self.tt("dve", mod.ap[:, i * 96:(i + 1) * 96], psA, c["adab_fm"].ap[:, i * 96:(i + 1) * 96], ALU.add,
                    reads=[self.psb[0], c["adab_fm"].b], writes=[mod.b])
        if self.debug and self.stage == 1:
            self.dma("sp", self.modd, mod.ap, reads=[mod.b])

        def mcol(i, chunk0, which):
            base = i * 96 + chunk0 * 2 + which
            return mod.ap[:, base: base + 2 * KC: 2]
        ng = c["ng"].ap
        tmp = self.tile(KC, F32, "tmpk")
        for i in range(2):
            for (A, Bv, gi, sc_chunk, sh_chunk, which) in [
                    (self.A1[i], self.B1[i], 2 * i, 8, 0, 0), (self.A2[i], self.B2[i], 2 * i + 1, 32, 24, 0)]:
                self.ts("dve", tmp.ap, mcol(i, sc_chunk, which), 1.0, ALU.add, reads=[mod.b], writes=[tmp.b])
                self.tt("dve", A.ap, tmp.ap, ng[:, gi * KC:(gi + 1) * KC], ALU.mult, reads=[tmp.b, c["ng"].b], writes=[A.b])
                self.copy("dve", Bv.ap, mcol(i, sh_chunk, which), reads=[mod.b], writes=[Bv.b])
        self.ts("dve", tmp.ap, mcol(0, 8, 1), 1.0, ALU.add, reads=[mod.b], writes=[tmp.b])
        self.tt("dve", self.A1c.ap, tmp.ap, ng[:, 0:KC], ALU.mult, reads=[tmp.b, c["ng"].b], writes=[self.A1c.b])
        self.copy("dve", self.B1c.ap, mcol(0, 0, 1), reads=[mod.b], writes=[self.B1c.b])
        asm = self.asm
        self.ts("dve", self.nrb.ap, asm.ap[:, 50:70], -1.0, ALU.mult, reads=[asm.b], writes=[self.nrb.b])
        self.ts("dve", self.nib.ap, asm.ap[:, 70:90], -1.0, ALU.mult, reads=[asm.b], writes=[self.nib.b])
        t20 = self.tile(20, F32, "t20")
        self.act(t20.ap, asm.ap[:, 90:110], AF.Exp, reads=[asm.b], writes=[t20.b], scale=-1.0)
        self.act(t20.ap, t20.ap, AF.Ln, reads=[t20.b], writes=[t20.b], bias=1.0)
        self.ts("dve", self.c8.ap, t20.ap, -8.0, ALU.mult, reads=[t20.b], writes=[self.c8.b])
        self.ts("dve", self.c16.ap, t20.ap, -16.0, ALU.mult, reads=[t20.b], writes=[self.c16.b])
        self.P.barrier()
        self.top = mark

    def norm_tile(self, xt, A, Bv, dst, nb, dst32=None):
        st = self.norm_a(xt, nb)
        self.norm_b(st, A, Bv, dst, nb, dst32)

    def norm_a(self, xt, nb):
        junk, ss, rstd, xs = nb["junkr"].next(), nb["ss"].next(), nb["rstd"].next(), nb["xs"].next()
        self.act(junk.ap, xt.ap, AF.Square, reads=[xt.b], writes=[ss.b, junk.b], accum_out=ss.ap)
        self.ts("pool", rstd.ap, ss.ap, 1.0 / D, ALU.mult, EPS, ALU.add, reads=[ss.b], writes=[rstd.b])
        self.tt("pool", rstd.ap, rstd.ap, self.neghalf.ap[:, 0:1], ALU.pow, reads=[rstd.b, self.neghalf.b],
                writes=[rstd.b])
        self.ts("dve", xs.ap, xt.ap, rstd.ap, ALU.mult, reads=[xt.b, rstd.b], writes=[xs.b])
        return xs

    def norm_b(self, xs, A, Bv, dst, nb, dst32=None):
        bk = nb["bk"].next()
        for c in range(KC):
            b = bk + c // 4
            self.tr(self.bank(b)[:, (c % 4) * 128:(c % 4 + 1) * 128], xs.ap[:, c * 128:(c + 1) * 128],
                    self.ident.ap, reads=[xs.b, self.ident.b], writes=[self.psb[b]])
        for c in range(KC):
            b = bk + c // 4
            src = self.bank(b)[:, (c % 4) * 128:(c % 4 + 1) * 128]
            if dst32 is not None:
                ap32, b32 = dst32(c)
                self.ts("dve", ap32, src, A.ap[:, c:c + 1], ALU.mult, Bv.ap[:, c:c + 1], ALU.add,
                        reads=[self.psb[b], A.b, Bv.b], writes=[b32])
                continue
            ap, bb = dst(c)
            self.act(ap, src, AF.Identity, reads=[self.psb[b], A.b, Bv.b], writes=[bb],
                     scale=A.ap[:, c:c + 1], bias=Bv.ap[:, c:c + 1])

    def norm_bufs(self, banks=((0, 2)), nxs=2):
        nb = {}
        nb["junkr"] = Ring([self.tile(D, BF16, f"junk{i}") for i in range(2)])
        nb["junk"] = nb["junkr"].items[0]
        nb["ss"] = Ring([self.tile(1, F32, f"ss{i}") for i in range(8)])
        nb["rstd"] = Ring([self.tile(1, F32, f"rstd{i}") for i in range(8)])
        nb["xs"] = Ring([self.tile(D, F32, f"xs{i}") for i in range(nxs)])
        nb["bk"] = Ring(list(banks))
        return nb

    def phase_a0(self):
        I = self.I
        winb = self.tile(KC * 2 * RW, BF16, "a_w_in_b")
        self.dma("q2", winb.ap.rearrange("p (kc n) -> p kc n", kc=KC),
                 I["a_w_in"].rearrange("(kc p) n -> p kc n", p=128), writes=[winb.b])
        SKEW = 3
        xts = Ring([self.tile(D, F32, f"xt{i}") for i in range(SKEW + 1)])
        pets = Ring([self.tile(D, F32, f"pet{i}") for i in range(SKEW + 1)])
        nb = self.norm_bufs(banks=(0, 2), nxs=SKEW + 1)
        pend = []
        hx = [[[T(None, f"hx{s}_{c}_{t}") for t in range(4)] for c in range(KC)] for s in range(2)]
        hx_ap = [self.alloc(KC * 512, BF16, f"hxblk{s}") for s in range(2)]
        ust = Ring([self.tile(NJ * 512, F32, f"ust{i}") for i in range(2)])
        gst = Ring([self.tile(NJ * 512, F32, f"gst{i}") for i in range(2)])
        pbank = Ring([4, 5, 6, 7])
        segs = [("ctx", I["ctxb"], None, 0, CTX, self.A1c, self.B1c),
                ("oth", I["x_oth"], I["pe_oth"], CTX, HALF, self.A1[0], self.B1[0]),
                ("own", I["x_own"], I["pe_own"], CTX + HALF, HALF, self.A1[0], self.B1[0])]
        blk_i = 0
        for (sname, xsrc, pesrc, col0, ntok, A, Bv) in segs:
            nblk = (ntok + 511) // 512
            for blk in range(nblk):
                ncol = min(512, ntok - blk * 512)
                s = blk_i % 2
                blk_i += 1
                hap = hx_ap[s].rearrange("p (kc n) -> p kc n", kc=KC)
                for tt_ in range(ncol // 128):
                    t = blk * 4 + tt_
                    xt = xts.next()
                    self.dma("sp", xt.ap, xsrc[t * 128:(t + 1) * 128, :], writes=[xt.b])
                    if pesrc is not None:
                        pet = pets.next()
                        self.dma("sp", pet.ap, pesrc[t * 128:(t + 1) * 128, :], writes=[pet.b])
                        self.tt("pool", xt.ap, xt.ap, pet.ap, ALU.add, reads=[xt.b, pet.b], writes=[xt.b])
                    xs_ = self.norm_a(xt, nb)
                    pend.append(("norm", xs_, A, Bv,
                                 (lambda c, tt_=tt_, hap=hap, s=s: (hap[:, c, tt_ * 128:(tt_ + 1) * 128], hx[s][c][tt_].b)),
                                 (xt, t) if sname == "own" else None))
                    if tt_ == ncol // 128 - 1:
                        pend.append(("proj", sname, blk, ncol, s, hap, col0))
                    while len([p for p in pend if p[0] == "norm"]) > SKEW:
                        self._a0_drain(pend, nb, winb, hx, ust, gst, pbank)
        while pend:
            self._a0_drain(pend, nb, winb, hx, ust, gst, pbank)

    def _a0_drain(self, pend, nb, winb, hx, ust, gst, pbank):
        it = pend.pop(0)
        if it[0] == "norm":
            _, xs_, A, Bv, dst, st = it
            if st is not None:
                xt, t = st
                self.dma("sp", self.XS[t * 128:(t + 1) * 128, :], xt.ap, reads=[xt.b], writes=[self.XS_b[t]])
                if self.debug and self.stage == 1:
                    self.dma("sp", self.dbgX[t * 128:(t + 1) * 128, :], xt.ap, reads=[xt.b])
            self.norm_b(xs_, A, Bv, dst, nb)
            return
        _, sname, blk, ncol, s, hap, col0 = it
        if True:
            if True:
                nt = ncol // 128
                us = ust.next()
                usv = us.ap.rearrange("p (j n) -> p j n", j=NJ)
                for j in range(NJ):
                    bk = pbank.next()
                    for kc in range(KC):
                        self.mm(self.bank(bk)[:, 0:ncol], winb.ap[:, kc * 2 * RW + RW + j * 128: kc * 2 * RW + RW + (j + 1) * 128],
                                hap[:, kc, 0:ncol], kc == 0, kc == KC - 1,
                                reads=[winb.b] + [hx[s][kc][q].b for q in range(nt)], writes=[self.psb[bk]])
                    self.copy("dve", usv[:, j, 0:ncol], self.bank(bk)[:, 0:ncol], reads=[self.psb[bk]], writes=[us.b])
                key = (sname, blk)
                self.uT_b[key] = Buf(f"uT{key}")
                self.dma("sp", self.uT.rearrange("j p n -> p j n")[:, :, col0 + blk * 512: col0 + blk * 512 + ncol],
                         usv[:, :, 0:ncol], reads=[us.b], writes=[self.uT_b[key]])
                if sname == "own":
                    gs = gst.next()
                    gsv = gs.ap.rearrange("p (j n) -> p j n", j=NJ)
                    for j in range(NJ):
                        bk = pbank.next()
                        for kc in range(KC):
                            self.mm(self.bank(bk), winb.ap[:, kc * 2 * RW + j * 128: kc * 2 * RW + (j + 1) * 128],
                                    hap[:, kc, :], kc == 0, kc == KC - 1,
                                    reads=[winb.b] + [hx[s][kc][q].b for q in range(4)], writes=[self.psb[bk]])
                        self.act(gsv[:, j, :], self.bank(bk), AF.Gelu_apprx_tanh, reads=[self.psb[bk]], writes=[gs.b])
                    self.gg_b[blk] = Buf(f"gg{blk}")
                    self.dma("sp", self.gg.rearrange("j p n -> p j n")[:, :, blk * 512:(blk + 1) * 512], gsv,
                             reads=[gs.b], writes=[self.gg_b[blk]])

    def phase_b0(self):
        I = self.I
        asm = self.asm
        rwb = self.tile(2 * NH * 2 * 256, BF16, "rwb")
        iwb = self.tile(2 * NH * 2 * 256, BF16, "iwb")
        for (wt, src) in ((rwb, I["a_rw"]), (iwb, I["a_iw"])):
            for d in range(2):
                self.dma("q2", wt.ap[:, d * NH * 512:(d + 1) * NH * 512].rearrange("p (h kc n) -> p h kc n", h=NH, kc=2),
                         src[d].rearrange("h (kc p) n -> p h kc n", p=128), writes=[wt.b])

        def gw(wt, d, h, kc, co):
            base = ((d * NH + h) * 2 + kc) * 256 + co * 128
            return wt.ap[:, base:base + 128]
        Ust = [self.tile(HALF + 3, F32, "Ust0")]
        UCs = [self.tile(2 * HALF, F32, f"UC{i}") for i in range(2)]
        UCbs = [self.tile(2 * HALF, BF16, f"UCb{i}") for i in range(2)]
        UC_b = [[[Buf(f"UC{p}_{c}_{b}") for b in range(8)] for c in range(2)] for p in range(2)]
        UCb_b = [[[Buf(f"UCb{p}_{c}_{b}") for b in range(8)] for c in range(2)] for p in range(2)]
        cbuf = []
        for co_ in range(2):
            cbuf.append({
                "er": Ring([self.tile(512, F32, f"er{co_}{i}") for i in range(1)]),
                "ei": Ring([self.tile(512, F32, f"ei{co_}{i}") for i in range(1)]),
                "a": Ring([self.tile(512, F32, f"a{co_}{i}") for i in range(1)]),
                "m": Ring([self.tile(512, F32, f"m{co_}{i}") for i in range(1)]),
                "yd": Ring([self.tile(512, F32, f"yd{co_}{i}") for i in range(2)]),
                "stmp": self.tile(1, F32, f"stmp{co_}"),
                "stmp2": self.tile(1, F32, f"stmp2{co_}"),
                "pairs": Ring([(2 * co_, 2 * co_ + 1)]),
            })
        Yf = [self.alloc(HALF, F32, f"Yf{c}") for c in range(2)]
        Yf_b = [[Buf(f"Yf{c}_{i}") for i in range(8)] for c in range(2)]
        ggt_r = Ring([self.tile(512, F32, f"ggt{i}") for i in range(2)])
        ygb_r = Ring([self.tile(512, BF16, f"ygb{i}") for i in range(2)])
        s_ctx = [[self.tile(1, F32, f"sctx{c}{d}") for d in range(2)] for c in range(2)]
        s_oth = [[self.tile(1, F32, f"soth{c}{d}") for d in range(2)] for c in range(2)]
        s_ini = [[self.tile(1, F32, f"sini{c}{d}") for d in range(2)] for c in range(2)]
        s_tmp = self.tile(1, F32, "stmp")
        pairs = Ring([(0, 1), (2, 3), (4, 5), (6, 7)])
        fl = self.flags
        own0 = CTX + HALF
        oth0 = CTX
        segs = [("ctx", 0, CTX), ("oth", CTX, HALF), ("own", CTX + HALF, HALF)]
        items = [(h, sg) for h in range(NH) for sg in segs]

        dg = self.tile(8 * 128, F32, "convdiag")
        cvb = Ring([4, 5, 6, 7])

        def conv_gen(idx):
            h, (sname, col0, n) = items[idx]
            par = idx % 2
            nblk = (n + 511) // 512
            UCv = UCs[par].ap.rearrange("p (c n) -> p c n", c=2)
            UCbv = UCbs[par].ap.rearrange("p (c n) -> p c n", c=2)
            if sname == "ctx":
                for cc in range(2):
                    for k in range(4):
                        jj = 2 * h + cc
                        self.ts("dve", dg.ap[:, (cc * 4 + k) * 128:(cc * 4 + k + 1) * 128], self.ident.ap,
                                asm.ap[:, jj * 4 + k: jj * 4 + k + 1], ALU.mult, reads=[self.ident.b, asm.b], writes=[dg.b])
            pending = None

            def evac(p):
                bk, ucv, ub, ucbv, ucbb, jj, ncol = p
                self.ts("dve", ucv, self.bank(bk)[:, 0:ncol], asm.ap[:, 40 + jj:41 + jj], ALU.add,
                        reads=[self.psb[bk], asm.b], writes=[ub])
                self.copy("pool", ucbv, ucv, reads=[ub], writes=[ucbb])

            for cc in range(2):
                j = 2 * h + cc
                U = Ust[0]
                srcb = [self.uT_b[(sname, b)] for b in range(nblk)]
                self.dma("sp", U.ap[:, 2:2 + n], self.uT[j][:, col0:col0 + n], reads=srcb, writes=[U.b])
                if sname != "ctx":
                    if sname == "oth":
                        lsrc, lfl, rsrc, rfl = own0 + HALF - 2, 0, own0, 1
                        lb, rb = self.uT_b[("own", 7)], self.uT_b[("own", 0)]
                    else:
                        lsrc, lfl, rsrc, rfl = oth0 + HALF - 2, 1, oth0, 0
                        lb, rb = self.uT_b[("oth", 7)], self.uT_b[("oth", 0)]
                    self.dma("sp", U.ap[:, 0:2], self.uT[j][:, lsrc:lsrc + 2], reads=[lb], writes=[U.b])
                    self.dma("sp", U.ap[:, n + 2:n + 3], self.uT[j][:, rsrc:rsrc + 1], reads=[rb], writes=[U.b], slow=True)
                if pending is not None:
                    evac(pending)
                    pending = None
                yield
                if sname == "ctx":
                    self.memset("dve", U.ap[:, 0:2], 0.0, writes=[U.b])
                    self.memset("dve", U.ap[:, n + 2:n + 3], 0.0, writes=[U.b])
                else:
                    self.ts("dve", U.ap[:, 0:2], U.ap[:, 0:2], fl.ap[:, lfl:lfl + 1], ALU.mult,
                            reads=[U.b, fl.b], writes=[U.b])
                    self.ts("dve", U.ap[:, n + 2:n + 3], U.ap[:, n + 2:n + 3], fl.ap[:, rfl:rfl + 1], ALU.mult,
                            reads=[U.b, fl.b], writes=[U.b])
                for blk in range(nblk):
                    ncol = min(512, n - blk * 512)
                    c0 = blk * 512
                    bk = cvb.next()
                    for k in range(4):
                        self.mm(self.bank(bk)[:, 0:ncol], dg.ap[:, (cc * 4 + k) * 128:(cc * 4 + k + 1) * 128],
                                U.ap[:, c0 + k:c0 + k + ncol], k == 0, k == 3, reads=[dg.b, U.b], writes=[self.psb[bk]])
                    if pending is not None:
                        evac(pending)
                    pending = (bk, UCv[:, cc, c0:c0 + ncol], UC_b[par][cc][blk], UCbv[:, cc, c0:c0 + ncol],
                               UCb_b[par][cc][blk], j, ncol)
                    yield
            if pending is not None:
                evac(pending)

        def run_all(g):
            for _ in g:
                pass

        run_all(conv_gen(0))
        for idx in range(len(items)):
            if True:
                h, (sname, col0, n) = items[idx]
                par = idx % 2
                nblk = (n + 511) // 512
                UCv = UCs[par].ap.rearrange("p (c n) -> p c n", c=2)
                UCbv = UCbs[par].ap.rearrange("p (c n) -> p c n", c=2)
                cgen = conv_gen(idx + 1) if idx + 1 < len(items) else iter(())
                def chain(co, d, sname=sname, n=n, nblk=nblk, h=h, par=par, UCv=UCv, UCbv=UCbv):
                    j = 2 * h + co
                    bcol = d * NJ + j
                    cb = cbuf[co]
                    if sname == "ctx":
                        init = (0.0, None)
                    elif sname == "oth":
                        init = (s_ctx[co][d].ap, s_ctx[co][d].b)
                    else:
                        f = fl.ap[:, 1:2] if d == 0 else fl.ap[:, 0:1]
                        st_ = cb["stmp"]
                        self.tt("dve", st_.ap, s_oth[co][d].ap, s_ctx[co][d].ap, ALU.subtract,
                                reads=[s_oth[co][d].b, s_ctx[co][d].b], writes=[st_.b])
                        self.stt(s_ini[co][d].ap, st_.ap, f, s_ctx[co][d].ap, ALU.mult, ALU.add,
                                 reads=[st_.b, fl.b, s_ctx[co][d].b], writes=[s_ini[co][d].b])
                        init = (s_ini[co][d].ap, s_ini[co][d].b)
                    order = list(range(nblk)) if d == 0 else list(range(nblk - 1, -1, -1))
                    for bi, blk in enumerate(order):
                        ncol = min(512, n - blk * 512)
                        cols = slice(blk * 512, blk * 512 + ncol)
                        pr, pi = cb["pairs"].next()
                        for (pb, wt) in ((pr, rwb), (pi, iwb)):
                            for kc in range(2):
                                self.mm(self.bank(pb)[:, 0:ncol], gw(wt, d, h, kc, co), UCbv[:, kc, cols], kc == 0, kc == 1,
                                        reads=[wt.b, UCb_b[par][0][blk], UCb_b[par][1][blk]], writes=[self.psb[pb]])
                        yield
                        er, ei, a, m = cb["er"].next(), cb["ei"].next(), cb["a"].next(), cb["m"].next()
                        erv, eiv, av, mv = er.ap[:, 0:ncol], ei.ap[:, 0:ncol], a.ap[:, 0:ncol], m.ap[:, 0:ncol]
                        self.act(erv, self.bank(pr)[:, 0:ncol], AF.Exp, reads=[self.psb[pr], self.nrb.b], writes=[er.b],
                                 scale=-1.0, bias=self.nrb.ap[:, bcol:bcol + 1])
                        yield
                        self.act(erv, erv, AF.Ln, reads=[er.b], writes=[er.b], bias=1.0)
                        yield
                        self.act(erv, erv, AF.Exp, reads=[er.b], writes=[er.b], scale=-1.0)
                        yield
                        self.act(av, erv, AF.Exp, reads=[er.b, self.c8.b], writes=[a.b], scale=self.c8.ap[:, bcol:bcol + 1])
                        self.tt("pool", mv, av, av, ALU.mult, reads=[a.b], writes=[m.b])
                        yield
                        self.act(eiv, self.bank(pi)[:, 0:ncol], AF.Exp, reads=[self.psb[pi], self.nib.b], writes=[ei.b],
                                 scale=-1.0, bias=self.nib.ap[:, bcol:bcol + 1])
                        yield
                        self.act(eiv, eiv, AF.Ln, reads=[ei.b], writes=[ei.b], bias=1.0)
                        yield
                        first_ctx = (sname == "ctx" and bi == 0)
                        if first_ctx:
                            c0 = 0 if d == 0 else ncol - 1
                            ig0 = cb["stmp2"]
                            self.act(ig0.ap, ei.ap[:, c0:c0 + 1], AF.Exp, reads=[ei.b], writes=[ig0.b], scale=-1.0)
                        self.act(mv, mv, AF.Ln, reads=[m.b], writes=[m.b], scale=-1.0, bias=1.0)
                        yield
                        self.stt(eiv, mv, 0.5, eiv, ALU.mult, ALU.subtract, reads=[m.b, ei.b], writes=[ei.b])
                        self.act(eiv, eiv, AF.Exp, reads=[ei.b], writes=[ei.b])
                        yield
                        self.tt("dve", mv, eiv, UCv[:, co, cols], ALU.mult, reads=[ei.b, UC_b[par][co][blk]], writes=[m.b])
                        if first_ctx:
                            self.tt("dve", m.ap[:, c0:c0 + 1], ig0.ap, UCv[:, co, blk * 512 + c0: blk * 512 + c0 + 1], ALU.mult,
                                    reads=[ig0.b, UC_b[par][co][blk], m.b], writes=[m.b])
                        if sname == "own" and d == 0:
                            yap, yb = Yf[co][:, cols], Yf_b[co][blk]
                        else:
                            yd = cb["yd"].next()
                            yap, yb = yd.ap[:, 0:ncol], yd.b
                        ini_ap, ini_b = init
                        rd = [a.b, m.b] + ([ini_b] if ini_b is not None else [])
                        if d == 0:
                            self.P.op("dve", lambda e, o=yap, d0=av, d1=mv, ii=ini_ap: e.tensor_tensor_scan(
                                out=o, data0=d0, data1=d1, initial=ii, op0=ALU.mult, op1=ALU.add), rd, [yb])
                            init = (yap[:, ncol - 1:ncol], yb)
                        else:
                            self.P.op("dve", lambda e, o=yap, d0=av, d1=mv, ii=ini_ap: e.tensor_tensor_scan(
                                out=o[:, ::-1], data0=d0[:, ::-1], data1=d1[:, ::-1], initial=ii, op0=ALU.mult, op1=ALU.add),
                                rd, [yb])
                            init = (yap[:, 0:1], yb)
                        if sname == "own" and d == 1:
                            self.tt("pool", Yf[co][:, cols], Yf[co][:, cols], yap, ALU.add, reads=[Yf_b[co][blk], yb],
                                    writes=[Yf_b[co][blk]])
                        yield
                    if sname == "ctx":
                        self.copy("dve", s_ctx[co][d].ap, init[0], reads=[init[1]], writes=[s_ctx[co][d].b])
                    elif sname == "oth":
                        self.copy("dve", s_oth[co][d].ap, init[0], reads=[init[1]], writes=[s_oth[co][d].b])

                stepn = 0
                for d in range(2):
                    gens = [chain(0, d), chain(1, d)]
                    while gens:
                        for g_ in list(gens):
                            try:
                                next(g_)
                            except StopIteration:
                                gens.remove(g_)
                            stepn += 1
                            if stepn % 16 == 0:
                                next(cgen, None)
                run_all(cgen)
                if sname == "own":
                    for co in range(2):
                        j = 2 * h + co
                        for blk in range(8):
                            cols = slice(blk * 512, (blk + 1) * 512)
                            ggt, ygb = ggt_r.next(), ygb_r.next()
                            self.dma("sp", ggt.ap, self.gg[j][:, cols], reads=[self.gg_b[blk]], writes=[ggt.b])
                            self.tt("pool", ygb.ap, ggt.ap, Yf[co][:, cols], ALU.mult, reads=[ggt.b, Yf_b[co][blk]], writes=[ygb.b])
                            key = (j, blk)
                            self.yg_b[key] = Buf(f"yg{key}")
                            self.dma("sp", self.ygT[j][:, cols], ygb.ap, reads=[ygb.b], writes=[self.yg_b[key]])

    def phase_m(self, L, hh):
        I = self.I
        NT = 16
        xacc = self.alloc(NT * D, F32, "xacc")
        xa = [T(xacc[:, t * D:(t + 1) * D], f"xacc{t}") for t in range(NT)]
        hxT = self.alloc(KC * 2048, BF16, "hxT").rearrange("p (kc n) -> p kc n", kc=KC)
        hx_b = [[Buf(f"hx{kc}_{t}") for t in range(NT)] for kc in range(KC)]
        gates = self.alloc(NT * NE, F32, "gates")
        gate_b = [Buf(f"gate{t}") for t in range(NT)]
        G2 = self.tile(D, F32, "G2bc")
        self.dma("sp", G2.ap, self.GBC[2 * L + 1], reads=[self.GBC_b[2 * L + 1]], writes=[G2.b])
        rw = self.tile(KC * NE, F32, "rw")
        self.dma("sp", rw.ap.rearrange("p (kc n) -> p kc n", kc=KC), I["router_w"][L].rearrange("(kc p) n -> p kc n", p=128),
                 writes=[rw.b])
        ot_r = Ring([self.tile(D, F32, f"ot{i}") for i in range(2)])
        ep_junk = self.tile(D, BF16, "ep_junk")
        ep_ss = Ring([self.tile(1, F32, f"ep_ss{i}") for i in range(4)])
        ep_rstd = Ring([self.tile(1, F32, f"ep_rstd{i}") for i in range(4)])
        mark = self.top
        MSKEW = 2
        nb = self.norm_bufs(banks=(0, 2), nxs=MSKEW + 1)
        pendn = []
        hx32_r = Ring([self.tile(KC * 128, F32, f"hx32_{i}") for i in range(2)])
        hx32_b = [[Buf(f"hx32_{i}_{c}") for c in range(KC)] for i in range(2)]
        sm = {n: self.tile(w, F32, n) for n, w in [("th", 64), ("sc", 64), ("sel", 64), ("mx", 64), ("gs", 8), ("g8", 8),
                                                    ("pen", 8), ("selm", 64), ("t8", 8), ("mask", 64), ("den", 1)]}
        obanks = Ring([(4, 5), (6, 7)])
        skipP = 'prologue' in os.environ.get('MSKIP', '')
        if L == 0 and not skipP:
            G1 = self.tile(D, F32, "G1bc")
            self.dma("sp", G1.ap, self.GBC[0], reads=[self.GBC_b[0]], writes=[G1.b])
            woutb = self.tile(NJ * D, BF16, "woutb")
            wst = Ring([self.tile(D, F32, f"wst{i}") for i in range(2)])
            for jc in range(NJ):
                st = wst.next()
                self.dma("sp", st.ap, I["a_w_out"][jc * 128:(jc + 1) * 128, :], writes=[st.b])
                self.tt("pool", woutb.ap[:, jc * D:(jc + 1) * D], st.ap, G1.ap, ALU.mult, reads=[st.b, G1.b], writes=[woutb.b])
            ygt_r = Ring([self.tile(NJ * 128, BF16, f"ygt{i}") for i in range(2)])
        stage_b = lambda t_, xs_: self._m_router(L, t_, xs_, nb, hxT, hx_b, hx32_r, hx32_b, rw, sm, obanks, gates, gate_b)
        for t in range(NT):
            tg = hh * NT + t
            self.dma("sp", xa[t].ap, self.XS[tg * 128:(tg + 1) * 128, :], reads=[self.XS_b[tg]], writes=[xa[t].b])
            if L == 0 and not skipP:
                ygt = ygt_r.next()
                self.dma("sp", ygt.ap.rearrange("p (j n) -> p j n", j=NJ),
                         self.ygT.rearrange("j p n -> p j n")[:, :, tg * 128:(tg + 1) * 128],
                         reads=[self.yg_b[(j, tg // 4)] for j in range(NJ)], writes=[ygt.b])
                ob = obanks.next()
                for n in range(2):
                    for jc in range(NJ):
                        self.mm(self.bank(ob[n]), ygt.ap[:, jc * 128:(jc + 1) * 128],
                                woutb.ap[:, jc * D + n * 512: jc * D + (n + 1) * 512], jc == 0, jc == NJ - 1,
                                reads=[ygt.b, woutb.b], writes=[self.psb[ob[n]]])
                self.tt("dve", xa[t].ap, self.bank(ob[0], 2), xa[t].ap, ALU.add,
                        reads=[self.psb[ob[0]], self.psb[ob[1]], xa[t].b], writes=[xa[t].b])
            xs_ = self.norm_a(xa[t], nb)
            pendn.append((t, xs_))
            if len(pendn) > MSKEW:
                stage_b(*pendn.pop(0))
        while pendn:
            stage_b(*pendn.pop(0))
        self.P.barrier()
        self.top = mark
        self._m_experts(L, hh, xa, hxT, hx_b, gates, gate_b, G2, obanks, ot_r, ep_junk, ep_ss, ep_rstd)

    def _m_router(self, L, t, xs_, nb, hxT, hx_b, hx32_r, hx32_b, rw, sm, obanks, gates, gate_b):
        if True:
            si = t % 2
            h32 = hx32_r.next()
            h32v = h32.ap.rearrange("p (kc n) -> p kc n", kc=KC)
            self.norm_b(xs_, self.A2[L], self.B2[L],
                        lambda c, t=t: (hxT[:, c, t * 128:(t + 1) * 128], hx_b[c][t]), nb,
                        dst32=lambda c, h32v=h32v, si=si: (h32v[:, c, :], hx32_b[si][c]))
            self.copy("act", hxT[:, :, t * 128:(t + 1) * 128], h32v, reads=[hx32_b[si][c] for c in range(KC)],
                      writes=[hx_b[c][t] for c in range(KC)])
            ob = obanks.next()
            lg = self.bank(ob[0])[:, 0:NE]
            for kc in range(KC):
                self.mm(lg, h32v[:, kc, :], rw.ap[:, kc * NE:(kc + 1) * NE], kc == 0, kc == KC - 1,
                        reads=[hx32_b[si][kc], rw.b], writes=[self.psb[ob[0]]])
            th, sc, sel, mx, gs, g8, pen, selm, t8, mask, den = [sm[k] for k in
                                                                 ("th", "sc", "sel", "mx", "gs", "g8", "pen", "selm", "t8", "mask", "den")]
            self.act(th.ap, lg, AF.Tanh, reads=[self.psb[ob[0]]], writes=[th.b], scale=0.5)
            self.ts("dve", sc.ap, th.ap, 0.5, ALU.mult, 0.5, ALU.add, reads=[th.b], writes=[sc.b])
            self.tt("dve", sel.ap, sc.ap, self.rb_bc.ap[:, L * NE:(L + 1) * NE], ALU.add, reads=[sc.b, self.rb_bc.b], writes=[sel.b])
            for g in range(8):
                self.P.op("dve", lambda e, o=mx.ap[:, g * 8:(g + 1) * 8], i_=sel.ap[:, g * 8:(g + 1) * 8]: e.max(out=o, in_=i_),
                          [sel.b], [mx.b])
            mxv = mx.ap.rearrange("p (g k) -> p g k", k=8)
            self.tt("dve", gs.ap, mxv[:, :, 0], mxv[:, :, 1], ALU.add, reads=[mx.b], writes=[gs.b])
            self.P.op("dve", lambda e, o=g8.ap, i_=gs.ap: e.max(out=o, in_=i_), [gs.b], [g8.b])
            self.ts("dve", pen.ap, gs.ap, g8.ap[:, 3:4], ALU.is_ge, reads=[gs.b, g8.b], writes=[pen.b])
            self.ts("dve", pen.ap, pen.ap, -1.0, ALU.add, 1e30, ALU.mult, reads=[pen.b], writes=[pen.b])
            for g in range(8):
                self.ts("dve", selm.ap[:, g * 8:(g + 1) * 8], sel.ap[:, g * 8:(g + 1) * 8], pen.ap[:, g:g + 1], ALU.add,
                        reads=[sel.b, pen.b], writes=[selm.b])
            self.P.op("dve", lambda e, o=t8.ap, i_=selm.ap: e.max(out=o, in_=i_), [selm.b], [t8.b])
            self.ts("dve", mask.ap, selm.ap, t8.ap[:, 7:8], ALU.is_ge, reads=[selm.b, t8.b], writes=[mask.b])
            self.tt("dve", mask.ap, mask.ap, sc.ap, ALU.mult, reads=[mask.b, sc.b], writes=[mask.b])
            self.P.op("dve", lambda e, o=den.ap, i_=mask.ap: e.tensor_reduce(out=o, in_=i_, axis=AX.X, op=ALU.add),
                      [mask.b], [den.b])
            self.P.op("dve", lambda e, o=den.ap: e.reciprocal(out=o, in_=o), [den.b], [den.b])
            self.ts("dve", gates[:, t * NE:(t + 1) * NE], mask.ap, den.ap, ALU.mult, 2.5, ALU.mult,
                    reads=[mask.b, den.b], writes=[gate_b[t]])

    def _m_experts(self, L, hh, xa, hxT, hx_b, gates, gate_b, G2, obanks, ot_r, ep_junk, ep_ss, ep_rstd):
        I = self.I
        NT = 16
        wgu_r = Ring([self.tile(KC * 512, BF16, f"wgu{i}") for i in range(2)])
        wdf_r = Ring([self.tile(2 * D, F32, f"wdf{i}") for i in range(2)])
        wdb_r = Ring([self.tile(2 * D, BF16, f"wdb{i}") for i in range(2)])
        s_r = Ring([self.tile(512, F32, f"s{i}") for i in range(2)])
        actT_r = Ring([[self.tile(512, BF16, f"actT{i}_{f}") for f in range(2)] for i in range(3)])
        gubanks = Ring([(0, 1), (2, 3)])
        elist = list(range(NE + 1)) if 'NE_DBG' not in os.environ else list(range(int(os.environ['NE_DBG']))) + [NE]
        if 'expert' in os.environ.get('MSKIP', ''):
            elist = []
        pend_down = None
        for e in elist:
            wgu, wdf, wdb = wgu_r.next(), wdf_r.next(), wdb_r.next()
            src_gu = I["moe_w_gu"][L, e] if e < NE else I["sh_w_gu"][L]
            src_dn = I["moe_w_down"][L, e] if e < NE else I["sh_w_down"][L]
            wguv = wgu.ap.rearrange("p (kc n) -> p kc n", kc=KC)
            self.dma("q2", wguv, src_gu.rearrange("(kc p) n -> p kc n", p=128), writes=[wgu.b])
            self.dma("sp", wdf.ap.rearrange("p (f n) -> p f n", f=2), src_dn.rearrange("(f p) n -> p f n", p=128), writes=[wdf.b])
            for fh in range(2):
                self.tt("pool", wdb.ap[:, fh * D:(fh + 1) * D], wdf.ap[:, fh * D:(fh + 1) * D], G2.ap, ALU.mult,
                        reads=[wdf.b, G2.b], writes=[wdb.b])
            for blk in range(4):
                cols = slice(blk * 512, (blk + 1) * 512)
                actT = actT_r.next()
                for fh in range(2):
                    bg, bu = gubanks.next()
                    rd = [wgu.b]
                    for kc in range(KC):
                        rdk = rd + [hx_b[kc][blk * 4 + q] for q in range(4)]
                        self.mm(self.bank(bg), wguv[:, kc, fh * 128:(fh + 1) * 128], hxT[:, kc, cols], kc == 0, kc == KC - 1,
                                reads=rdk, writes=[self.psb[bg]])
                    for kc in range(KC):
                        rdk = rd + [hx_b[kc][blk * 4 + q] for q in range(4)]
                        self.mm(self.bank(bu), wguv[:, kc, 256 + fh * 128:256 + (fh + 1) * 128], hxT[:, kc, cols], kc == 0,
                                kc == KC - 1, reads=rdk, writes=[self.psb[bu]])
                    s = s_r.next()
                    self.act(s.ap, self.bank(bg), AF.Silu, reads=[self.psb[bg]], writes=[s.b])
                    self.tt("dve", actT[fh].ap, self.bank(bu), s.ap, ALU.mult, reads=[self.psb[bu], s.b], writes=[actT[fh].b])
                def down(blk=blk, actT=actT, wdb=wdb, e=e):
                    for q in range(4):
                        t = blk * 4 + q
                        ob = obanks.next()
                        for n in range(2):
                            for fh in range(2):
                                self.mm(self.bank(ob[n]), actT[fh].ap[:, q * 128:(q + 1) * 128],
                                        wdb.ap[:, fh * D + n * 512: fh * D + (n + 1) * 512], fh == 0, fh == 1,
                                        reads=[actT[fh].b, wdb.b], writes=[self.psb[ob[n]]])
                        gsc = gates[:, t * NE + e: t * NE + e + 1] if e < NE else 1.0
                        self.stt(xa[t].ap, self.bank(ob[0], 2), gsc, xa[t].ap, ALU.mult, ALU.add,
                                 reads=[self.psb[ob[0]], self.psb[ob[1]], xa[t].b] + ([gate_b[t]] if e < NE else []),
                                 writes=[xa[t].b])
                if pend_down is not None:
                    pend_down()
                pend_down = down
        if pend_down is not None:
            pend_down()
        for t in range(NT):
            tg = hh * NT + t
            if L == 0:
                self.dma("sp", self.XS[tg * 128:(tg + 1) * 128, :], xa[t].ap, reads=[xa[t].b], writes=[self.XS_b[tg]])
                if self.debug and self.stage == 3:
                    self.dma("sp", self.dbgX[tg * 128:(tg + 1) * 128, :], xa[t].ap, reads=[xa[t].b])
            else:
                ss, rstd = ep_ss.next(), ep_rstd.next()
                self.act(ep_junk.ap, xa[t].ap, AF.Square, reads=[xa[t].b], writes=[ss.b, ep_junk.b], accum_out=ss.ap)
                self.ts("pool", rstd.ap, ss.ap, 1.0 / D, ALU.mult, EPS, ALU.add, reads=[ss.b], writes=[rstd.b])
                self.tt("pool", rstd.ap, rstd.ap, self.neghalf.ap[:, 0:1], ALU.pow, reads=[rstd.b, self.neghalf.b], writes=[rstd.b])
                ot = ot_r.next()
                self.stt(ot.ap, xa[t].ap, rstd.ap, self.fin_bc.ap, ALU.mult, ALU.mult,
                         reads=[xa[t].b, rstd.b, self.fin_bc.b], writes=[ot.b])
                self.outs.append(self.dma("sp", self.out[tg * 128:(tg + 1) * 128, :], ot.ap, reads=[ot.b]))

    def phase_s1(self):
        I = self.I
        NF = 16
        winu = self.tile(KC * 2048, BF16, "winu")
        winv = self.tile(KC * 2048, BF16, "winv")
        winuv = winu.ap.rearrange("p (kc n) -> p kc n", kc=KC)
        winvv = winv.ap.rearrange("p (kc n) -> p kc n", kc=KC)
        src = I["b_w_in"].rearrange("(kc p) n -> p kc n", p=128)
        self.dma("q2", winuv, src[:, :, 0:2048], writes=[winu.b])
        self.dma("q2", winvv, src[:, :, 2048:4096], writes=[winv.b])
        woutb = self.tile(NF * D, BF16, "s_woutb")
        wsTb = self.tile(8 * 128, BF16, "wsTb")
        Cm = self.tile(NF * 128, F32, "Cmat")
        Gbc = self.tile(2048, BF16, "s_Gbc")
        lnG = self.lng_fm.ap[:, 0:16]
        lnB = self.lng_fm.ap[:, 16:32]
        mark = self.top
        G1 = self.tile(D, F32, "s_G1")
        self.dma("sp", G1.ap, self.GBC[2], reads=[self.GBC_b[2]], writes=[G1.b])
        wst = Ring([self.tile(D, F32, f"s_wst{i}") for i in range(2)])
        for fc in range(NF):
            st = wst.next()
            self.dma("sp", st.ap, I["b_w_out"][fc * 128:(fc + 1) * 128, :], writes=[st.b])
            self.tt("pool", woutb.ap[:, fc * D:(fc + 1) * D], st.ap, G1.ap, ALU.mult, reads=[st.b, G1.b], writes=[woutb.b])
        ws32 = self.tile(8 * 128, F32, "ws32")
        self.dma("sp", ws32.ap.rearrange("p (g q) -> p g q", g=8), I["b_w_s"].rearrange("g p q -> p g q"), writes=[ws32.b])
        wsT32 = self.tile(8 * 128, F32, "wsT32")
        rs_bc = self.tile(8 * 128, F32, "rs_bc")
        bs_bc = self.tile(8 * 128, F32, "bs_bc")
        bsrow = self.tile(D, F32, "bsrow")
        self.dma("sp", bsrow.ap[0:1, :], I["b_bs_row"], writes=[bsrow.b])
        for hb in range(2):
            for g4 in range(4):
                g = hb * 4 + g4
                self.tr(self.bank(hb)[:, g4 * 128:(g4 + 1) * 128], ws32.ap[:, g * 128:(g + 1) * 128], self.ident.ap,
                        reads=[ws32.b, self.ident.b], writes=[self.psb[hb]])
            self.copy("dve", wsT32.ap[:, hb * 512:(hb + 1) * 512], self.bank(hb), reads=[self.psb[hb]], writes=[wsT32.b])
        self.copy("pool", wsTb.ap, wsT32.ap, reads=[wsT32.b], writes=[wsTb.b])
        for hb in range(2):
            self.mm(self.bank(2 + hb), self.ones.ap, wsT32.ap[:, hb * 512:(hb + 1) * 512], True, True,
                    reads=[self.ones.b, wsT32.b], writes=[self.psb[2 + hb]])
            self.copy("dve", rs_bc.ap[:, hb * 512:(hb + 1) * 512], self.bank(2 + hb), reads=[self.psb[2 + hb]], writes=[rs_bc.b])
            self.mm(self.bank(4 + hb), self.ones.ap[0:1, :], bsrow.ap[0:1, hb * 512:(hb + 1) * 512], True, True,
                    reads=[self.ones.b, bsrow.b], writes=[self.psb[4 + hb]])
            self.copy("dve", bs_bc.ap[:, hb * 512:(hb + 1) * 512], self.bank(4 + hb), reads=[self.psb[4 + hb]], writes=[bs_bc.b])
        lnrow = self.tile(2048, F32, "lnrow")
        self.dma("sp", lnrow.ap[0:1, :], I["b_ln_row"][:, 0:2048], writes=[lnrow.b])
        for n4 in range(4):
            bk = 6 + n4 % 2
            self.mm(self.bank(bk), self.ones.ap[0:1, :], lnrow.ap[0:1, n4 * 512:(n4 + 1) * 512], True, True,
                    reads=[self.ones.b, lnrow.b], writes=[self.psb[bk]])
            self.copy("dve", Gbc.ap[:, n4 * 512:(n4 + 1) * 512], self.bank(bk), reads=[self.psb[bk]], writes=[Gbc.b])
        for fc in range(NF):
            g = fc // 2
            self.stt(Cm.ap[:, fc * 128:(fc + 1) * 128], rs_bc.ap[:, g * 128:(g + 1) * 128], lnB[:, fc:fc + 1],
                     bs_bc.ap[:, g * 128:(g + 1) * 128], ALU.mult, ALU.add,
                     reads=[rs_bc.b, bs_bc.b, self.lng_fm.b], writes=[Cm.b])
        self.P.barrier()
        self.top = mark
        SK = os.environ.get('SSKIP', '')
        if 'main' in SK:
            return
        xts = Ring([self.tile(D, F32, f"s_xt{i}") for i in range(2)])
        nb = self.norm_bufs(banks=(0,), nxs=1)
        hx_ap = [self.alloc(KC * 512, BF16, f"s_hx{i}").rearrange("p (kc n) -> p kc n", kc=KC) for i in range(2)]
        hx_b = [[[Buf(f"shx{i}_{c}_{q}") for q in range(4)] for c in range(KC)] for i in range(2)]
        uT = self.tile(NF * 512, BF16, "s_uT")
        uTv = uT.ap.rearrange("p (f n) -> p f n", f=NF)
        uT_b = [Buf(f"s_uT{f}") for f in range(NF)]
        v = self.tile(2048, F32, "s_v")
        v_b = [Buf(f"s_v{i}") for i in range(4)]
        vn_r = Ring([self.tile(2048, BF16, f"s_vn{i}") for i in range(2)])
        th_r = Ring([self.tile(1024, F32, f"s_th{i}") for i in range(1)])
        vn0 = self.tile(2048, BF16, "s_vn0")
        zT_r = Ring([self.tile(NF * 128, BF16, f"s_zT{i}") for i in range(2)])
        st_ = {n: self.tile(w, F32, "s_" + n) for n, w in [("s1", 4), ("s2", 2), ("mean", 1), ("ex2", 1), ("msq", 1),
                                                            ("rstd", 1), ("nmr", 1), ("t2", 2)]}
        uvb = Ring([2, 3])
        xres_r = Ring([self.tile(D, F32, f"s_xres{i}") for i in range(2)])

        def stage_n(blk):
            si = blk % 2
            hap = hx_ap[si]
            for q in range(4):
                tg = blk * 4 + q
                xt = xts.next()
                self.dma("sp", xt.ap, self.XS[tg * 128:(tg + 1) * 128, :], reads=[self.XS_b[tg]], writes=[xt.b])
                self.norm_tile(xt, self.A1[1], self.B1[1],
                               lambda c, q=q, hap=hap, si=si: (hap[:, c, q * 128:(q + 1) * 128], hx_b[si][c][q]), nb)

        def stage_u(blk):
            si = blk % 2
            hap = hx_ap[si]
            for fc in range(NF):
                bk = uvb.next()
                for kc in range(KC):
                    self.mm(self.bank(bk), winuv[:, kc, fc * 128:(fc + 1) * 128], hap[:, kc, :], kc == 0, kc == KC - 1,
                            reads=[winu.b] + [hx_b[si][kc][q] for q in range(4)], writes=[self.psb[bk]])
                self.act(uTv[:, fc, :], self.bank(bk), AF.Gelu_apprx_tanh, reads=[self.psb[bk]], writes=[uT_b[fc]])

        def stage_a(blk, q):
            si = blk % 2
            hap = hx_ap[si]
            s1, s2 = st_["s1"], st_["s2"]
            for cg in range(4):
                bk = uvb.next()
                for kc in range(KC):
                    self.mm(self.bank(bk), hap[:, kc, q * 128:(q + 1) * 128], winvv[:, kc, cg * 512:(cg + 1) * 512],
                            kc == 0, kc == KC - 1, reads=[winv.b, hx_b[si][kc][q]], writes=[self.psb[bk]])
                self.act(v.ap[:, cg * 512:(cg + 1) * 512], self.bank(bk), AF.Gelu_apprx_tanh,
                         reads=[self.psb[bk]], writes=[v_b[cg], s1.b], accum_out=s1.ap[:, cg:cg + 1])
            for hv in range(2):
                jk = nb["junkr"].next()
                self.act(jk.ap, v.ap[:, hv * 1024:(hv + 1) * 1024], AF.Square, reads=[v_b[2 * hv], v_b[2 * hv + 1]],
                         writes=[s2.b, jk.b], accum_out=s2.ap[:, hv:hv + 1])
            mean, ex2, msq, rstd, nmr, t2 = [st_[k] for k in ("mean", "ex2", "msq", "rstd", "nmr", "t2")]
            self.tt("pool", t2.ap, s1.ap[:, 0:2], s1.ap[:, 2:4], ALU.add, reads=[s1.b], writes=[t2.b])
            self.tt("pool", mean.ap, t2.ap[:, 0:1], t2.ap[:, 1:2], ALU.add, reads=[t2.b], writes=[mean.b])
            self.tt("pool", ex2.ap, s2.ap[:, 0:1], s2.ap[:, 1:2], ALU.add, reads=[s2.b], writes=[ex2.b])
            self.ts("pool", msq.ap, mean.ap, mean.ap, ALU.mult, 1.0 / (2048.0 * 2048.0), ALU.mult, reads=[mean.b], writes=[msq.b])
            self.ts("pool", ex2.ap, ex2.ap, 1.0 / 2048, ALU.mult, EPS, ALU.add, reads=[ex2.b], writes=[ex2.b])
            self.tt("pool", rstd.ap, ex2.ap, msq.ap, ALU.subtract, reads=[ex2.b, msq.b], writes=[rstd.b])
            self.tt("pool", rstd.ap, rstd.ap, self.neghalf.ap[:, 0:1], ALU.pow, reads=[rstd.b, self.neghalf.b], writes=[rstd.b])
            self.ts("pool", nmr.ap, mean.ap, rstd.ap, ALU.mult, -1.0 / 2048, ALU.mult, reads=[mean.b, rstd.b], writes=[nmr.b])
            vn = vn_r.next()
            self.ts("dve", vn0.ap, v.ap, rstd.ap, ALU.mult, nmr.ap, ALU.add, reads=v_b + [rstd.b, nmr.b], writes=[vn0.b])
            self.tt("dve", vn.ap, vn0.ap, Gbc.ap, ALU.mult, reads=[vn0.b, Gbc.b], writes=[vn.b])
            return vn

        def stage_s(blk, q, vn):
            for fc in range(NF):
                bk = 4 + fc // 4
                self.mm(self.bank(bk)[:, (fc % 4) * 128:(fc % 4 + 1) * 128], vn.ap[:, fc * 128:(fc + 1) * 128],
                        wsTb.ap[:, (fc // 2) * 128:(fc // 2 + 1) * 128], True, True,
                        reads=[vn.b, wsTb.b], writes=[self.psb[bk]])
            zT = zT_r.next()
            for hf in range(2):
                th = th_r.next()
                self.tt("dve", th.ap, self.bank(4 + 2 * hf, 2), Cm.ap[:, hf * 1024:(hf + 1) * 1024], ALU.add,
                        reads=[self.psb[4 + 2 * hf], self.psb[5 + 2 * hf], Cm.b], writes=[th.b])
                self.tt("dve", zT.ap[:, hf * 1024:(hf + 1) * 1024].rearrange("p (f n) -> p f n", f=8),
                        th.ap.rearrange("p (f n) -> p f n", f=8), uTv[:, hf * 8:(hf + 1) * 8, q * 128:(q + 1) * 128], ALU.mult,
                        reads=[th.b] + uT_b[hf * 8:(hf + 1) * 8], writes=[zT.b])
            return zT

        def stage_o(blk, q, zT):
            tg = blk * 4 + q
            xr = xres_r.next()
            self.dma("sp", xr.ap, self.XS[tg * 128:(tg + 1) * 128, :], reads=[self.XS_b[tg]], writes=[xr.b])
            for n in range(2):
                for fc in range(NF):
                    self.mm(self.bank(2 + n), zT.ap[:, fc * 128:(fc + 1) * 128], woutb.ap[:, fc * D + n * 512: fc * D + (n + 1) * 512],
                            fc == 0, fc == NF - 1, reads=[zT.b, woutb.b], writes=[self.psb[2 + n]])
            self.tt("dve", xr.ap, self.bank(2, 2), xr.ap, ALU.add, reads=[self.psb[2], self.psb[3], xr.b], writes=[xr.b])
            self.dma("sp", self.XS[tg * 128:(tg + 1) * 128, :], xr.ap, reads=[xr.b], writes=[self.XS_b[tg]])
            if self.debug and self.stage == 4:
                self.dma("sp", self.dbgX[tg * 128:(tg + 1) * 128, :], xr.ap, reads=[xr.b])

        pendS, pendO = [], []

        def step_s():
            if pendS:
                b_, q_, vn_ = pendS.pop(0)
                pendO.append((b_, q_, stage_s(b_, q_, vn_)))

        def step_o():
            if pendO:
                stage_o(*pendO.pop(0))

        stage_n(0)
        for blk in range(8):
            while pendS:
                step_o()
                step_s()
            stage_u(blk)
            for q in range(4):
                vn = stage_a(blk, q)
                step_o()
                step_s()
                pendS.append((blk, q, vn))
                if q == 1 and blk + 1 < 8:
                    stage_n(blk + 1)
        while pendS or pendO:
            step_o()
            step_s()


def _fm(v, nchunk):
    return np.ascontiguousarray(np.asarray(v, np.float32).reshape(nchunk, 128).T)


def _sincos_table():
    quarter = D // 4
    omega = (1.0 / (np.float32(10000.0) ** (np.arange(quarter, dtype=np.float32) / np.float32(quarter)))).astype(np.float32)

    def emb(n):
        p = np.arange(n, dtype=np.float32)[:, None] * omega[None, :]
        return np.concatenate([np.sin(p), np.cos(p)], axis=-1).astype(np.float32)
    rows = SEQ // 64
    er, ec = emb(rows), emb(64)
    pe = np.concatenate([np.broadcast_to(er[:, None, :], (rows, 64, D // 2)),
                         np.broadcast_to(ec[None, :, :], (rows, 64, D // 2))], axis=-1)
    return np.ascontiguousarray(pe.reshape(SEQ, D).astype(np.float32))


def make_in_maps(inp):
    f = lambda a: np.ascontiguousarray(np.asarray(a, np.float32))
    pe = _sincos_table()
    shared = {}
    shared["ident"] = np.eye(128, dtype=np.float32)
    shared["ada_w"] = f(inp["ada_w"])
    ab = f(inp["ada_b"])
    abf = np.stack([_fm(ab[i], 48) for i in range(2)], 0)
    shared["adab_fm"] = np.ascontiguousarray(np.repeat(abf.transpose(1, 0, 2)[:, :, :, None], 2, axis=3).reshape(128, 192))
    shared["adab_row"] = f(ab.reshape(1, -1))
    shared["ng"] = np.ascontiguousarray(np.concatenate(
        [_fm(inp["mix_norm_g"][0], 8), _fm(inp["ffn_norm_g"][0], 8), _fm(inp["mix_norm_g"][1], 8), _fm(inp["ffn_norm_g"][1], 8)], axis=1))
    shared["fin_row"] = f(inp["final_norm_g"]).reshape(1, D)
    shared["a_w_in"] = f(inp["a_w_in"][0])
    cw = f(inp["a_conv_w"][0])
    cwf = np.stack([_fm(cw[k], 10) for k in range(4)], axis=2).reshape(128, 40)
    parts = [cwf, _fm(inp["a_conv_b"][0], 10)]
    for nm in ("a_gate_r_b", "a_gate_i_b", "a_lambda"):
        v = f(inp[nm][0])
        parts.append(np.concatenate([_fm(v[0], 10), _fm(v[1], 10)], axis=1))
    shared["a_small"] = np.ascontiguousarray(np.concatenate(parts, axis=1))
    shared["a_rw"] = f(inp["a_gate_r_w"][0])
    shared["a_iw"] = f(inp["a_gate_i_w"][0])
    shared["a_w_out"] = f(inp["a_w_out"][0])
    shared["b_w_in"] = f(inp["b_w_in"][0])
    shared["b_ln_row"] = np.ascontiguousarray(np.concatenate([f(inp["b_ln_g"][0]), f(inp["b_ln_b"][0])]).reshape(1, 4096))
    shared["b_lng_fm"] = np.ascontiguousarray(np.concatenate([_fm(inp["b_ln_g"][0], 16), _fm(inp["b_ln_b"][0], 16)], axis=1))
    shared["b_w_s"] = f(inp["b_w_s"][0])
    shared["b_bs_row"] = f(inp["b_b_s"][0]).reshape(1, 1024)
    shared["b_w_out"] = f(inp["b_w_out"][0])
    shared["router_w"] = f(inp["router_w"])
    shared["rb_row"] = f(inp["router_b"]).reshape(1, 2 * NE)
    shared["moe_w_gu"] = f(inp["moe_w_gu"])
    shared["moe_w_down"] = f(inp["moe_w_down"])
    shared["sh_w_gu"] = f(inp["shared_w_gu"])
    shared["sh_w_down"] = f(inp["shared_w_down"])
    x = f(inp["x"])
    ctx = f(inp["ctx"])
    c = f(inp["c"])
    cc = f(inp["c_ctx"])
    maps = []
    for k in range(8):
        b, h = k // 2, k % 2
        m = dict(shared)
        m["x_own"] = np.ascontiguousarray(x[b, h * HALF:(h + 1) * HALF])
        m["x_oth"] = np.ascontiguousarray(x[b, (1 - h) * HALF:(2 - h) * HALF])
        m["pe_own"] = np.ascontiguousarray(pe[h * HALF:(h + 1) * HALF])
        m["pe_oth"] = np.ascontiguousarray(pe[(1 - h) * HALF:(2 - h) * HALF])
        m["ctxb"] = np.ascontiguousarray(ctx[b])
        cv = np.stack([_fm(c[b], 8), _fm(cc, 8)], axis=2).reshape(128, 16)
        m["cvec"] = np.ascontiguousarray(cv)
        fl = np.zeros((128, 2), np.float32)
        fl[:, h] = 1.0
        m["flags"] = fl
        maps.append(m)
    return maps


_NC_CACHE = {}


def kernel(**inputs):
    if "nc" not in _NC_CACHE:
        _NC_CACHE["nc"] = Builder(stage=99).build()
    nc = _NC_CACHE["nc"]
    maps = make_in_maps(inputs)
    res = run_bass_kernel_spmd(nc, maps, core_ids=list(range(8)))
    out = np.empty((4, SEQ, D), np.float32)
    for k in range(8):
        b, h = k // 2, k % 2
        out[b, h * HALF:(h + 1) * HALF] = res.results[k]["out"]
    return out
```

```python
import os
import numpy as np
import concourse.bass as bass
import concourse.mybir as mybir
from concourse.bass_utils import run_bass_kernel_spmd
from contextlib import ExitStack

F32 = mybir.dt.float32
BF16 = mybir.dt.bfloat16
AF = mybir.ActivationFunctionType
ALU = mybir.AluOpType
AX = mybir.AxisListType

D = 1024
KC = 8
SEQ = 8192
HALF = 4096
CTX = 256
RW = 1280
NJ = 10
NH = 5
NE = 64
EPS = 1e-6
NTOT = CTX + 2 * HALF

COMPUTE = ("pe", "act", "dve", "pool")
QUEUES = ("sp", "q2")
NSEM = 8
SAME_ENGINE_SYNC = True


class Buf:
    __slots__ = ("name", "w", "r")

    def __init__(self, name=""):
        self.name = name
        self.w = None
        self.r = []


class Op:
    __slots__ = ("eng", "fn", "deps", "signal", "val", "idx", "is_dma", "qn")

    def __init__(self, eng, fn, is_dma=False):
        self.eng = eng
        self.fn = fn
        self.deps = []
        self.signal = False
        self.val = None
        self.is_dma = is_dma
        self.qn = 0


class Prog:
    def __init__(self):
        self.streams = {"pe": [], "act": [], "dve": [], "pool": [], "sp": []}
        self.ndma = {"sp": 0, "q2": 0}
        self.ring = {"sp": [None] * NSEM, "q2": [None] * NSEM}

    @staticmethod
    def _stream_of(eng):
        return "pool" if eng == "q2" else eng

    def op(self, eng, fn, reads=(), writes=(), extra=()):
        is_dma = eng in QUEUES
        o = Op(eng, fn, is_dma)
        if is_dma:
            o.qn = self.ndma[eng]
            self.ndma[eng] += 1
            self.ring[eng][o.qn % NSEM] = o
        st = self._stream_of(eng)
        cand = []
        for b in reads:
            if b.w is not None:
                cand.append((b.w, True))
        for b in writes:
            if b.w is not None:
                cand.append((b.w, True))
            for r in b.r:
                cand.append((r, False))
        for d in extra:
            cand.append((d, True))
        seen = set()
        for d, strong in cand:
            if d is o or id(d) in seen:
                continue
            same = (not d.is_dma) and (not is_dma) and self._stream_of(d.eng) == st
            if same:
                if d.eng == "pe" or not SAME_ENGINE_SYNC:
                    continue
            seen.add(id(d))
            o.deps.append(d)
            d.signal = True
        for b in reads:
            b.r.append(o)
        for b in writes:
            b.w = o
            b.r = []
        o.idx = len(self.streams[st])
        self.streams[st].append(o)
        return o

    def barrier(self):
        last = []
        for st, ops in self.streams.items():
            for o in reversed(ops):
                if not o.is_dma:
                    last.append(o)
                    break
        for q in QUEUES:
            for o in self.ring[q]:
                if o is not None:
                    last.append(o)
        for st in ("pe", "act", "dve", "pool", "sp"):
            self.op(st, (lambda e: e.nop()), extra=[d for d in last])

    def assign(self):
        for st, ops in self.streams.items():
            cnt = 0
            for o in ops:
                if o.is_dma:
                    o.val = 16 * (o.qn // NSEM + 1)
                elif o.signal:
                    cnt += 1
                    o.val = cnt

    def emit_stream(self, st, engobj, sem_eng, sem_dma):
        waited = {}

        def wait(key, sem, val):
            if waited.get(key, 0) >= val:
                return
            engobj.wait_ge(sem, val)
            waited[key] = val

        for o in self.streams[st]:
            for d in o.deps:
                if d.is_dma:
                    wait((d.eng, d.qn % NSEM), sem_dma[d.eng][d.qn % NSEM], d.val)
                else:
                    dst = self._stream_of(d.eng)
                    wait(dst, sem_eng[dst], d.val)
            if o.is_dma:
                if o.qn >= NSEM:
                    wait((o.eng, o.qn % NSEM), sem_dma[o.eng][o.qn % NSEM], o.val - 16)
                ins = o.fn(engobj)
                ins.then_inc(sem_dma[o.eng][o.qn % NSEM], 16)
            else:
                ins = o.fn(engobj)
                if o.signal:
                    ins.then_inc(sem_eng[st], 1)

    def final_waits(self, engobj, sem_eng, sem_dma, ops):
        for d in ops:
            if d.is_dma:
                engobj.wait_ge(sem_dma[d.eng][d.qn % NSEM], d.val)
            else:
                engobj.wait_ge(sem_eng[self._stream_of(d.eng)], d.val)


def run_prog(nc, P, final_ops):
    P.assign()
    with ExitStack() as es:
        sem_eng = {s: es.enter_context(nc.semaphore("sem_" + s)) for s in COMPUTE}
        sem_dma = {q: [es.enter_context(nc.semaphore(f"semd_{q}_{i}")) for i in range(NSEM)]
                   for q in QUEUES}
        block = es.enter_context(nc.Block())

        @block.tensor
        def _(e):
            P.emit_stream("pe", e, sem_eng, sem_dma)

        @block.scalar
        def _(e):
            P.emit_stream("act", e, sem_eng, sem_dma)

        @block.vector
        def _(e):
            P.emit_stream("dve", e, sem_eng, sem_dma)

        @block.gpsimd
        def _(e):
            P.emit_stream("pool", e, sem_eng, sem_dma)

        @block.sync
        def _(e):
            P.emit_stream("sp", e, sem_eng, sem_dma)
            P.final_waits(e, sem_eng, sem_dma, final_ops)


class T:
    __slots__ = ("ap", "b")

    def __init__(self, ap, name=""):
        self.ap = ap
        self.b = Buf(name)


class Ring:
    def __init__(self, items):
        self.items = items
        self.i = 0

    def next(self):
        t = self.items[self.i % len(self.items)]
        self.i += 1
        return t


ARENA_COLS = 52992


class Builder:
    def __init__(self, stage=99, debug=False):
        self.stage = stage
        self.debug = debug
        self.nc = bass.Bass("TRN2", target_bir_lowering=False)
        self.P = Prog()
        self.outs = []
        self.es = ExitStack()

    def din(self, name, shape, dt=F32):
        return self.nc.dram_tensor(name, list(shape), dt, kind="ExternalInput").ap()

    def dscr(self, name, shape, dt=F32, out=False):
        kind = "ExternalOutput" if out else "Internal"
        return self.nc.dram_tensor(name, list(shape), dt, kind=kind).ap()

    def alloc(self, ncols, dt=F32, name=""):
        n32 = ncols if dt == F32 else (ncols + 1) // 2
        assert self.top + n32 <= ARENA_COLS, f"arena overflow {self.top}+{n32} ({name})"
        ap = self.arena[:, self.top:self.top + n32]
        self.top += n32
        if dt != F32:
            ap = ap.bitcast(dt)
        return ap

    def tile(self, ncols, dt=F32, name=""):
        return T(self.alloc(ncols, dt, name), name)

    def dma(self, q, out, in_, reads=(), writes=(), slow=False):
        if slow:
            return self.P.op(q, lambda e: e.dma_start(out=out, in_=in_, allow_slow_non_contiguous=True), reads, writes)
        return self.P.op(q, lambda e: e.dma_start(out=out, in_=in_), reads, writes)

    def act(self, out, in_, func, reads=(), writes=(), **kw):
        return self.P.op("act", lambda e: e.activation(out=out, in_=in_, func=func, **kw), reads, writes)

    def ts(self, eng, out, in0, s1, op0, s2=None, op1=None, reads=(), writes=()):
        if op1 is None:
            return self.P.op(eng, lambda e: e.tensor_scalar(out=out, in0=in0, scalar1=s1, scalar2=None, op0=op0),
                             reads, writes)
        return self.P.op(eng, lambda e: e.tensor_scalar(out=out, in0=in0, scalar1=s1, scalar2=s2, op0=op0, op1=op1),
                         reads, writes)

    def tt(self, eng, out, in0, in1, op, reads=(), writes=()):
        return self.P.op(eng, lambda e: e.tensor_tensor(out=out, in0=in0, in1=in1, op=op), reads, writes)

    def stt(self, out, in0, scalar, in1, op0, op1, reads=(), writes=()):
        return self.P.op("dve", lambda e: e.scalar_tensor_tensor(out=out, in0=in0, scalar=scalar, in1=in1,
                                                                 op0=op0, op1=op1), reads, writes)

    def mm(self, out, lhsT, rhs, start, stop, reads=(), writes=()):
        return self.P.op("pe", lambda e: e.matmul(out, lhsT, rhs, start=start, stop=stop), reads, writes)

    def tr(self, out, in_, ident, reads=(), writes=()):
        return self.P.op("pe", lambda e: e.transpose(out=out, in_=in_, identity=ident), reads, writes)

    def copy(self, eng, out, in_, reads=(), writes=()):
        if eng == "act":
            return self.P.op("act", lambda e: e.copy(out=out, in_=in_), reads, writes)
        return self.P.op(eng, lambda e: e.tensor_copy(out=out, in_=in_), reads, writes)

    def memset(self, eng, ap, val, writes=()):
        return self.P.op(eng, lambda e: e.memset(ap, val), (), writes)

    def declare(self):
        dbg = self.debug
        I = {}
        I["x_own"] = self.din("x_own", [HALF, D])
        I["x_oth"] = self.din("x_oth", [HALF, D])
        I["ctxb"] = self.din("ctxb", [CTX, D])
        I["pe_own"] = self.din("pe_own", [HALF, D])
        I["pe_oth"] = self.din("pe_oth", [HALF, D])
        I["cvec"] = self.din("cvec", [128, 16])
        I["flags"] = self.din("flags", [128, 2])
        I["ident"] = self.din("ident", [128, 128])
        I["ada_w"] = self.din("ada_w", [2, D, 6 * D])
        I["adab_fm"] = self.din("adab_fm", [128, 2 * 96])
        I["adab_row"] = self.din("adab_row", [1, 2 * 6 * D])
        I["ng"] = self.din("ng", [128, 4 * KC])
        I["fin_row"] = self.din("fin_row", [1, D])
        I["a_w_in"] = self.din("a_w_in", [D, 2 * RW])
        I["a_small"] = self.din("a_small", [128, 110])
        I["a_rw"] = self.din("a_rw", [2, NH, 256, 256])
        I["a_iw"] = self.din("a_iw", [2, NH, 256, 256])
        I["a_w_out"] = self.din("a_w_out", [RW, D])
        I["b_w_in"] = self.din("b_w_in", [D, 4096])
        I["b_ln_row"] = self.din("b_ln_row", [1, 4096])
        I["b_lng_fm"] = self.din("b_lng_fm", [128, 32])
        I["b_w_s"] = self.din("b_w_s", [8, 128, 128])
        I["b_bs_row"] = self.din("b_bs_row", [1, 1024])
        I["b_w_out"] = self.din("b_w_out", [2048, D])
        I["router_w"] = self.din("router_w", [2, D, NE])
        I["rb_row"] = self.din("rb_row", [1, 2 * NE])
        if self.stage >= 3:
            I["moe_w_gu"] = self.din("moe_w_gu", [2, NE, D, 512])
            I["moe_w_down"] = self.din("moe_w_down", [2, NE, 256, D])
        I["sh_w_gu"] = self.din("sh_w_gu", [2, D, 512])
        I["sh_w_down"] = self.din("sh_w_down", [2, 256, D])
        self.I = I
        st = self.stage
        self.out = self.nc.dram_tensor("out", [HALF, D], F32, kind="ExternalOutput").ap()
        self.uT = self.dscr("uT", [NJ, 128, NTOT], F32, out=(dbg and st == 1))
        self.gg = self.dscr("gg", [NJ, 128, HALF], F32, out=(dbg and st == 1))
        self.ygT = self.dscr("ygT", [NJ, 128, HALF], BF16, out=(dbg and st == 2))
        self.XS = self.dscr("XS", [HALF, D], F32)
        self.dbgX = self.dscr("dbgX", [HALF, D], F32, out=True) if dbg else None
        self.GBC = self.dscr("GBC", [4, 128, D], F32, out=(dbg and st == 1))
        self.modd = self.dscr("modd", [128, 192], F32, out=(dbg and st == 1))
        self.uT_b = {}
        self.gg_b = {}
        self.yg_b = {}
        self.XS_b = [Buf(f"XS{t}") for t in range(32)]
        self.GBC_b = [Buf(f"GBC{i}") for i in range(4)]

    def build(self):
        nc = self.nc
        self.declare()
        es = self.es
        self.arena = es.enter_context(nc.sbuf_tensor("arena", [128, ARENA_COLS], F32))
        self.ps = es.enter_context(nc.psum_tensor("ps", [128, 4096], F32))
        self.psb = [Buf(f"bank{i}") for i in range(8)]
        self.top = 0
        self.phase_pro()
        self.const_top = self.top
        if self.stage >= 1:
            self.phase_a0()
        dbgskip = os.environ.get('SKIPSTAGES', '')
        if self.stage >= 2 and 'b0' not in dbgskip:
            self.P.barrier()
            self.top = self.const_top
            self.phase_b0()
        if self.stage >= 3 and 'm0' not in dbgskip:
            for hh in range(2):
                self.P.barrier()
                self.top = self.const_top
                self.phase_m(0, hh)
        if self.stage >= 4:
            self.P.barrier()
            self.top = self.const_top
            self.phase_s1()
        if self.stage >= 5:
            for hh in range(2):
                self.P.barrier()
                self.top = self.const_top
                self.phase_m(1, hh)
        if self.stage < 5:
            self.P.barrier()
            self.top = self.const_top
            z = self.tile(D, F32, "zero")
            self.memset("dve", z.ap, 0.0, writes=[z.b])
            for t in range(32):
                self.outs.append(self.dma("sp", self.out[t * 128:(t + 1) * 128, :], z.ap, reads=[z.b]))
        self.P.barrier()
        run_prog(nc, self.P, self.outs)
        es.close()
        return nc

    def bank(self, b, n=1):
        return self.ps[:, b * 512:(b + n) * 512]

    def phase_pro(self):
        I = self.I
        c = {}
        for name, n in [("ident", 128), ("cvec", 16), ("flags", 2), ("adab_fm", 192), ("ng", 32),
                        ("a_small", 110), ("b_lng_fm", 32)]:
            c[name] = self.tile(n, F32, name)
            self.dma("sp", c[name].ap, I[name], writes=[c[name].b])
        ones = self.tile(128, F32, "ones")
        self.memset("dve", ones.ap, 1.0, writes=[ones.b])
        self.ident, self.ones, self.flags = c["ident"], ones, c["flags"]
        silu = self.tile(16, F32, "silu_c")
        self.act(silu.ap, c["cvec"].ap, AF.Silu, reads=[c["cvec"].b], writes=[silu.b])
        mod = self.tile(192, F32, "mod")
        self.mod = mod
        fin_bc = self.tile(D, F32, "fin_bc")
        self.fin_bc = fin_bc
        rb_bc = self.tile(2 * NE, F32, "rb_bc")
        self.rb_bc = rb_bc
        self.A1 = [self.tile(KC, F32, f"A1_{i}") for i in range(2)]
        self.B1 = [self.tile(KC, F32, f"B1_{i}") for i in range(2)]
        self.A2 = [self.tile(KC, F32, f"A2_{i}") for i in range(2)]
        self.B2 = [self.tile(KC, F32, f"B2_{i}") for i in range(2)]
        self.A1c = self.tile(KC, F32, "A1c")
        self.B1c = self.tile(KC, F32, "B1c")
        self.asm = c["a_small"]
        self.nrb = self.tile(20, F32, "nrb")
        self.nib = self.tile(20, F32, "nib")
        self.c8 = self.tile(20, F32, "c8")
        self.c16 = self.tile(20, F32, "c16")
        self.neghalf = self.tile(8, F32, "neghalf")
        self.memset("dve", self.neghalf.ap, -0.5, writes=[self.neghalf.b])
        self.lng_fm = c["b_lng_fm"]
        mark = self.top
        srep = self.tile(KC * 128, F32, "silu_rep")
        for kc in range(KC):
            self.ts("dve", srep.ap[:, kc * 128:(kc + 1) * 128], ones.ap, silu.ap[:, 2 * kc:2 * kc + 1], ALU.mult,
                    reads=[ones.b, silu.b], writes=[srep.b])
        rows = self.tile(2 * 6 * D, F32, "adab_row")
        self.dma("sp", rows.ap[0:1, :], I["adab_row"], writes=[rows.b])
        frow = self.tile(D + 2 * NE, F32, "fin_row")
        self.dma("sp", frow.ap[0:1, 0:D], I["fin_row"], writes=[frow.b])
        self.dma("sp", frow.ap[0:1, D:D + 2 * NE], I["rb_row"], writes=[frow.b])
        adat = Ring([self.tile(KC * 512, F32, f"adat{i}") for i in range(2)])
        gst = Ring([self.tile(512, F32, f"gst{i}") for i in range(2)])
        for n in range(2):
            self.mm(self.bank(3)[:, 0:512], ones.ap[0:1, :], frow.ap[0:1, n * 512:(n + 1) * 512], True, True,
                    reads=[ones.b, frow.b], writes=[self.psb[3]])
            self.copy("dve", fin_bc.ap[:, n * 512:(n + 1) * 512], self.bank(3), reads=[self.psb[3]], writes=[fin_bc.b])
        self.mm(self.bank(3)[:, 0:2 * NE], ones.ap[0:1, :], frow.ap[0:1, D:D + 2 * NE], True, True,
                reads=[ones.b, frow.b], writes=[self.psb[3]])
        self.copy("dve", rb_bc.ap, self.bank(3)[:, 0:2 * NE], reads=[self.psb[3]], writes=[rb_bc.b])
        for i in range(2):
            psA = self.bank(0)[:, 0:96]
            for g in range(12):
                at = adat.next()
                src = I["ada_w"][i][:, g * 512:(g + 1) * 512].rearrange("(kc p) n -> p kc n", p=128)
                self.dma("sp" if g % 2 == 0 else "q2", at.ap.rearrange("p (kc n) -> p kc n", kc=KC), src, writes=[at.b])
                for fc in range(4):
                    col = (g * 4 + fc) * 2
                    for kc in range(KC):
                        self.mm(psA[:, col:col + 2], at.ap[:, kc * 512 + fc * 128: kc * 512 + (fc + 1) * 128],
                                silu.ap[:, 2 * kc:2 * kc + 2], kc == 0, kc == KC - 1,
                                reads=[at.b, silu.b], writes=[self.psb[0]])
                if g in (4, 5, 10, 11):
                    bk = 1 + (g % 2)
                    for kc in range(KC):
                        self.mm(self.bank(bk), srep.ap[:, kc * 128:(kc + 1) * 128], at.ap[:, kc * 512:(kc + 1) * 512],
                                kc == 0, False, reads=[srep.b, at.b], writes=[self.psb[bk]])
                    self.mm(self.bank(bk), ones.ap[0:1, :], rows.ap[0:1, i * 6 * D + g * 512: i * 6 * D + (g + 1) * 512],
                            False, True, reads=[ones.b, rows.b], writes=[self.psb[bk]])
                    s = gst.next()
                    self.copy("dve", s.ap, self.bank(bk), reads=[self.psb[bk]], writes=[s.b])
                    idx = i * 2 + (0 if g < 6 else 1)
                    self.dma("sp", self.GBC[idx][:, (g % 2) * 512:(g % 2 + 1) * 512], s.ap, reads=[s.b],
                             writes=[self.GBC_b[idx]])
            self.tt("dve", mod.ap[:, i * 96:(i + 1) * 96], psA, c["adab_fm"].ap[:, i * 96:(i + 1) * 96], ALU.add,
                    reads=[self.psb[0], c["adab_fm"].b], writes=[mod.b])
        if self.debug and self.stage == 1:
            self.dma("sp", self.modd, mod.ap, reads=[mod.b])

        def mcol(i, chunk0, which):
            base = i * 96 + chunk0 * 2 + which
            return mod.ap[:, base: base + 2 * KC: 2]
        ng = c["ng"].ap
        tmp = self.tile(KC, F32, "tmpk")
        for i in range(2):
            for (A, Bv, gi, sc_chunk, sh_chunk, which) in [
                    (self.A1[i], self.B1[i], 2 * i, 8, 0, 0), (self.A2[i], self.B2[i], 2 * i + 1, 32, 24, 0)]:
                self.ts("dve", tmp.ap, mcol(i, sc_chunk, which), 1.0, ALU.add, reads=[mod.b], writes=[tmp.b])
                self.tt("dve", A.ap, tmp.ap, ng[:, gi * KC:(gi + 1) * KC], ALU.mult, reads=[tmp.b, c["ng"].b], writes=[A.b])
                self.copy("dve", Bv.ap, mcol(i, sh_chunk, which), reads=[mod.b], writes=[Bv.b])
        self.ts("dve", tmp.ap, mcol(0, 8, 1), 1.0, ALU.add, reads=[mod.b], writes=[tmp.b])
        self.tt("dve", self.A1c.ap, tmp.ap, ng[:, 0:KC], ALU.mult, reads=[tmp.b, c["ng"].b], writes=[self.A1c.b])
        self.copy("dve", self.B1c.ap, mcol(0, 0, 1), reads=[mod.b], writes=[self.B1c.b])
        asm = self.asm
        self.ts("dve", self.nrb.ap, asm.ap[:, 50:70], -1.0, ALU.mult, reads=[asm.b], writes=[self.nrb.b])
        self.ts("dve", self.nib.ap, asm.ap[:, 70:90], -1.0, ALU.mult, reads=[asm.b], writes=[self.nib.b])
        t20 = self.tile(20, F32, "t20")
        self.act(t20.ap, asm.ap[:, 90:110], AF.Exp, reads=[asm.b], writes=[t20.b], scale=-1.0)
        self.act(t20.ap, t20.ap, AF.Ln, reads=[t20.b], writes=[t20.b], bias=1.0)
        self.ts("dve", self.c8.ap, t20.ap, -8.0, ALU.mult, reads=[t20.b], writes=[self.c8.b])
        self.ts("dve", self.c16.ap, t20.ap, -16.0, ALU.mult, reads=[t20.b], writes=[self.c16.b])
        self.P.barrier()
        self.top = mark

    def norm_tile(self, xt, A, Bv, dst, nb, dst32=None):
        st = self.norm_a(xt, nb)
        self.norm_b(st, A, Bv, dst, nb, dst32)

    def norm_a(self, xt, nb):
        junk, ss, rstd, xs = nb["junkr"].next(), nb["ss"].next(), nb["rstd"].next(), nb["xs"].next()
        self.act(junk.ap, xt.ap, AF.Square, reads=[xt.b], writes=[ss.b, junk.b], accum_out=ss.ap)
        self.ts("pool", rstd.ap, ss.ap, 1.0 / D, ALU.mult, EPS, ALU.add, reads=[ss.b], writes=[rstd.b])
        self.tt("pool", rstd.ap, rstd.ap, self.neghalf.ap[:, 0:1], ALU.pow, reads=[rstd.b, self.neghalf.b],
                writes=[rstd.b])
        self.ts("dve", xs.ap, xt.ap, rstd.ap, ALU.mult, reads=[xt.b, rstd.b], writes=[xs.b])
        return xs

    def norm_b(self, xs, A, Bv, dst, nb, dst32=None):
        bk = nb["bk"].next()
        for c in range(KC):
            b = bk + c // 4
            self.tr(self.bank(b)[:, (c % 4) * 128:(c % 4 + 1) * 128], xs.ap[:, c * 128:(c + 1) * 128],
                    self.ident.ap, reads=[xs.b, self.ident.b], writes=[self.psb[b]])
        for c in range(KC):
            b = bk + c // 4
            src = self.bank(b)[:, (c % 4) * 128:(c % 4 + 1) * 128]
            if dst32 is not None:
                ap32, b32 = dst32(c)
                self.ts("dve", ap32, src, A.ap[:, c:c + 1], ALU.mult, Bv.ap[:, c:c + 1], ALU.add,
                        reads=[self.psb[b], A.b, Bv.b], writes=[b32])
                continue
            ap, bb = dst(c)
            self.act(ap, src, AF.Identity, reads=[self.psb[b], A.b, Bv.b], writes=[bb],
                     scale=A.ap[:, c:c + 1], bias=Bv.ap[:, c:c + 1])

    def norm_bufs(self, banks=((0, 2)), nxs=2):
        nb = {}
        nb["junkr"] = Ring([self.tile(D, BF16, f"junk{i}") for i in range(2)])
        nb["junk"] = nb["junkr"].items[0]
        nb["ss"] = Ring([self.tile(1, F32, f"ss{i}") for i in range(8)])
        nb["rstd"] = Ring([self.tile(1, F32, f"rstd{i}") for i in range(8)])
        nb["xs"] = Ring([self.tile(D, F32, f"xs{i}") for i in range(nxs)])
        nb["bk"] = Ring(list(banks))
        return nb

    def phase_a0(self):
        I = self.I
        winb = self.tile(KC * 2 * RW, BF16, "a_w_in_b")
        self.dma("q2", winb.ap.rearrange("p (kc n) -> p kc n", kc=KC),
                 I["a_w_in"].rearrange("(kc p) n -> p kc n", p=128), writes=[winb.b])
        SKEW = 3
        xts = Ring([self.tile(D, F32, f"xt{i}") for i in range(SKEW + 1)])
        pets = Ring([self.tile(D, F32, f"pet{i}") for i in range(SKEW + 1)])
        nb = self.norm_bufs(banks=(0, 2), nxs=SKEW + 1)
        pend = []
        hx = [[[T(None, f"hx{s}_{c}_{t}") for t in range(4)] for c in range(KC)] for s in range(2)]
        hx_ap = [self.alloc(KC * 512, BF16, f"hxblk{s}") for s in range(2)]
        ust = Ring([self.tile(NJ * 512, F32, f"ust{i}") for i in range(2)])
        gst = Ring([self.tile(NJ * 512, F32, f"gst{i}") for i in range(2)])
        pbank = Ring([4, 5, 6, 7])
        segs = [("ctx", I["ctxb"], None, 0, CTX, self.A1c, self.B1c),
                ("oth", I["x_oth"], I["pe_oth"], CTX, HALF, self.A1[0], self.B1[0]),
                ("own", I["x_own"], I["pe_own"], CTX + HALF, HALF, self.A1[0], self.B1[0])]
        blk_i = 0
        for (sname, xsrc, pesrc, col0, ntok, A, Bv) in segs:
            nblk = (ntok + 511) // 512
            for blk in range(nblk):
                ncol = min(512, ntok - blk * 512)
                s = blk_i % 2
                blk_i += 1
                hap = hx_ap[s].rearrange("p (kc n) -> p kc n", kc=KC)
                for tt_ in range(ncol // 128):
                    t = blk * 4 + tt_
                    xt = xts.next()
                    self.dma("sp", xt.ap, xsrc[t * 128:(t + 1) * 128, :], writes=[xt.b])
                    if pesrc is not None:
                        pet = pets.next()
                        self.dma("sp", pet.ap, pesrc[t * 128:(t + 1) * 128, :], writes=[pet.b])
                        self.tt("pool", xt.ap, xt.ap, pet.ap, ALU.add, reads=[xt.b, pet.b], writes=[xt.b])
                    xs_ = self.norm_a(xt, nb)
                    pend.append(("norm", xs_, A, Bv,
                                 (lambda c, tt_=tt_, hap=hap, s=s: (hap[:, c, tt_ * 128:(tt_ + 1) * 128], hx[s][c][tt_].b)),
                                 (xt, t) if sname == "own" else None))
                    if tt_ == ncol // 128 - 1:
                        pend.append(("proj", sname, blk, ncol, s, hap, col0))
                    while len([p for p in pend if p[0] == "norm"]) > SKEW:
                        self._a0_drain(pend, nb, winb, hx, ust, gst, pbank)
        while pend:
            self._a0_drain(pend, nb, winb, hx, ust, gst, pbank)

    def _a0_drain(self, pend, nb, winb, hx, ust, gst, pbank):
        it = pend.pop(0)
        if it[0] == "norm":
            _, xs_, A, Bv, dst, st = it
            if st is not None:
                xt, t = st
                self.dma("sp", self.XS[t * 128:(t + 1) * 128, :], xt.ap, reads=[xt.b], writes=[self.XS_b[t]])
                if self.debug and self.stage == 1:
                    self.dma("sp", self.dbgX[t * 128:(t + 1) * 128, :], xt.ap, reads=[xt.b])
            self.norm_b(xs_, A, Bv, dst, nb)
            return
        _, sname, blk, ncol, s, hap, col0 = it
        if True:
            if True:
                nt = ncol // 128
                us = ust.next()
                usv = us.ap.rearrange("p (j n) -> p j n", j=NJ)
                for j in range(NJ):
                    bk = pbank.next()
                    for kc in range(KC):
                        self.mm(self.bank(bk)[:, 0:ncol], winb.ap[:, kc * 2 * RW + RW + j * 128: kc * 2 * RW + RW + (j + 1) * 128],
                                hap[:, kc, 0:ncol], kc == 0, kc == KC - 1,
                                reads=[winb.b] + [hx[s][kc][q].b for q in range(nt)], writes=[self.psb[bk]])
                    self.copy("dve", usv[:, j, 0:ncol], self.bank(bk)[:, 0:ncol], reads=[self.psb[bk]], writes=[us.b])
                key = (sname, blk)
                self.uT_b[key] = Buf(f"uT{key}")
                self.dma("sp", self.uT.rearrange("j p n -> p j n")[:, :, col0 + blk * 512: col0 + blk * 512 + ncol],
                         usv[:, :, 0:ncol], reads=[us.b], writes=[self.uT_b[key]])
                if sname == "own":
                    gs = gst.next()
                    gsv = gs.ap.rearrange("p (j n) -> p j n", j=NJ)
                    for j in range(NJ):
                        bk = pbank.next()
                        for kc in range(KC):
                            self.mm(self.bank(bk), winb.ap[:, kc * 2 * RW + j * 128: kc * 2 * RW + (j + 1) * 128],
                                    hap[:, kc, :], kc == 0, kc == KC - 1,
                                    reads=[winb.b] + [hx[s][kc][q].b for q in range(4)], writes=[self.psb[bk]])
                        self.act(gsv[:, j, :], self.bank(bk), AF.Gelu_apprx_tanh, reads=[self.psb[bk]], writes=[gs.b])
                    self.gg_b[blk] = Buf(f"gg{blk}")
                    self.dma("sp", self.gg.rearrange("j p n -> p j n")[:, :, blk * 512:(blk + 1) * 512], gsv,
                             reads=[gs.b], writes=[self.gg_b[blk]])

    def phase_b0(self):
        I = self.I
        asm = self.asm
        rwb = self.tile(2 * NH * 2 * 256, BF16, "rwb")
        iwb = self.tile(2 * NH * 2 * 256, BF16, "iwb")
        for (wt, src) in ((rwb, I["a_rw"]), (iwb, I["a_iw"])):
            for d in range(2):
                self.dma("q2", wt.ap[:, d * NH * 512:(d + 1) * NH * 512].rearrange("p (h kc n) -> p h kc n", h=NH, kc=2),
                         src[d].rearrange("h (kc p) n -> p h kc n", p=128), writes=[wt.b])

        def gw(wt, d, h, kc, co):
            base = ((d * NH + h) * 2 + kc) * 256 + co * 128
            return wt.ap[:, base:base + 128]
        Ust = [self.tile(HALF + 3, F32, "Ust0")]
        UCs = [self.tile(2 * HALF, F32, f"UC{i}") for i in range(2)]
        UCbs = [self.tile(2 * HALF, BF16, f"UCb{i}") for i in range(2)]
        UCc = self.tile(2 * CTX, F32, "UCc")
        UCcb = self.tile(2 * CTX, BF16, "UCcb")
        UCc_b = [[Buf(f"UCc_{c}")] for c in range(2)]
        UCcb_b = [[Buf(f"UCcb_{c}")] for c in range(2)]
        UC_b = [[[Buf(f"UC{p}_{c}_{b}") for b in range(8)] for c in range(2)] for p in range(2)]
        UCb_b = [[[Buf(f"UCb{p}_{c}_{b}") for b in range(8)] for c in range(2)] for p in range(2)]
        cbuf = []
        for co_ in range(2):
            cbuf.append({
                "er": Ring([self.tile(512, F32, f"er{co_}{i}") for i in range(1)]),
                "ei": Ring([self.tile(512, F32, f"ei{co_}{i}") for i in range(1)]),
                "a": Ring([self.tile(512, F32, f"a{co_}{i}") for i in range(1)]),
                "m": Ring([self.tile(512, F32, f"m{co_}{i}") for i in range(1)]),
                "yd": Ring([self.tile(512, F32, f"yd{co_}{i}") for i in range(2)]),
                "stmp": self.tile(1, F32, f"stmp{co_}"),
                "stmp2": self.tile(1, F32, f"stmp2{co_}"),
                "pairs": Ring([(2 * co_, 2 * co_ + 1)]),
            })
        Yf = [self.alloc(HALF, F32, f"Yf{c}") for c in range(2)]
        Yf_b = [[Buf(f"Yf{c}_{i}") for i in range(8)] for c in range(2)]
        ggt_r = Ring([self.tile(512, F32, f"ggt{i}") for i in range(1)])
        ygb_r = Ring([self.tile(512, BF16, f"ygb{i}") for i in range(1)])
        s_ctx = [[self.tile(1, F32, f"sctx{c}{d}") for d in range(2)] for c in range(2)]
        s_oth = [[self.tile(1, F32, f"soth{c}{d}") for d in range(2)] for c in range(2)]
        s_ini = [[self.tile(1, F32, f"sini{c}{d}") for d in range(2)] for c in range(2)]
        s_tmp = self.tile(1, F32, "stmp")
        pairs = Ring([(0, 1), (2, 3), (4, 5), (6, 7)])
        fl = self.flags
        own0 = CTX + HALF
        oth0 = CTX
        segs = [("ctx", 0, CTX), ("oth", CTX, HALF), ("own", CTX + HALF, HALF)]
        items = [(h, sg) for h in range(NH) for sg in segs]

        dg = self.tile(8 * 128, F32, "convdiag")
        cvb = Ring([4, 5, 6, 7])

        def bufsel(sname):
            if sname == "ctx":
                return (UCc.ap.rearrange("p (c n) -> p c n", c=2), UCcb.ap.rearrange("p (c n) -> p c n", c=2), UCc_b, UCcb_b)
            par = 0 if sname == "oth" else 1
            return (UCs[par].ap.rearrange("p (c n) -> p c n", c=2), UCbs[par].ap.rearrange("p (c n) -> p c n", c=2),
                    UC_b[par], UCb_b[par])

        def conv_gen(idx):
            h, (sname, col0, n) = items[idx]
            nblk = (n + 511) // 512
            UCv, UCbv, ucb_l, ucbb_l = bufsel(sname)
            if sname == "ctx":
                for cc in range(2):
                    for k in range(4):
                        jj = 2 * h + cc
                        self.ts("dve", dg.ap[:, (cc * 4 + k) * 128:(cc * 4 + k + 1) * 128], self.ident.ap,
                                asm.ap[:, jj * 4 + k: jj * 4 + k + 1], ALU.mult, reads=[self.ident.b, asm.b], writes=[dg.b])
            pending = None

            def evac(p):
                bk, ucv, ub, ucbv, ucbb, jj, ncol = p
                self.ts("dve", ucv, self.bank(bk)[:, 0:ncol], asm.ap[:, 40 + jj:41 + jj], ALU.add,
                        reads=[self.psb[bk], asm.b], writes=[ub])
                self.copy("pool", ucbv, ucv, reads=[ub], writes=[ucbb])

            for cc in range(2):
                j = 2 * h + cc
                U = Ust[0]
                srcb = [self.uT_b[(sname, b)] for b in range(nblk)]
                self.dma("sp", U.ap[:, 2:2 + n], self.uT[j][:, col0:col0 + n], reads=srcb, writes=[U.b])
                if sname != "ctx":
                    if sname == "oth":
                        lsrc, lfl, rsrc, rfl = own0 + HALF - 2, 0, own0, 1
                        lb, rb = self.uT_b[("own", 7)], self.uT_b[("own", 0)]
                    else:
                        lsrc, lfl, rsrc, rfl = oth0 + HALF - 2, 1, oth0, 0
                        lb, rb = self.uT_b[("oth", 7)], self.uT_b[("oth", 0)]
                    self.dma("sp", U.ap[:, 0:2], self.uT[j][:, lsrc:lsrc + 2], reads=[lb], writes=[U.b])
                    self.dma("sp", U.ap[:, n + 2:n + 3], self.uT[j][:, rsrc:rsrc + 1], reads=[rb], writes=[U.b], slow=True)
                if pending is not None:
                    evac(pending)
                    pending = None
                yield
                if sname == "ctx":
                    self.memset("dve", U.ap[:, 0:2], 0.0, writes=[U.b])
                    self.memset("dve", U.ap[:, n + 2:n + 3], 0.0, writes=[U.b])
                else:
                    self.ts("dve", U.ap[:, 0:2], U.ap[:, 0:2], fl.ap[:, lfl:lfl + 1], ALU.mult,
                            reads=[U.b, fl.b], writes=[U.b])
                    self.ts("dve", U.ap[:, n + 2:n + 3], U.ap[:, n + 2:n + 3], fl.ap[:, rfl:rfl + 1], ALU.mult,
                            reads=[U.b, fl.b], writes=[U.b])
                for blk in range(nblk):
                    ncol = min(512, n - blk * 512)
                    c0 = blk * 512
                    bk = cvb.next()
                    for k in range(4):
                        self.mm(self.bank(bk)[:, 0:ncol], dg.ap[:, (cc * 4 + k) * 128:(cc * 4 + k + 1) * 128],
                                U.ap[:, c0 + k:c0 + k + ncol], k == 0, k == 3, reads=[dg.b, U.b], writes=[self.psb[bk]])
                    if pending is not None:
                        evac(pending)
                    pending = (bk, UCv[:, cc, c0:c0 + ncol], ucb_l[cc][blk], UCbv[:, cc, c0:c0 + ncol],
                               ucbb_l[cc][blk], j, ncol)
                    yield
            if pending is not None:
                evac(pending)

        def run_all(g):
            for _ in g:
                pass

        import itertools
        run_all(conv_gen(0))
        run_all(conv_gen(1))
        fin_gen = iter(())
        for idx in range(len(items)):
            if True:
                h, (sname, col0, n) = items[idx]
                nblk = (n + 511) // 512
                UCv, UCbv, ucb_l, ucbb_l = bufsel(sname)
                if sname == "ctx":
                    cgen = iter(())
                elif sname == "oth":
                    cgen = conv_gen(idx + 1)
                else:
                    run_all(fin_gen)
                    cgen = itertools.chain(*[conv_gen(i) for i in (idx + 1, idx + 2) if i < len(items)])
                def chain(co, d, sname=sname, n=n, nblk=nblk, h=h, UCv=UCv, UCbv=UCbv, ucb_l=ucb_l, ucbb_l=ucbb_l):
                    j = 2 * h + co
                    bcol = d * NJ + j
                    cb = cbuf[co]
                    if sname == "ctx":
                        init = (0.0, None)
                    elif sname == "oth":
                        init = (s_ctx[co][d].ap, s_ctx[co][d].b)
                    else:
                        f = fl.ap[:, 1:2] if d == 0 else fl.ap[:, 0:1]
                        st_ = cb["stmp"]
                        self.tt("dve", st_.ap, s_oth[co][d].ap, s_ctx[co][d].ap, ALU.subtract,
                                reads=[s_oth[co][d].b, s_ctx[co][d].b], writes=[st_.b])
                        self.stt(s_ini[co][d].ap, st_.ap, f, s_ctx[co][d].ap, ALU.mult, ALU.add,
                                 reads=[st_.b, fl.b, s_ctx[co][d].b], writes=[s_ini[co][d].b])
                        init = (s_ini[co][d].ap, s_ini[co][d].b)
                    order = list(range(nblk)) if d == 0 else list(range(nblk - 1, -1, -1))
                    for bi, blk in enumerate(order):
                        ncol = min(512, n - blk * 512)
                        cols = slice(blk * 512, blk * 512 + ncol)
                        pr, pi = cb["pairs"].next()
                        for (pb, wt) in ((pr, rwb), (pi, iwb)):
                            for kc in range(2):
                                self.mm(self.bank(pb)[:, 0:ncol], gw(wt, d, h, kc, co), UCbv[:, kc, cols], kc == 0, kc == 1,
                                        reads=[wt.b, ucbb_l[0][blk], ucbb_l[1][blk]], writes=[self.psb[pb]])
                        yield
                        er, ei, a, m = cb["er"].next(), cb["ei"].next(), cb["a"].next(), cb["m"].next()
                        erv, eiv, av, mv = er.ap[:, 0:ncol], ei.ap[:, 0:ncol], a.ap[:, 0:ncol], m.ap[:, 0:ncol]
                        self.act(erv, self.bank(pr)[:, 0:ncol], AF.Exp, reads=[self.psb[pr], self.nrb.b], writes=[er.b],
                                 scale=-1.0, bias=self.nrb.ap[:, bcol:bcol + 1])
                        yield
                        self.act(erv, erv, AF.Ln, reads=[er.b], writes=[er.b], bias=1.0)
                        yield
                        self.act(erv, erv, AF.Exp, reads=[er.b], writes=[er.b], scale=-1.0)
                        yield
                        self.act(av, erv, AF.Exp, reads=[er.b, self.c8.b], writes=[a.b], scale=self.c8.ap[:, bcol:bcol + 1])
                        self.tt("pool", mv, av, av, ALU.mult, reads=[a.b], writes=[m.b])
                        yield
                        self.act(eiv, self.bank(pi)[:, 0:ncol], AF.Exp, reads=[self.psb[pi], self.nib.b], writes=[ei.b],
                                 scale=-1.0, bias=self.nib.ap[:, bcol:bcol + 1])
                        yield
                        self.act(eiv, eiv, AF.Ln, reads=[ei.b], writes=[ei.b], bias=1.0)
                        yield
                        first_ctx = (sname == "ctx" and bi == 0)
                        if first_ctx:
                            c0 = 0 if d == 0 else ncol - 1
                            ig0 = cb["stmp2"]
                            self.act(ig0.ap, ei.ap[:, c0:c0 + 1], AF.Exp, reads=[ei.b], writes=[ig0.b], scale=-1.0)
                        self.act(mv, mv, AF.Ln, reads=[m.b], writes=[m.b], scale=-1.0, bias=1.0)
                        yield
                        self.stt(eiv, mv, 0.5, eiv, ALU.mult, ALU.subtract, reads=[m.b, ei.b], writes=[ei.b])
                        self.act(eiv, eiv, AF.Exp, reads=[ei.b], writes=[ei.b])
                        yield
                        self.tt("dve", mv, eiv, UCv[:, co, cols], ALU.mult, reads=[ei.b, ucb_l[co][blk]], writes=[m.b])
                        if first_ctx:
                            self.tt("dve", m.ap[:, c0:c0 + 1], ig0.ap, UCv[:, co, blk * 512 + c0: blk * 512 + c0 + 1], ALU.mult,
                                    reads=[ig0.b, ucb_l[co][blk], m.b], writes=[m.b])
                        if sname == "own" and d == 0:
                            yap, yb = Yf[co][:, cols], Yf_b[co][blk]
                        else:
                            yd = cb["yd"].next()
                            yap, yb = yd.ap[:, 0:ncol], yd.b
                        ini_ap, ini_b = init
                        rd = [a.b, m.b] + ([ini_b] if ini_b is not None else [])
                        if d == 0:
                            self.P.op("dve", lambda e, o=yap, d0=av, d1=mv, ii=ini_ap: e.tensor_tensor_scan(
                                out=o, data0=d0, data1=d1, initial=ii, op0=ALU.mult, op1=ALU.add), rd, [yb])
                            init = (yap[:, ncol - 1:ncol], yb)
                        else:
                            self.P.op("dve", lambda e, o=yap, d0=av, d1=mv, ii=ini_ap: e.tensor_tensor_scan(
                                out=o[:, ::-1], data0=d0[:, ::-1], data1=d1[:, ::-1], initial=ii, op0=ALU.mult, op1=ALU.add),
                                rd, [yb])
                            init = (yap[:, 0:1], yb)
                        if sname == "own" and d == 1:
                            self.tt("pool", Yf[co][:, cols], Yf[co][:, cols], yap, ALU.add, reads=[Yf_b[co][blk], yb],
                                    writes=[Yf_b[co][blk]])
                        yield
                    if sname == "ctx":
                        self.copy("dve", s_ctx[co][d].ap, init[0], reads=[init[1]], writes=[s_ctx[co][d].b])
                    elif sname == "oth":
                        self.copy("dve", s_oth[co][d].ap, init[0], reads=[init[1]], writes=[s_oth[co][d].b])

                stepn = 0
                for d in range(2):
                    gens = [chain(0, d), chain(1, d)]
                    while gens:
                        for g_ in list(gens):
                            try:
                                next(g_)
                            except StopIteration:
                                gens.remove(g_)
                            stepn += 1
                            if stepn % 14 == 0:
                                next(cgen, None)
                            if stepn % 9 == 0:
                                next(fin_gen, None)
                run_all(cgen)
                if sname == "own":
                    def finalize_gen(h=h):
                        prev = None
                        for co in range(2):
                            j = 2 * h + co
                            for blk in range(8):
                                cols = slice(blk * 512, (blk + 1) * 512)
                                if prev is not None:
                                    pj, pcols, pco, pblk, pggt = prev
                                    ygb = ygb_r.next()
                                    self.tt("pool", ygb.ap, pggt.ap, Yf[pco][:, pcols], ALU.mult, reads=[pggt.b, Yf_b[pco][pblk]], writes=[ygb.b])
                                    key = (pj, pblk)
                                    self.yg_b[key] = Buf(f"yg{key}")
                                    self.dma("sp", self.ygT[pj][:, pcols], ygb.ap, reads=[ygb.b], writes=[self.yg_b[key]])
                                ggt = ggt_r.next()
                                self.dma("sp", ggt.ap, self.gg[j][:, cols], reads=[self.gg_b[blk]], writes=[ggt.b])
                                prev = (j, cols, co, blk, ggt)
                                yield
                        pj, pcols, pco, pblk, pggt = prev
                        ygb = ygb_r.next()
                        self.tt("pool", ygb.ap, pggt.ap, Yf[pco][:, pcols], ALU.mult, reads=[pggt.b, Yf_b[pco][pblk]], writes=[ygb.b])
                        key = (pj, pblk)
                        self.yg_b[key] = Buf(f"yg{key}")
                        self.dma("sp", self.ygT[pj][:, pcols], ygb.ap, reads=[ygb.b], writes=[self.yg_b[key]])
                    fin_gen = finalize_gen()
        run_all(fin_gen)

    def phase_m(self, L, hh):
        I = self.I
        NT = 16
        xacc = self.alloc(NT * D, F32, "xacc")
        xa = [T(xacc[:, t * D:(t + 1) * D], f"xacc{t}") for t in range(NT)]
        hxT = self.alloc(KC * 2048, BF16, "hxT").rearrange("p (kc n) -> p kc n", kc=KC)
        hx_b = [[Buf(f"hx{kc}_{t}") for t in range(NT)] for kc in range(KC)]
        gates = self.alloc(NT * NE, F32, "gates")
        gate_b = [Buf(f"gate{t}") for t in range(NT)]
        G2 = self.tile(D, F32, "G2bc")
        self.dma("sp", G2.ap, self.GBC[2 * L + 1], reads=[self.GBC_b[2 * L + 1]], writes=[G2.b])
        rw = self.tile(KC * NE, F32, "rw")
        self.dma("sp", rw.ap.rearrange("p (kc n) -> p kc n", kc=KC), I["router_w"][L].rearrange("(kc p) n -> p kc n", p=128),
                 writes=[rw.b])
        ot_r = Ring([self.tile(D, F32, f"ot{i}") for i in range(2)])
        ep_junk = self.tile(D, BF16, "ep_junk")
        ep_ss = Ring([self.tile(1, F32, f"ep_ss{i}") for i in range(4)])
        ep_rstd = Ring([self.tile(1, F32, f"ep_rstd{i}") for i in range(4)])
        mark = self.top
        MSKEW = 2
        nb = self.norm_bufs(banks=(0, 2), nxs=MSKEW + 1)
        pendn = []
        hx32_r = Ring([self.tile(KC * 128, F32, f"hx32_{i}") for i in range(2)])
        hx32_b = [[Buf(f"hx32_{i}_{c}") for c in range(KC)] for i in range(2)]
        sm = {n: self.tile(w, F32, n) for n, w in [("th", 64), ("sc", 64), ("sel", 64), ("mx", 64), ("gs", 8), ("g8", 8),
                                                    ("pen", 8), ("selm", 64), ("t8", 8), ("mask", 64), ("den", 1)]}
        obanks = Ring([(4, 5), (6, 7)])
        skipP = 'prologue' in os.environ.get('MSKIP', '')
        if L == 0 and not skipP:
            G1 = self.tile(D, F32, "G1bc")
            self.dma("sp", G1.ap, self.GBC[0], reads=[self.GBC_b[0]], writes=[G1.b])
            woutb = self.tile(NJ * D, BF16, "woutb")
            wst = Ring([self.tile(D, F32, f"wst{i}") for i in range(2)])
            for jc in range(NJ):
                st = wst.next()
                self.dma("sp", st.ap, I["a_w_out"][jc * 128:(jc + 1) * 128, :], writes=[st.b])
                self.tt("pool", woutb.ap[:, jc * D:(jc + 1) * D], st.ap, G1.ap, ALU.mult, reads=[st.b, G1.b], writes=[woutb.b])
            ygt_r = Ring([self.tile(NJ * 128, BF16, f"ygt{i}") for i in range(2)])
        stage_b = lambda t_, xs_: self._m_router(L, t_, xs_, nb, hxT, hx_b, hx32_r, hx32_b, rw, sm, obanks, gates, gate_b)
        for t in range(NT):
            tg = hh * NT + t
            self.dma("sp", xa[t].ap, self.XS[tg * 128:(tg + 1) * 128, :], reads=[self.XS_b[tg]], writes=[xa[t].b])
            if L == 0 and not skipP:
                ygt = ygt_r.next()
                self.dma("sp", ygt.ap.rearrange("p (j n) -> p j n", j=NJ),
                         self.ygT.rearrange("j p n -> p j n")[:, :, tg * 128:(tg + 1) * 128],
                         reads=[self.yg_b[(j, tg // 4)] for j in range(NJ)], writes=[ygt.b])
                ob = obanks.next()
                for n in range(2):
                    for jc in range(NJ):
                        self.mm(self.bank(ob[n]), ygt.ap[:, jc * 128:(jc + 1) * 128],
                                woutb.ap[:, jc * D + n * 512: jc * D + (n + 1) * 512], jc == 0, jc == NJ - 1,
                                reads=[ygt.b, woutb.b], writes=[self.psb[ob[n]]])
                self.tt("dve", xa[t].ap, self.bank(ob[0], 2), xa[t].ap, ALU.add,
                        reads=[self.psb[ob[0]], self.psb[ob[1]], xa[t].b], writes=[xa[t].b])
            xs_ = self.norm_a(xa[t], nb)
            pendn.append((t, xs_))
            if len(pendn) > MSKEW:
                stage_b(*pendn.pop(0))
        while pendn:
            stage_b(*pendn.pop(0))
        self.P.barrier()
        self.top = mark
        self._m_experts(L, hh, xa, hxT, hx_b, gates, gate_b, G2, obanks, ot_r, ep_junk, ep_ss, ep_rstd)

    def _m_router(self, L, t, xs_, nb, hxT, hx_b, hx32_r, hx32_b, rw, sm, obanks, gates, gate_b):
        if True:
            si = t % 2
            h32 = hx32_r.next()
            h32v = h32.ap.rearrange("p (kc n) -> p kc n", kc=KC)
            self.norm_b(xs_, self.A2[L], self.B2[L],
                        lambda c, t=t: (hxT[:, c, t * 128:(t + 1) * 128], hx_b[c][t]), nb,
                        dst32=lambda c, h32v=h32v, si=si: (h32v[:, c, :], hx32_b[si][c]))
            self.copy("act", hxT[:, :, t * 128:(t + 1) * 128], h32v, reads=[hx32_b[si][c] for c in range(KC)],
                      writes=[hx_b[c][t] for c in range(KC)])
            ob = obanks.next()
            lg = self.bank(ob[0])[:, 0:NE]
            for kc in range(KC):
                self.mm(lg, h32v[:, kc, :], rw.ap[:, kc * NE:(kc + 1) * NE], kc == 0, kc == KC - 1,
                        reads=[hx32_b[si][kc], rw.b], writes=[self.psb[ob[0]]])
            th, sc, sel, mx, gs, g8, pen, selm, t8, mask, den = [sm[k] for k in
                                                                 ("th", "sc", "sel", "mx", "gs", "g8", "pen", "selm", "t8", "mask", "den")]
            self.act(th.ap, lg, AF.Tanh, reads=[self.psb[ob[0]]], writes=[th.b], scale=0.5)
            self.ts("dve", sc.ap, th.ap, 0.5, ALU.mult, 0.5, ALU.add, reads=[th.b], writes=[sc.b])
            self.tt("dve", sel.ap, sc.ap, self.rb_bc.ap[:, L * NE:(L + 1) * NE], ALU.add, reads=[sc.b, self.rb_bc.b], writes=[sel.b])
            for g in range(8):
                self.P.op("dve", lambda e, o=mx.ap[:, g * 8:(g + 1) * 8], i_=sel.ap[:, g * 8:(g + 1) * 8]: e.max(out=o, in_=i_),
                          [sel.b], [mx.b])
            mxv = mx.ap.rearrange("p (g k) -> p g k", k=8)
            self.tt("dve", gs.ap, mxv[:, :, 0], mxv[:, :, 1], ALU.add, reads=[mx.b], writes=[gs.b])
            self.P.op("dve", lambda e, o=g8.ap, i_=gs.ap: e.max(out=o, in_=i_), [gs.b], [g8.b])
            self.ts("dve", pen.ap, gs.ap, g8.ap[:, 3:4], ALU.is_ge, reads=[gs.b, g8.b], writes=[pen.b])
            self.ts("dve", pen.ap, pen.ap, -1.0, ALU.add, 1e30, ALU.mult, reads=[pen.b], writes=[pen.b])
            for g in range(8):
                self.ts("dve", selm.ap[:, g * 8:(g + 1) * 8], sel.ap[:, g * 8:(g + 1) * 8], pen.ap[:, g:g + 1], ALU.add,
                        reads=[sel.b, pen.b], writes=[selm.b])
            self.P.op("dve", lambda e, o=t8.ap, i_=selm.ap: e.max(out=o, in_=i_), [selm.b], [t8.b])
            self.ts("dve", mask.ap, selm.ap, t8.ap[:, 7:8], ALU.is_ge, reads=[selm.b, t8.b], writes=[mask.b])
            self.tt("dve", mask.ap, mask.ap, sc.ap, ALU.mult, reads=[mask.b, sc.b], writes=[mask.b])
            self.P.op("dve", lambda e, o=den.ap, i_=mask.ap: e.tensor_reduce(out=o, in_=i_, axis=AX.X, op=ALU.add),
                      [mask.b], [den.b])
            self.P.op("dve", lambda e, o=den.ap: e.reciprocal(out=o, in_=o), [den.b], [den.b])
            self.ts("dve", gates[:, t * NE:(t + 1) * NE], mask.ap, den.ap, ALU.mult, 2.5, ALU.mult,
                    reads=[mask.b, den.b], writes=[gate_b[t]])

    def _m_experts(self, L, hh, xa, hxT, hx_b, gates, gate_b, G2, obanks, ot_r, ep_junk, ep_ss, ep_rstd):
        I = self.I
        NT = 16
        wgu_r = Ring([self.tile(KC * 512, BF16, f"wgu{i}") for i in range(2)])
        wdf_r = Ring([self.tile(2 * D, F32, f"wdf{i}") for i in range(2)])
        wdb_r = Ring([self.tile(2 * D, BF16, f"wdb{i}") for i in range(2)])
        s_r = Ring([self.tile(512, F32, f"s{i}") for i in range(2)])
        actT_r = Ring([[self.tile(512, BF16, f"actT{i}_{f}") for f in range(2)] for i in range(3)])
        gubanks = Ring([(0, 1), (2, 3)])
        elist = list(range(NE + 1)) if 'NE_DBG' not in os.environ else list(range(int(os.environ['NE_DBG']))) + [NE]
        if 'expert' in os.environ.get('MSKIP', ''):
            elist = []
        pend_down = None
        for e in elist:
            wgu, wdf, wdb = wgu_r.next(), wdf_r.next(), wdb_r.next()
            src_gu = I["moe_w_gu"][L, e] if e < NE else I["sh_w_gu"][L]
            src_dn = I["moe_w_down"][L, e] if e < NE else I["sh_w_down"][L]
            wguv = wgu.ap.rearrange("p (kc n) -> p kc n", kc=KC)
            self.dma("q2", wguv, src_gu.rearrange("(kc p) n -> p kc n", p=128), writes=[wgu.b])
            self.dma("sp", wdf.ap.rearrange("p (f n) -> p f n", f=2), src_dn.rearrange("(f p) n -> p f n", p=128), writes=[wdf.b])
            for fh in range(2):
                self.tt("pool", wdb.ap[:, fh * D:(fh + 1) * D], wdf.ap[:, fh * D:(fh + 1) * D], G2.ap, ALU.mult,
                        reads=[wdf.b, G2.b], writes=[wdb.b])
            for blk in range(4):
                cols = slice(blk * 512, (blk + 1) * 512)
                actT = actT_r.next()
                for fh in range(2):
                    bg, bu = gubanks.next()
                    rd = [wgu.b]
                    for kc in range(KC):
                        rdk = rd + [hx_b[kc][blk * 4 + q] for q in range(4)]
                        self.mm(self.bank(bg), wguv[:, kc, fh * 128:(fh + 1) * 128], hxT[:, kc, cols], kc == 0, kc == KC - 1,
                                reads=rdk, writes=[self.psb[bg]])
                    for kc in range(KC):
                        rdk = rd + [hx_b[kc][blk * 4 + q] for q in range(4)]
                        self.mm(self.bank(bu), wguv[:, kc, 256 + fh * 128:256 + (fh + 1) * 128], hxT[:, kc, cols], kc == 0,
                                kc == KC - 1, reads=rdk, writes=[self.psb[bu]])
                    s = s_r.next()
                    self.act(s.ap, self.bank(bg), AF.Silu, reads=[self.psb[bg]], writes=[s.b])
                    self.tt("dve", actT[fh].ap, self.bank(bu), s.ap, ALU.mult, reads=[self.psb[bu], s.b], writes=[actT[fh].b])
                    if pend_down is not None:
                        pend_down(range(2 * fh, 2 * fh + 2))
                def down(qs, blk=blk, actT=actT, wdb=wdb, e=e):
                    for q in qs:
                        t = blk * 4 + q
                        ob = obanks.next()
                        for n in range(2):
                            for fh in range(2):
                                self.mm(self.bank(ob[n]), actT[fh].ap[:, q * 128:(q + 1) * 128],
                                        wdb.ap[:, fh * D + n * 512: fh * D + (n + 1) * 512], fh == 0, fh == 1,
                                        reads=[actT[fh].b, wdb.b], writes=[self.psb[ob[n]]])
                        gsc = gates[:, t * NE + e: t * NE + e + 1] if e < NE else 1.0
                        self.stt(xa[t].ap, self.bank(ob[0], 2), gsc, xa[t].ap, ALU.mult, ALU.add,
                                 reads=[self.psb[ob[0]], self.psb[ob[1]], xa[t].b] + ([gate_b[t]] if e < NE else []),
                                 writes=[xa[t].b])
                pend_down = down
        if pend_down is not None:
            pend_down(range(4))
        for t in range(NT):
            tg = hh * NT + t
            if L == 0:
                self.dma("sp", self.XS[tg * 128:(tg + 1) * 128, :], xa[t].ap, reads=[xa[t].b], writes=[self.XS_b[tg]])
                if self.debug and self.stage == 3:
                    self.dma("sp", self.dbgX[tg * 128:(tg + 1) * 128, :], xa[t].ap, reads=[xa[t].b])
            else:
                ss, rstd = ep_ss.next(), ep_rstd.next()
                self.act(ep_junk.ap, xa[t].ap, AF.Square, reads=[xa[t].b], writes=[ss.b, ep_junk.b], accum_out=ss.ap)
                self.ts("pool", rstd.ap, ss.ap, 1.0 / D, ALU.mult, EPS, ALU.add, reads=[ss.b], writes=[rstd.b])
                self.tt("pool", rstd.ap, rstd.ap, self.neghalf.ap[:, 0:1], ALU.pow, reads=[rstd.b, self.neghalf.b], writes=[rstd.b])
                ot = ot_r.next()
                self.stt(ot.ap, xa[t].ap, rstd.ap, self.fin_bc.ap, ALU.mult, ALU.mult,
                         reads=[xa[t].b, rstd.b, self.fin_bc.b], writes=[ot.b])
                self.outs.append(self.dma("sp", self.out[tg * 128:(tg + 1) * 128, :], ot.ap, reads=[ot.b]))

    def phase_s1(self):
        I = self.I
        NF = 16
        winu = self.tile(KC * 2048, BF16, "winu")
        winv = self.tile(KC * 2048, BF16, "winv")
        winuv = winu.ap.rearrange("p (kc n) -> p kc n", kc=KC)
        winvv = winv.ap.rearrange("p (kc n) -> p kc n", kc=KC)
        src = I["b_w_in"].rearrange("(kc p) n -> p kc n", p=128)
        self.dma("q2", winuv, src[:, :, 0:2048], writes=[winu.b])
        self.dma("q2", winvv, src[:, :, 2048:4096], writes=[winv.b])
        woutb = self.tile(NF * D, BF16, "s_woutb")
        wsTb = self.tile(8 * 128, BF16, "wsTb")
        Cm = self.tile(NF * 128, F32, "Cmat")
        Gbc = self.tile(2048, BF16, "s_Gbc")
        lnG = self.lng_fm.ap[:, 0:16]
        lnB = self.lng_fm.ap[:, 16:32]
        mark = self.top
        G1 = self.tile(D, F32, "s_G1")
        self.dma("sp", G1.ap, self.GBC[2], reads=[self.GBC_b[2]], writes=[G1.b])
        wst = Ring([self.tile(D, F32, f"s_wst{i}") for i in range(2)])
        for fc in range(NF):
            st = wst.next()
            self.dma("sp", st.ap, I["b_w_out"][fc * 128:(fc + 1) * 128, :], writes=[st.b])
            self.tt("pool", woutb.ap[:, fc * D:(fc + 1) * D], st.ap, G1.ap, ALU.mult, reads=[st.b, G1.b], writes=[woutb.b])
        ws32 = self.tile(8 * 128, F32, "ws32")
        self.dma("sp", ws32.ap.rearrange("p (g q) -> p g q", g=8), I["b_w_s"].rearrange("g p q -> p g q"), writes=[ws32.b])
        wsT32 = self.tile(8 * 128, F32, "wsT32")
        rs_bc = self.tile(8 * 128, F32, "rs_bc")
        bs_bc = self.tile(8 * 128, F32, "bs_bc")
        bsrow = self.tile(D, F32, "bsrow")
        self.dma("sp", bsrow.ap[0:1, :], I["b_bs_row"], writes=[bsrow.b])
        for hb in range(2):
            for g4 in range(4):
                g = hb * 4 + g4
                self.tr(self.bank(hb)[:, g4 * 128:(g4 + 1) * 128], ws32.ap[:, g * 128:(g + 1) * 128], self.ident.ap,
                        reads=[ws32.b, self.ident.b], writes=[self.psb[hb]])
            self.copy("dve", wsT32.ap[:, hb * 512:(hb + 1) * 512], self.bank(hb), reads=[self.psb[hb]], writes=[wsT32.b])
        self.copy("pool", wsTb.ap, wsT32.ap, reads=[wsT32.b], writes=[wsTb.b])
        for hb in range(2):
            self.mm(self.bank(2 + hb), self.ones.ap, wsT32.ap[:, hb * 512:(hb + 1) * 512], True, True,
                    reads=[self.ones.b, wsT32.b], writes=[self.psb[2 + hb]])
            self.copy("dve", rs_bc.ap[:, hb * 512:(hb + 1) * 512], self.bank(2 + hb), reads=[self.psb[2 + hb]], writes=[rs_bc.b])
            self.mm(self.bank(4 + hb), self.ones.ap[0:1, :], bsrow.ap[0:1, hb * 512:(hb + 1) * 512], True, True,
                    reads=[self.ones.b, bsrow.b], writes=[self.psb[4 + hb]])
            self.copy("dve", bs_bc.ap[:, hb * 512:(hb + 1) * 512], self.bank(4 + hb), reads=[self.psb[4 + hb]], writes=[bs_bc.b])
        lnrow = self.tile(2048, F32, "lnrow")
        self.dma("sp", lnrow.ap[0:1, :], I["b_ln_row"][:, 0:2048], writes=[lnrow.b])
        for n4 in range(4):
            bk = 6 + n4 % 2
            self.mm(self.bank(bk), self.ones.ap[0:1, :], lnrow.ap[0:1, n4 * 512:(n4 + 1) * 512], True, True,
                    reads=[self.ones.b, lnrow.b], writes=[self.psb[bk]])
            self.copy("dve", Gbc.ap[:, n4 * 512:(n4 + 1) * 512], self.bank(bk), reads=[self.psb[bk]], writes=[Gbc.b])
        for fc in range(NF):
            g = fc // 2
            self.stt(Cm.ap[:, fc * 128:(fc + 1) * 128], rs_bc.ap[:, g * 128:(g + 1) * 128], lnB[:, fc:fc + 1],
                     bs_bc.ap[:, g * 128:(g + 1) * 128], ALU.mult, ALU.add,
                     reads=[rs_bc.b, bs_bc.b, self.lng_fm.b], writes=[Cm.b])
        self.P.barrier()
        self.top = mark
        SK = os.environ.get('SSKIP', '')
        if 'main' in SK:
            return
        xts = Ring([self.tile(D, F32, f"s_xt{i}") for i in range(2)])
        nb = self.norm_bufs(banks=(0,), nxs=1)
        hx_ap = [self.alloc(KC * 512, BF16, f"s_hx{i}").rearrange("p (kc n) -> p kc n", kc=KC) for i in range(2)]
        hx_b = [[[Buf(f"shx{i}_{c}_{q}") for q in range(4)] for c in range(KC)] for i in range(2)]
        uT = self.tile(NF * 512, BF16, "s_uT")
        uTv = uT.ap.rearrange("p (f n) -> p f n", f=NF)
        uT_b = [Buf(f"s_uT{f}") for f in range(NF)]
        v = self.tile(2048, F32, "s_v")
        v_b = [Buf(f"s_v{i}") for i in range(4)]
        vn_r = Ring([self.tile(2048, BF16, f"s_vn{i}") for i in range(2)])
        th_r = Ring([self.tile(1024, F32, f"s_th{i}") for i in range(1)])
        vn0 = self.tile(2048, BF16, "s_vn0")
        zT_r = Ring([self.tile(NF * 128, BF16, f"s_zT{i}") for i in range(2)])
        st_ = {n: self.tile(w, F32, "s_" + n) for n, w in [("s1", 4), ("s2", 2), ("mean", 1), ("ex2", 1), ("msq", 1),
                                                            ("rstd", 1), ("nmr", 1), ("t2", 2)]}
        uvb = Ring([2, 3])
        xres_r = Ring([self.tile(D, F32, f"s_xres{i}") for i in range(2)])

        def stage_n(blk):
            si = blk % 2
            hap = hx_ap[si]
            for q in range(4):
                tg = blk * 4 + q
                xt = xts.next()
                self.dma("sp", xt.ap, self.XS[tg * 128:(tg + 1) * 128, :], reads=[self.XS_b[tg]], writes=[xt.b])
                self.norm_tile(xt, self.A1[1], self.B1[1],
                               lambda c, q=q, hap=hap, si=si: (hap[:, c, q * 128:(q + 1) * 128], hx_b[si][c][q]), nb)

        def stage_u(blk):
            si = blk % 2
            hap = hx_ap[si]
            for fc in range(NF):
                bk = uvb.next()
                for kc in range(KC):
                    self.mm(self.bank(bk), winuv[:, kc, fc * 128:(fc + 1) * 128], hap[:, kc, :], kc == 0, kc == KC - 1,
                            reads=[winu.b] + [hx_b[si][kc][q] for q in range(4)], writes=[self.psb[bk]])
                self.act(uTv[:, fc, :], self.bank(bk), AF.Gelu_apprx_tanh, reads=[self.psb[bk]], writes=[uT_b[fc]])

        def stage_a(blk, q):
            si = blk % 2
            hap = hx_ap[si]
            s1, s2 = st_["s1"], st_["s2"]
            for cg in range(4):
                bk = uvb.next()
                for kc in range(KC):
                    self.mm(self.bank(bk), hap[:, kc, q * 128:(q + 1) * 128], winvv[:, kc, cg * 512:(cg + 1) * 512],
                            kc == 0, kc == KC - 1, reads=[winv.b, hx_b[si][kc][q]], writes=[self.psb[bk]])
                self.act(v.ap[:, cg * 512:(cg + 1) * 512], self.bank(bk), AF.Gelu_apprx_tanh,
                         reads=[self.psb[bk]], writes=[v_b[cg], s1.b], accum_out=s1.ap[:, cg:cg + 1])
            for hv in range(2):
                jk = nb["junkr"].next()
                self.act(jk.ap, v.ap[:, hv * 1024:(hv + 1) * 1024], AF.Square, reads=[v_b[2 * hv], v_b[2 * hv + 1]],
                         writes=[s2.b, jk.b], accum_out=s2.ap[:, hv:hv + 1])
            mean, ex2, msq, rstd, nmr, t2 = [st_[k] for k in ("mean", "ex2", "msq", "rstd", "nmr", "t2")]
            self.tt("pool", t2.ap, s1.ap[:, 0:2], s1.ap[:, 2:4], ALU.add, reads=[s1.b], writes=[t2.b])
            self.tt("pool", mean.ap, t2.ap[:, 0:1], t2.ap[:, 1:2], ALU.add, reads=[t2.b], writes=[mean.b])
            self.tt("pool", ex2.ap, s2.ap[:, 0:1], s2.ap[:, 1:2], ALU.add, reads=[s2.b], writes=[ex2.b])
            self.ts("pool", msq.ap, mean.ap, mean.ap, ALU.mult, 1.0 / (2048.0 * 2048.0), ALU.mult, reads=[mean.b], writes=[msq.b])
            self.ts("pool", ex2.ap, ex2.ap, 1.0 / 2048, ALU.mult, EPS, ALU.add, reads=[ex2.b], writes=[ex2.b])
            self.tt("pool", rstd.ap, ex2.ap, msq.ap, ALU.subtract, reads=[ex2.b, msq.b], writes=[rstd.b])
            self.tt("pool", rstd.ap, rstd.ap, self.neghalf.ap[:, 0:1], ALU.pow, reads=[rstd.b, self.neghalf.b], writes=[rstd.b])
            self.ts("pool", nmr.ap, mean.ap, rstd.ap, ALU.mult, -1.0 / 2048, ALU.mult, reads=[mean.b, rstd.b], writes=[nmr.b])
            vn = vn_r.next()
            self.ts("dve", vn0.ap, v.ap, rstd.ap, ALU.mult, nmr.ap, ALU.add, reads=v_b + [rstd.b, nmr.b], writes=[vn0.b])
            self.tt("dve", vn.ap, vn0.ap, Gbc.ap, ALU.mult, reads=[vn0.b, Gbc.b], writes=[vn.b])
            return vn

        def stage_s(blk, q, vn):
            for fc in range(NF):
                bk = 4 + fc // 4
                self.mm(self.bank(bk)[:, (fc % 4) * 128:(fc % 4 + 1) * 128], vn.ap[:, fc * 128:(fc + 1) * 128],
                        wsTb.ap[:, (fc // 2) * 128:(fc // 2 + 1) * 128], True, True,
                        reads=[vn.b, wsTb.b], writes=[self.psb[bk]])
            zT = zT_r.next()
            for hf in range(2):
                th = th_r.next()
                self.tt("dve", th.ap, self.bank(4 + 2 * hf, 2), Cm.ap[:, hf * 1024:(hf + 1) * 1024], ALU.add,
                        reads=[self.psb[4 + 2 * hf], self.psb[5 + 2 * hf], Cm.b], writes=[th.b])
                self.tt("dve", zT.ap[:, hf * 1024:(hf + 1) * 1024].rearrange("p (f n) -> p f n", f=8),
                        th.ap.rearrange("p (f n) -> p f n", f=8), uTv[:, hf * 8:(hf + 1) * 8, q * 128:(q + 1) * 128], ALU.mult,
                        reads=[th.b] + uT_b[hf * 8:(hf + 1) * 8], writes=[zT.b])
            return zT

        def stage_o(blk, q, zT):
            tg = blk * 4 + q
            xr = xres_r.next()
            self.dma("sp", xr.ap, self.XS[tg * 128:(tg + 1) * 128, :], reads=[self.XS_b[tg]], writes=[xr.b])
            for n in range(2):
                for fc in range(NF):
                    self.mm(self.bank(2 + n), zT.ap[:, fc * 128:(fc + 1) * 128], woutb.ap[:, fc * D + n * 512: fc * D + (n + 1) * 512],
                            fc == 0, fc == NF - 1, reads=[zT.b, woutb.b], writes=[self.psb[2 + n]])
            self.tt("dve", xr.ap, self.bank(2, 2), xr.ap, ALU.add, reads=[self.psb[2], self.psb[3], xr.b], writes=[xr.b])
            self.dma("sp", self.XS[tg * 128:(tg + 1) * 128, :], xr.ap, reads=[xr.b], writes=[self.XS_b[tg]])
            if self.debug and self.stage == 4:
                self.dma("sp", self.dbgX[tg * 128:(tg + 1) * 128, :], xr.ap, reads=[xr.b])

        pendS, pendO = [], []

        def step_s():
            if pendS:
                b_, q_, vn_ = pendS.pop(0)
                pendO.append((b_, q_, stage_s(b_, q_, vn_)))

        def step_o():
            if pendO:
                stage_o(*pendO.pop(0))

        stage_n(0)
        for blk in range(8):
            while pendS:
                step_o()
                step_s()
            stage_u(blk)
            for q in range(4):
                vn = stage_a(blk, q)
                step_o()
                step_s()
                pendS.append((blk, q, vn))
                if q == 1 and blk + 1 < 8:
                    stage_n(blk + 1)
        while pendS or pendO:
            step_o()
            step_s()


def _fm(v, nchunk):
    return np.ascontiguousarray(np.asarray(v, np.float32).reshape(nchunk, 128).T)


def _sincos_table():
    quarter = D // 4
    omega = (1.0 / (np.float32(10000.0) ** (np.arange(quarter, dtype=np.float32) / np.float32(quarter)))).astype(np.float32)

    def emb(n):
        p = np.arange(n, dtype=np.float32)[:, None] * omega[None, :]
        return np.concatenate([np.sin(p), np.cos(p)], axis=-1).astype(np.float32)
    rows = SEQ // 64
    er, ec = emb(rows), emb(64)
    pe = np.concatenate([np.broadcast_to(er[:, None, :], (rows, 64, D // 2)),
                         np.broadcast_to(ec[None, :, :], (rows, 64, D // 2))], axis=-1)
    return np.ascontiguousarray(pe.reshape(SEQ, D).astype(np.float32))


def make_in_maps(inp):
    f = lambda a: np.ascontiguousarray(np.asarray(a, np.float32))
    pe = _sincos_table()
    shared = {}
    shared["ident"] = np.eye(128, dtype=np.float32)
    shared["ada_w"] = f(inp["ada_w"])
    ab = f(inp["ada_b"])
    abf = np.stack([_fm(ab[i], 48) for i in range(2)], 0)
    shared["adab_fm"] = np.ascontiguousarray(np.repeat(abf.transpose(1, 0, 2)[:, :, :, None], 2, axis=3).reshape(128, 192))
    shared["adab_row"] = f(ab.reshape(1, -1))
    shared["ng"] = np.ascontiguousarray(np.concatenate(
        [_fm(inp["mix_norm_g"][0], 8), _fm(inp["ffn_norm_g"][0], 8), _fm(inp["mix_norm_g"][1], 8), _fm(inp["ffn_norm_g"][1], 8)], axis=1))
    shared["fin_row"] = f(inp["final_norm_g"]).reshape(1, D)
    shared["a_w_in"] = f(inp["a_w_in"][0])
    cw = f(inp["a_conv_w"][0])
    cwf = np.stack([_fm(cw[k], 10) for k in range(4)], axis=2).reshape(128, 40)
    parts = [cwf, _fm(inp["a_conv_b"][0], 10)]
    for nm in ("a_gate_r_b", "a_gate_i_b", "a_lambda"):
        v = f(inp[nm][0])
        parts.append(np.concatenate([_fm(v[0], 10), _fm(v[1], 10)], axis=1))
    shared["a_small"] = np.ascontiguousarray(np.concatenate(parts, axis=1))
    shared["a_rw"] = f(inp["a_gate_r_w"][0])
    shared["a_iw"] = f(inp["a_gate_i_w"][0])
    shared["a_w_out"] = f(inp["a_w_out"][0])
    shared["b_w_in"] = f(inp["b_w_in"][0])
    shared["b_ln_row"] = np.ascontiguousarray(np.concatenate([f(inp["b_ln_g"][0]), f(inp["b_ln_b"][0])]).reshape(1, 4096))
    shared["b_lng_fm"] = np.ascontiguousarray(np.concatenate([_fm(inp["b_ln_g"][0], 16), _fm(inp["b_ln_b"][0], 16)], axis=1))
    shared["b_w_s"] = f(inp["b_w_s"][0])
    shared["b_bs_row"] = f(inp["b_b_s"][0]).reshape(1, 1024)
    shared["b_w_out"] = f(inp["b_w_out"][0])
    shared["router_w"] = f(inp["router_w"])
    shared["rb_row"] = f(inp["router_b"]).reshape(1, 2 * NE)
    shared["moe_w_gu"] = f(inp["moe_w_gu"])
    shared["moe_w_down"] = f(inp["moe_w_down"])
    shared["sh_w_gu"] = f(inp["shared_w_gu"])
    shared["sh_w_down"] = f(inp["shared_w_down"])
    x = f(inp["x"])
    ctx = f(inp["ctx"])
    c = f(inp["c"])
    cc = f(inp["c_ctx"])
    maps = []
    for k in range(8):
        b, h = k // 2, k % 2
        m = dict(shared)
        m["x_own"] = np.ascontiguousarray(x[b, h * HALF:(h + 1) * HALF])
        m["x_oth"] = np.ascontiguousarray(x[b, (1 - h) * HALF:(2 - h) * HALF])
        m["pe_own"] = np.ascontiguousarray(pe[h * HALF:(h + 1) * HALF])
        m["pe_oth"] = np.ascontiguousarray(pe[(1 - h) * HALF:(2 - h) * HALF])
        m["ctxb"] = np.ascontiguousarray(ctx[b])
        cv = np.stack([_fm(c[b], 8), _fm(cc, 8)], axis=2).reshape(128, 16)
        m["cvec"] = np.ascontiguousarray(cv)
        fl = np.zeros((128, 2), np.float32)
        fl[:, h] = 1.0
        m["flags"] = fl
        maps.append(m)
    return maps


_NC_CACHE = {}


def kernel(**inputs):
    if "nc" not in _NC_CACHE:
        _NC_CACHE["nc"] = Builder(stage=99).build()
    nc = _NC_CACHE["nc"]
    maps = make_in_maps(inputs)
    res = run_bass_kernel_spmd(nc, maps, core_ids=list(range(8)))
    out = np.empty((4, SEQ, D), np.float32)
    for k in range(8):
        b, h = k // 2, k % 2
        out[b, h * HALF:(h + 1) * HALF] = res.results[k]["out"]
    return out
```

```python
import os
import numpy as np
import concourse.bass as bass
import concourse.mybir as mybir
from concourse.bass_utils import run_bass_kernel_spmd
from contextlib import ExitStack

F32 = mybir.dt.float32
BF16 = mybir.dt.bfloat16
AF = mybir.ActivationFunctionType
ALU = mybir.AluOpType
AX = mybir.AxisListType

D = 1024
KC = 8
SEQ = 8192
HALF = 4096
CTX = 256
RW = 1280
NJ = 10
NH = 5
NE = 64
EPS = 1e-6
NTOT = CTX + 2 * HALF

COMPUTE = ("pe", "act", "dve", "pool")
QUEUES = ("sp", "q2")
NSEM = 8
SAME_ENGINE_SYNC = True


class Buf:
    __slots__ = ("name", "w", "r")

    def __init__(self, name=""):
        self.name = name
        self.w = None
        self.r = []


class Op:
    __slots__ = ("eng", "fn", "deps", "signal", "val", "idx", "is_dma", "qn")

    def __init__(self, eng, fn, is_dma=False):
        self.eng = eng
        self.fn = fn
        self.deps = []
        self.signal = False
        self.val = None
        self.is_dma = is_dma
        self.qn = 0


class Prog:
    def __init__(self):
        self.streams = {"pe": [], "act": [], "dve": [], "pool": [], "sp": []}
        self.ndma = {"sp": 0, "q2": 0}
        self.ring = {"sp": [None] * NSEM, "q2": [None] * NSEM}

    @staticmethod
    def _stream_of(eng):
        return "pool" if eng == "q2" else eng

    def op(self, eng, fn, reads=(), writes=(), extra=()):
        is_dma = eng in QUEUES
        o = Op(eng, fn, is_dma)
        if is_dma:
            o.qn = self.ndma[eng]
            self.ndma[eng] += 1
            self.ring[eng][o.qn % NSEM] = o
        st = self._stream_of(eng)
        cand = []
        for b in reads:
            if b.w is not None:
                cand.append((b.w, True))
        for b in writes:
            if b.w is not None:
                cand.append((b.w, True))
            for r in b.r:
                cand.append((r, False))
        for d in extra:
            cand.append((d, True))
        seen = set()
        for d, strong in cand:
            if d is o or id(d) in seen:
                continue
            same = (not d.is_dma) and (not is_dma) and self._stream_of(d.eng) == st
            if same:
                if d.eng == "pe" or not SAME_ENGINE_SYNC:
                    continue
            seen.add(id(d))
            o.deps.append(d)
            d.signal = True
        for b in reads:
            b.r.append(o)
        for b in writes:
            b.w = o
            b.r = []
        o.idx = len(self.streams[st])
        self.streams[st].append(o)
        return o

    def barrier(self):
        last = []
        for st, ops in self.streams.items():
            for o in reversed(ops):
                if not o.is_dma:
                    last.append(o)
                    break
        for q in QUEUES:
            for o in self.ring[q]:
                if o is not None:
                    last.append(o)
        for st in ("pe", "act", "dve", "pool", "sp"):
            self.op(st, (lambda e: e.nop()), extra=[d for d in last])

    def assign(self):
        for st, ops in self.streams.items():
            cnt = 0
            for o in ops:
                if o.is_dma:
                    o.val = 16 * (o.qn // NSEM + 1)
                elif o.signal:
                    cnt += 1
                    o.val = cnt

    def emit_stream(self, st, engobj, sem_eng, sem_dma):
        waited = {}

        def wait(key, sem, val):
            if waited.get(key, 0) >= val:
                return
            engobj.wait_ge(sem, val)
            waited[key] = val

        for o in self.streams[st]:
            for d in o.deps:
                if d.is_dma:
                    wait((d.eng, d.qn % NSEM), sem_dma[d.eng][d.qn % NSEM], d.val)
                else:
                    dst = self._stream_of(d.eng)
                    wait(dst, sem_eng[dst], d.val)
            if o.is_dma:
                if o.qn >= NSEM:
                    wait((o.eng, o.qn % NSEM), sem_dma[o.eng][o.qn % NSEM], o.val - 16)
                ins = o.fn(engobj)
                ins.then_inc(sem_dma[o.eng][o.qn % NSEM], 16)
            else:
                ins = o.fn(engobj)
                if o.signal:
                    ins.then_inc(sem_eng[st], 1)

    def final_waits(self, engobj, sem_eng, sem_dma, ops):
        for d in ops:
            if d.is_dma:
                engobj.wait_ge(sem_dma[d.eng][d.qn % NSEM], d.val)
            else:
                engobj.wait_ge(sem_eng[self._stream_of(d.eng)], d.val)


def run_prog(nc, P, final_ops):
    P.assign()
    with ExitStack() as es:
        sem_eng = {s: es.enter_context(nc.semaphore("sem_" + s)) for s in COMPUTE}
        sem_dma = {q: [es.enter_context(nc.semaphore(f"semd_{q}_{i}")) for i in range(NSEM)]
                   for q in QUEUES}
        block = es.enter_context(nc.Block())

        @block.tensor
        def _(e):
            P.emit_stream("pe", e, sem_eng, sem_dma)

        @block.scalar
        def _(e):
            P.emit_stream("act", e, sem_eng, sem_dma)

        @block.vector
        def _(e):
            P.emit_stream("dve", e, sem_eng, sem_dma)

        @block.gpsimd
        def _(e):
            P.emit_stream("pool", e, sem_eng, sem_dma)

        @block.sync
        def _(e):
            P.emit_stream("sp", e, sem_eng, sem_dma)
            P.final_waits(e, sem_eng, sem_dma, final_ops)


class T:
    __slots__ = ("ap", "b")

    def __init__(self, ap, name=""):
        self.ap = ap
        self.b = Buf(name)


class Ring:
    def __init__(self, items):
        self.items = items
        self.i = 0

    def next(self):
        t = self.items[self.i % len(self.items)]
        self.i += 1
        return t


ARENA_COLS = 52992


class Builder:
    def __init__(self, stage=99, debug=False):
        self.stage = stage
        self.debug = debug
        self.nc = bass.Bass("TRN2", target_bir_lowering=False)
        self.P = Prog()
        self.outs = []
        self.es = ExitStack()

    def din(self, name, shape, dt=F32):
        return self.nc.dram_tensor(name, list(shape), dt, kind="ExternalInput").ap()

    def dscr(self, name, shape, dt=F32, out=False):
        kind = "ExternalOutput" if out else "Internal"
        return self.nc.dram_tensor(name, list(shape), dt, kind=kind).ap()

    def alloc(self, ncols, dt=F32, name=""):
        n32 = ncols if dt == F32 else (ncols + 1) // 2
        assert self.top + n32 <= ARENA_COLS, f"arena overflow {self.top}+{n32} ({name})"
        ap = self.arena[:, self.top:self.top + n32]
        self.top += n32
        if dt != F32:
            ap = ap.bitcast(dt)
        return ap

    def tile(self, ncols, dt=F32, name=""):
        return T(self.alloc(ncols, dt, name), name)

    def dma(self, q, out, in_, reads=(), writes=(), slow=False):
        if slow:
            return self.P.op(q, lambda e: e.dma_start(out=out, in_=in_, allow_slow_non_contiguous=True), reads, writes)
        return self.P.op(q, lambda e: e.dma_start(out=out, in_=in_), reads, writes)

    def act(self, out, in_, func, reads=(), writes=(), **kw):
        return self.P.op("act", lambda e: e.activation(out=out, in_=in_, func=func, **kw), reads, writes)

    def ts(self, eng, out, in0, s1, op0, s2=None, op1=None, reads=(), writes=()):
        if op1 is None:
            return self.P.op(eng, lambda e: e.tensor_scalar(out=out, in0=in0, scalar1=s1, scalar2=None, op0=op0),
                             reads, writes)
        return self.P.op(eng, lambda e: e.tensor_scalar(out=out, in0=in0, scalar1=s1, scalar2=s2, op0=op0, op1=op1),
                         reads, writes)

    def tt(self, eng, out, in0, in1, op, reads=(), writes=()):
        return self.P.op(eng, lambda e: e.tensor_tensor(out=out, in0=in0, in1=in1, op=op), reads, writes)

    def stt(self, out, in0, scalar, in1, op0, op1, reads=(), writes=()):
        return self.P.op("dve", lambda e: e.scalar_tensor_tensor(out=out, in0=in0, scalar=scalar, in1=in1,
                                                                 op0=op0, op1=op1), reads, writes)

    def mm(self, out, lhsT, rhs, start, stop, reads=(), writes=()):
        return self.P.op("pe", lambda e: e.matmul(out, lhsT, rhs, start=start, stop=stop), reads, writes)

    def tr(self, out, in_, ident, reads=(), writes=()):
        return self.P.op("pe", lambda e: e.transpose(out=out, in_=in_, identity=ident), reads, writes)

    def copy(self, eng, out, in_, reads=(), writes=()):
        if eng == "act":
            return self.P.op("act", lambda e: e.copy(out=out, in_=in_), reads, writes)
        return self.P.op(eng, lambda e: e.tensor_copy(out=out, in_=in_), reads, writes)

    def memset(self, eng, ap, val, writes=()):
        return self.P.op(eng, lambda e: e.memset(ap, val), (), writes)

    def declare(self):
        dbg = self.debug
        I = {}
        I["x_own"] = self.din("x_own", [HALF, D])
        I["x_oth"] = self.din("x_oth", [HALF, D])
        I["ctxb"] = self.din("ctxb", [CTX, D])
        I["pe_own"] = self.din("pe_own", [HALF, D])
        I["pe_oth"] = self.din("pe_oth", [HALF, D])
        I["cvec"] = self.din("cvec", [128, 16])
        I["flags"] = self.din("flags", [128, 2])
        I["ident"] = self.din("ident", [128, 128])
        I["ada_w"] = self.din("ada_w", [2, D, 6 * D])
        I["adab_fm"] = self.din("adab_fm", [128, 2 * 96])
        I["adab_row"] = self.din("adab_row", [1, 2 * 6 * D])
        I["ng"] = self.din("ng", [128, 4 * KC])
        I["fin_row"] = self.din("fin_row", [1, D])
        I["a_w_in"] = self.din("a_w_in", [D, 2 * RW])
        I["a_small"] = self.din("a_small", [128, 110])
        I["a_rw"] = self.din("a_rw", [2, NH, 256, 256])
        I["a_iw"] = self.din("a_iw", [2, NH, 256, 256])
        I["a_w_out"] = self.din("a_w_out", [RW, D])
        I["b_w_in"] = self.din("b_w_in", [D, 4096])
        I["b_ln_row"] = self.din("b_ln_row", [1, 4096])
        I["b_lng_fm"] = self.din("b_lng_fm", [128, 32])
        I["b_w_s"] = self.din("b_w_s", [8, 128, 128])
        I["b_bs_row"] = self.din("b_bs_row", [1, 1024])
        I["b_w_out"] = self.din("b_w_out", [2048, D])
        I["router_w"] = self.din("router_w", [2, D, NE])
        I["rb_row"] = self.din("rb_row", [1, 2 * NE])
        if self.stage >= 3:
            I["moe_w_gu"] = self.din("moe_w_gu", [2, NE, D, 512])
            I["moe_w_down"] = self.din("moe_w_down", [2, NE, 256, D])
        I["sh_w_gu"] = self.din("sh_w_gu", [2, D, 512])
        I["sh_w_down"] = self.din("sh_w_down", [2, 256, D])
        self.I = I
        st = self.stage
        self.out = self.nc.dram_tensor("out", [HALF, D], F32, kind="ExternalOutput").ap()
        self.uT = self.dscr("uT", [NJ, 128, NTOT], F32, out=(dbg and st == 1))
        self.gg = self.dscr("gg", [NJ, 128, HALF], F32, out=(dbg and st == 1))
        self.ygT = self.dscr("ygT", [NJ, 128, HALF], BF16, out=(dbg and st == 2))
        self.XS = self.dscr("XS", [HALF, D], F32)
        self.dbgX = self.dscr("dbgX", [HALF, D], F32, out=True) if dbg else None
        self.GBC = self.dscr("GBC", [4, 128, D], F32, out=(dbg and st == 1))
        self.modd = self.dscr("modd", [128, 192], F32, out=(dbg and st == 1))
        self.uT_b = {}
        self.gg_b = {}
        self.yg_b = {}
        self.XS_b = [Buf(f"XS{t}") for t in range(32)]
        self.GBC_b = [Buf(f"GBC{i}") for i in range(4)]

    def build(self):
        nc = self.nc
        self.declare()
        es = self.es
        self.arena = es.enter_context(nc.sbuf_tensor("arena", [128, ARENA_COLS], F32))
        self.ps = es.enter_context(nc.psum_tensor("ps", [128, 4096], F32))
        self.psb = [Buf(f"bank{i}") for i in range(8)]
        self.top = 0
        self.phase_pro()
        self.const_top = self.top
        if self.stage >= 1:
            self.phase_a0()
        dbgskip = os.environ.get('SKIPSTAGES', '')
        if self.stage >= 2 and 'b0' not in dbgskip:
            self.P.barrier()
            self.top = self.const_top
            self.phase_b0()
        if self.stage >= 3 and 'm0' not in dbgskip:
            for hh in range(2):
                self.P.barrier()
                self.top = self.const_top
                self.phase_m(0, hh)
        if self.stage >= 4:
            self.P.barrier()
            self.top = self.const_top
            self.phase_s1()
        if self.stage >= 5:
            for hh in range(2):
                self.P.barrier()
                self.top = self.const_top
                self.phase_m(1, hh)
        if self.stage < 5:
            self.P.barrier()
            self.top = self.const_top
            z = self.tile(D, F32, "zero")
            self.memset("dve", z.ap, 0.0, writes=[z.b])
            for t in range(32):
                self.outs.append(self.dma("sp", self.out[t * 128:(t + 1) * 128, :], z.ap, reads=[z.b]))
        self.P.barrier()
        run_prog(nc, self.P, self.outs)
        es.close()
        return nc

    def bank(self, b, n=1):
        return self.ps[:, b * 512:(b + n) * 512]

    def phase_pro(self):
        I = self.I
        c = {}
        for name, n in [("ident", 128), ("cvec", 16), ("flags", 2), ("adab_fm", 192), ("ng", 32),
                        ("a_small", 110), ("b_lng_fm", 32)]:
            c[name] = self.tile(n, F32, name)
            self.dma("sp", c[name].ap, I[name], writes=[c[name].b])
        ones = self.tile(128, F32, "ones")
        self.memset("dve", ones.ap, 1.0, writes=[ones.b])
        self.ident, self.ones, self.flags = c["ident"], ones, c["flags"]
        silu = self.tile(16, F32, "silu_c")
        self.act(silu.ap, c["cvec"].ap, AF.Silu, reads=[c["cvec"].b], writes=[silu.b])
        mod = self.tile(192, F32, "mod")
        self.mod = mod
        fin_bc = self.tile(D, F32, "fin_bc")
        self.fin_bc = fin_bc
        rb_bc = self.tile(2 * NE, F32, "rb_bc")
        self.rb_bc = rb_bc
        self.A1 = [self.tile(KC, F32, f"A1_{i}") for i in range(2)]
        self.B1 = [self.tile(KC, F32, f"B1_{i}") for i in range(2)]
        self.A2 = [self.tile(KC, F32, f"A2_{i}") for i in range(2)]
        self.B2 = [self.tile(KC, F32, f"B2_{i}") for i in range(2)]
        self.A1c = self.tile(KC, F32, "A1c")
        self.B1c = self.tile(KC, F32, "B1c")
        self.asm = c["a_small"]
        self.nrb = self.tile(20, F32, "nrb")
        self.nib = self.tile(20, F32, "nib")
        self.c8 = self.tile(20, F32, "c8")
        self.c16 = self.tile(20, F32, "c16")
        self.neghalf = self.tile(8, F32, "neghalf")
        self.memset("dve", self.neghalf.ap, -0.5, writes=[self.neghalf.b])
        self.lng_fm = c["b_lng_fm"]
        mark = self.top
        srep = self.tile(KC * 128, F32, "silu_rep")
        for kc in range(KC):
            self.ts("dve", srep.ap[:, kc * 128:(kc + 1) * 128], ones.ap, silu.ap[:, 2 * kc:2 * kc + 1], ALU.mult,
                    reads=[ones.b, silu.b], writes=[srep.b])
        rows = self.tile(2 * 6 * D, F32, "adab_row")
        self.dma("sp", rows.ap[0:1, :], I["adab_row"], writes=[rows.b])
        frow = self.tile(D + 2 * NE, F32, "fin_row")
        self.dma("sp", frow.ap[0:1, 0:D], I["fin_row"], writes=[frow.b])
        self.dma("sp", frow.ap[0:1, D:D + 2 * NE], I["rb_row"], writes=[frow.b])
        adat = Ring([self.tile(KC * 512, F32, f"adat{i}") for i in range(2)])
        gst = Ring([self.tile(512, F32, f"gst{i}") for i in range(2)])
        for n in range(2):
            self.mm(self.bank(3)[:, 0:512], ones.ap[0:1, :], frow.ap[0:1, n * 512:(n + 1) * 512], True, True,
                    reads=[ones.b, frow.b], writes=[self.psb[3]])
            self.copy("dve", fin_bc.ap[:, n * 512:(n + 1) * 512], self.bank(3), reads=[self.psb[3]], writes=[fin_bc.b])
        self.mm(self.bank(3)[:, 0:2 * NE], ones.ap[0:1, :], frow.ap[0:1, D:D + 2 * NE], True, True,
                reads=[ones.b, frow.b], writes=[self.psb[3]])
        self.copy("dve", rb_bc.ap, self.bank(3)[:, 0:2 * NE], reads=[self.psb[3]], writes=[rb_bc.b])
        for i in range(2):
            psA = self.bank(0)[:, 0:96]
            for g in range(12):
                at = adat.next()
                src = I["ada_w"][i][:, g * 512:(g + 1) * 512].rearrange("(kc p) n -> p kc n", p=128)
                self.dma("sp" if g % 2 == 0 else "q2", at.ap.rearrange("p (kc n) -> p kc n", kc=KC), src, writes=[at.b])
                for fc in range(4):
                    col = (g * 4 + fc) * 2
                    for kc in range(KC):
                        self.mm(psA[:, col:col + 2], at.ap[:, kc * 512 + fc * 128: kc * 512 + (fc + 1) * 128],
                                silu.ap[:, 2 * kc:2 * kc + 2], kc == 0, kc == KC - 1,
                                reads=[at.b, silu.b], writes=[self.psb[0]])
                if g in (4, 5, 10, 11):
                    bk = 1 + (g % 2)
                    for kc in range(KC):
                        self.mm(self.bank(bk), srep.ap[:, kc * 128:(kc + 1) * 128], at.ap[:, kc * 512:(kc + 1) * 512],
                                kc == 0, False, reads=[srep.b, at.b], writes=[self.psb[bk]])
                    self.mm(self.bank(bk), ones.ap[0:1, :], rows.ap[0:1, i * 6 * D + g * 512: i * 6 * D + (g + 1) * 512],
                            False, True, reads=[ones.b, rows.b], writes=[self.psb[bk]])
                    s = gst.next()
                    self.copy("dve", s.ap, self.bank(bk), reads=[self.psb[bk]], writes=[s.b])
                    idx = i * 2 + (0 if g < 6 else 1)
                    self.dma("sp", self.GBC[idx][:, (g % 2) * 512:(g % 2 + 1) * 512], s.ap, reads=[s.b],
                             writes=[self.GBC_b[idx]])
            self.tt("dve", mod.ap[:, i * 96:(i + 1) * 96], psA, c["adab_fm"].ap[:, i * 96:(i + 1) * 96], ALU.add,
                    reads=[self.psb[0], c["adab_fm"].b], writes=[mod.b])
        if self.debug and self.stage == 1:
            self.dma("sp", self.modd, mod.ap, reads=[mod.b])

        def mcol(i, chunk0, which):
            base = i * 96 + chunk0 * 2 + which
            return mod.ap[:, base: base + 2 * KC: 2]
        ng = c["ng"].ap
        tmp = self.tile(KC, F32, "tmpk")
        for i in range(2):
            for (A, Bv, gi, sc_chunk, sh_chunk, which) in [
                    (self.A1[i], self.B1[i], 2 * i, 8, 0, 0), (self.A2[i], self.B2[i], 2 * i + 1, 32, 24, 0)]:
                self.ts("dve", tmp.ap, mcol(i, sc_chunk, which), 1.0, ALU.add, reads=[mod.b], writes=[tmp.b])
                self.tt("dve", A.ap, tmp.ap, ng[:, gi * KC:(gi + 1) * KC], ALU.mult, reads=[tmp.b, c["ng"].b], writes=[A.b])
                self.copy("dve", Bv.ap, mcol(i, sh_chunk, which), reads=[mod.b], writes=[Bv.b])
        self.ts("dve", tmp.ap, mcol(0, 8, 1), 1.0, ALU.add, reads=[mod.b], writes=[tmp.b])
        self.tt("dve", self.A1c.ap, tmp.ap, ng[:, 0:KC], ALU.mult, reads=[tmp.b, c["ng"].b], writes=[self.A1c.b])
        self.copy("dve", self.B1c.ap, mcol(0, 0, 1), reads=[mod.b], writes=[self.B1c.b])
        asm = self.asm
        self.ts("dve", self.nrb.ap, asm.ap[:, 50:70], -1.0, ALU.mult, reads=[asm.b], writes=[self.nrb.b])
        self.ts("dve", self.nib.ap, asm.ap[:, 70:90], -1.0, ALU.mult, reads=[asm.b], writes=[self.nib.b])
        t20 = self.tile(20, F32, "t20")
        self.act(t20.ap, asm.ap[:, 90:110], AF.Exp, reads=[asm.b], writes=[t20.b], scale=-1.0)
        self.act(t20.ap, t20.ap, AF.Ln, reads=[t20.b], writes=[t20.b], bias=1.0)
        self.ts("dve", self.c8.ap, t20.ap, -8.0, ALU.mult, reads=[t20.b], writes=[self.c8.b])
        self.ts("dve", self.c16.ap, t20.ap, -16.0, ALU.mult, reads=[t20.b], writes=[self.c16.b])
        self.P.barrier()
        self.top = mark

    def norm_tile(self, xt, A, Bv, dst, nb, dst32=None):
        st = self.norm_a(xt, nb)
        self.norm_b(st, A, Bv, dst, nb, dst32)

    def norm_a(self, xt, nb):
        junk, ss, rstd, xs = nb["junkr"].next(), nb["ss"].next(), nb["rstd"].next(), nb["xs"].next()
        self.act(junk.ap, xt.ap, AF.Square, reads=[xt.b], writes=[ss.b, junk.b], accum_out=ss.ap)
        self.ts("pool", rstd.ap, ss.ap, 1.0 / D, ALU.mult, EPS, ALU.add, reads=[ss.b], writes=[rstd.b])
        self.tt("pool", rstd.ap, rstd.ap, self.neghalf.ap[:, 0:1], ALU.pow, reads=[rstd.b, self.neghalf.b],
                writes=[rstd.b])
        self.ts("dve", xs.ap, xt.ap, rstd.ap, ALU.mult, reads=[xt.b, rstd.b], writes=[xs.b])
        return xs

    def norm_b(self, xs, A, Bv, dst, nb, dst32=None):
        bk = nb["bk"].next()
        for c in range(KC):
            b = bk + c // 4
            self.tr(self.bank(b)[:, (c % 4) * 128:(c % 4 + 1) * 128], xs.ap[:, c * 128:(c + 1) * 128],
                    self.ident.ap, reads=[xs.b, self.ident.b], writes=[self.psb[b]])
        for c in range(KC):
            b = bk + c // 4
            src = self.bank(b)[:, (c % 4) * 128:(c % 4 + 1) * 128]
            if dst32 is not None:
                ap32, b32 = dst32(c)
                self.act(ap32, src, AF.Identity, reads=[self.psb[b], A.b, Bv.b], writes=[b32],
                         scale=A.ap[:, c:c + 1], bias=Bv.ap[:, c:c + 1])
                continue
            ap, bb = dst(c)
            self.act(ap, src, AF.Identity, reads=[self.psb[b], A.b, Bv.b], writes=[bb],
                     scale=A.ap[:, c:c + 1], bias=Bv.ap[:, c:c + 1])

    def norm_bufs(self, banks=((0, 2)), nxs=2):
        nb = {}
        nb["junkr"] = Ring([self.tile(D, BF16, f"junk{i}") for i in range(2)])
        nb["junk"] = nb["junkr"].items[0]
        nb["ss"] = Ring([self.tile(1, F32, f"ss{i}") for i in range(8)])
        nb["rstd"] = Ring([self.tile(1, F32, f"rstd{i}") for i in range(8)])
        nb["xs"] = Ring([self.tile(D, F32, f"xs{i}") for i in range(nxs)])
        nb["bk"] = Ring(list(banks))
        return nb

    def phase_a0(self):
        I = self.I
        winb = self.tile(KC * 2 * RW, BF16, "a_w_in_b")
        self.dma("q2", winb.ap.rearrange("p (kc n) -> p kc n", kc=KC),
                 I["a_w_in"].rearrange("(kc p) n -> p kc n", p=128), writes=[winb.b])
        SKEW = 3
        xts = Ring([self.tile(D, F32, f"xt{i}") for i in range(SKEW + 1)])
        pets = Ring([self.tile(D, F32, f"pet{i}") for i in range(SKEW + 1)])
        nb = self.norm_bufs(banks=(0, 2), nxs=SKEW + 1)
        pend = []
        hx = [[[T(None, f"hx{s}_{c}_{t}") for t in range(4)] for c in range(KC)] for s in range(2)]
        hx_ap = [self.alloc(KC * 512, BF16, f"hxblk{s}") for s in range(2)]
        ust = Ring([self.tile(NJ * 512, F32, f"ust{i}") for i in range(2)])
        gst = Ring([self.tile(NJ * 512, F32, f"gst{i}") for i in range(2)])
        pbank = Ring([4, 5, 6, 7])
        segs = [("ctx", I["ctxb"], None, 0, CTX, self.A1c, self.B1c),
                ("oth", I["x_oth"], I["pe_oth"], CTX, HALF, self.A1[0], self.B1[0]),
                ("own", I["x_own"], I["pe_own"], CTX + HALF, HALF, self.A1[0], self.B1[0])]
        blk_i = 0
        for (sname, xsrc, pesrc, col0, ntok, A, Bv) in segs:
            nblk = (ntok + 511) // 512
            for blk in range(nblk):
                ncol = min(512, ntok - blk * 512)
                s = blk_i % 2
                blk_i += 1
                hap = hx_ap[s].rearrange("p (kc n) -> p kc n", kc=KC)
                for tt_ in range(ncol // 128):
                    t = blk * 4 + tt_
                    xt = xts.next()
                    self.dma("sp", xt.ap, xsrc[t * 128:(t + 1) * 128, :], writes=[xt.b])
                    if pesrc is not None:
                        pet = pets.next()
                        self.dma("sp", pet.ap, pesrc[t * 128:(t + 1) * 128, :], writes=[pet.b])
                        self.tt("pool", xt.ap, xt.ap, pet.ap, ALU.add, reads=[xt.b, pet.b], writes=[xt.b])
                    xs_ = self.norm_a(xt, nb)
                    pend.append(("norm", xs_, A, Bv,
                                 (lambda c, tt_=tt_, hap=hap, s=s: (hap[:, c, tt_ * 128:(tt_ + 1) * 128], hx[s][c][tt_].b)),
                                 (xt, t) if sname == "own" else None))
                    if tt_ == ncol // 128 - 1:
                        pend.append(("proj", sname, blk, ncol, s, hap, col0))
                    while len([p for p in pend if p[0] == "norm"]) > SKEW:
                        self._a0_drain(pend, nb, winb, hx, ust, gst, pbank)
        while pend:
            self._a0_drain(pend, nb, winb, hx, ust, gst, pbank)

    def _a0_drain(self, pend, nb, winb, hx, ust, gst, pbank):
        it = pend.pop(0)
        if it[0] == "norm":
            _, xs_, A, Bv, dst, st = it
            if st is not None:
                xt, t = st
                self.dma("sp", self.XS[t * 128:(t + 1) * 128, :], xt.ap, reads=[xt.b], writes=[self.XS_b[t]])
                if self.debug and self.stage == 1:
                    self.dma("sp", self.dbgX[t * 128:(t + 1) * 128, :], xt.ap, reads=[xt.b])
            self.norm_b(xs_, A, Bv, dst, nb)
            return
        _, sname, blk, ncol, s, hap, col0 = it
        if True:
            if True:
                nt = ncol // 128
                us = ust.next()
                usv = us.ap.rearrange("p (j n) -> p j n", j=NJ)
                for j in range(NJ):
                    bk = pbank.next()
                    for kc in range(KC):
                        self.mm(self.bank(bk)[:, 0:ncol], winb.ap[:, kc * 2 * RW + RW + j * 128: kc * 2 * RW + RW + (j + 1) * 128],
                                hap[:, kc, 0:ncol], kc == 0, kc == KC - 1,
                                reads=[winb.b] + [hx[s][kc][q].b for q in range(nt)], writes=[self.psb[bk]])
                    self.copy("dve", usv[:, j, 0:ncol], self.bank(bk)[:, 0:ncol], reads=[self.psb[bk]], writes=[us.b])
                key = (sname, blk)
                self.uT_b[key] = Buf(f"uT{key}")
                self.dma("sp", self.uT.rearrange("j p n -> p j n")[:, :, col0 + blk * 512: col0 + blk * 512 + ncol],
                         usv[:, :, 0:ncol], reads=[us.b], writes=[self.uT_b[key]])
                if sname == "own":
                    gs = gst.next()
                    gsv = gs.ap.rearrange("p (j n) -> p j n", j=NJ)
                    for j in range(NJ):
                        bk = pbank.next()
                        for kc in range(KC):
                            self.mm(self.bank(bk), winb.ap[:, kc * 2 * RW + j * 128: kc * 2 * RW + (j + 1) * 128],
                                    hap[:, kc, :], kc == 0, kc == KC - 1,
                                    reads=[winb.b] + [hx[s][kc][q].b for q in range(4)], writes=[self.psb[bk]])
                        self.act(gsv[:, j, :], self.bank(bk), AF.Gelu_apprx_tanh, reads=[self.psb[bk]], writes=[gs.b])
                    self.gg_b[blk] = Buf(f"gg{blk}")
                    self.dma("sp", self.gg.rearrange("j p n -> p j n")[:, :, blk * 512:(blk + 1) * 512], gsv,
                             reads=[gs.b], writes=[self.gg_b[blk]])

    def phase_b0(self):
        I = self.I
        asm = self.asm
        rwb = self.tile(2 * NH * 2 * 256, BF16, "rwb")
        iwb = self.tile(2 * NH * 2 * 256, BF16, "iwb")
        for (wt, src) in ((rwb, I["a_rw"]), (iwb, I["a_iw"])):
            for d in range(2):
                self.dma("q2", wt.ap[:, d * NH * 512:(d + 1) * NH * 512].rearrange("p (h kc n) -> p h kc n", h=NH, kc=2),
                         src[d].rearrange("h (kc p) n -> p h kc n", p=128), writes=[wt.b])

        def gw(wt, d, h, kc, co):
            base = ((d * NH + h) * 2 + kc) * 256 + co * 128
            return wt.ap[:, base:base + 128]
        Ust = [self.tile(HALF + 3, F32, "Ust0")]
        UCs = [self.tile(2 * HALF, F32, f"UC{i}") for i in range(2)]
        UCbs = [self.tile(2 * HALF, BF16, f"UCb{i}") for i in range(2)]
        UCc = self.tile(2 * CTX, F32, "UCc")
        UCcb = self.tile(2 * CTX, BF16, "UCcb")
        UCc_b = [[Buf(f"UCc_{c}")] for c in range(2)]
        UCcb_b = [[Buf(f"UCcb_{c}")] for c in range(2)]
        UC_b = [[[Buf(f"UC{p}_{c}_{b}") for b in range(8)] for c in range(2)] for p in range(2)]
        UCb_b = [[[Buf(f"UCb{p}_{c}_{b}") for b in range(8)] for c in range(2)] for p in range(2)]
        cbuf = []
        for co_ in range(2):
            cbuf.append({
                "er": Ring([self.tile(512, F32, f"er{co_}{i}") for i in range(1)]),
                "ei": Ring([self.tile(512, F32, f"ei{co_}{i}") for i in range(1)]),
                "a": Ring([self.tile(512, F32, f"a{co_}{i}") for i in range(1)]),
                "m": Ring([self.tile(512, F32, f"m{co_}{i}") for i in range(1)]),
                "yd": Ring([self.tile(512, F32, f"yd{co_}{i}") for i in range(2)]),
                "stmp": self.tile(1, F32, f"stmp{co_}"),
                "stmp2": self.tile(1, F32, f"stmp2{co_}"),
                "pairs": Ring([(2 * co_, 2 * co_ + 1)]),
            })
        Yf = [self.alloc(HALF, F32, f"Yf{c}") for c in range(2)]
        Yf_b = [[Buf(f"Yf{c}_{i}") for i in range(8)] for c in range(2)]
        ggt_r = Ring([self.tile(512, F32, f"ggt{i}") for i in range(1)])
        ygb_r = Ring([self.tile(512, BF16, f"ygb{i}") for i in range(1)])
        s_ctx = [[self.tile(1, F32, f"sctx{c}{d}") for d in range(2)] for c in range(2)]
        s_oth = [[self.tile(1, F32, f"soth{c}{d}") for d in range(2)] for c in range(2)]
        s_ini = [[self.tile(1, F32, f"sini{c}{d}") for d in range(2)] for c in range(2)]
        s_tmp = self.tile(1, F32, "stmp")
        pairs = Ring([(0, 1), (2, 3), (4, 5), (6, 7)])
        fl = self.flags
        own0 = CTX + HALF
        oth0 = CTX
        segs = [("ctx", 0, CTX), ("oth", CTX, HALF), ("own", CTX + HALF, HALF)]
        items = [(h, sg) for h in range(NH) for sg in segs]

        dg = self.tile(8 * 128, F32, "convdiag")
        cvb = Ring([4, 5, 6, 7])

        def bufsel(sname):
            if sname == "ctx":
                return (UCc.ap.rearrange("p (c n) -> p c n", c=2), UCcb.ap.rearrange("p (c n) -> p c n", c=2), UCc_b, UCcb_b)
            par = 0 if sname == "oth" else 1
            return (UCs[par].ap.rearrange("p (c n) -> p c n", c=2), UCbs[par].ap.rearrange("p (c n) -> p c n", c=2),
                    UC_b[par], UCb_b[par])

        def conv_gen(idx):
            h, (sname, col0, n) = items[idx]
            nblk = (n + 511) // 512
            UCv, UCbv, ucb_l, ucbb_l = bufsel(sname)
            if sname == "ctx":
                for cc in range(2):
                    for k in range(4):
                        jj = 2 * h + cc
                        self.ts("dve", dg.ap[:, (cc * 4 + k) * 128:(cc * 4 + k + 1) * 128], self.ident.ap,
                                asm.ap[:, jj * 4 + k: jj * 4 + k + 1], ALU.mult, reads=[self.ident.b, asm.b], writes=[dg.b])
            pending = None

            def evac(p):
                bk, ucv, ub, ucbv, ucbb, jj, ncol = p
                self.ts("dve", ucv, self.bank(bk)[:, 0:ncol], asm.ap[:, 40 + jj:41 + jj], ALU.add,
                        reads=[self.psb[bk], asm.b], writes=[ub])
                self.copy("pool", ucbv, ucv, reads=[ub], writes=[ucbb])

            for cc in range(2):
                j = 2 * h + cc
                U = Ust[0]
                srcb = [self.uT_b[(sname, b)] for b in range(nblk)]
                self.dma("sp", U.ap[:, 2:2 + n], self.uT[j][:, col0:col0 + n], reads=srcb, writes=[U.b])
                if sname != "ctx":
                    if sname == "oth":
                        lsrc, lfl, rsrc, rfl = own0 + HALF - 2, 0, own0, 1
                        lb, rb = self.uT_b[("own", 7)], self.uT_b[("own", 0)]
                    else:
                        lsrc, lfl, rsrc, rfl = oth0 + HALF - 2, 1, oth0, 0
                        lb, rb = self.uT_b[("oth", 7)], self.uT_b[("oth", 0)]
                    self.dma("sp", U.ap[:, 0:2], self.uT[j][:, lsrc:lsrc + 2], reads=[lb], writes=[U.b])
                    self.dma("sp", U.ap[:, n + 2:n + 3], self.uT[j][:, rsrc:rsrc + 1], reads=[rb], writes=[U.b], slow=True)
                if pending is not None:
                    evac(pending)
                    pending = None
                yield
                if sname == "ctx":
                    self.memset("dve", U.ap[:, 0:2], 0.0, writes=[U.b])
                    self.memset("dve", U.ap[:, n + 2:n + 3], 0.0, writes=[U.b])
                else:
                    self.ts("dve", U.ap[:, 0:2], U.ap[:, 0:2], fl.ap[:, lfl:lfl + 1], ALU.mult,
                            reads=[U.b, fl.b], writes=[U.b])
                    self.ts("dve", U.ap[:, n + 2:n + 3], U.ap[:, n + 2:n + 3], fl.ap[:, rfl:rfl + 1], ALU.mult,
                            reads=[U.b, fl.b], writes=[U.b])
                for blk in range(nblk):
                    ncol = min(512, n - blk * 512)
                    c0 = blk * 512
                    bk = cvb.next()
                    for k in range(4):
                        self.mm(self.bank(bk)[:, 0:ncol], dg.ap[:, (cc * 4 + k) * 128:(cc * 4 + k + 1) * 128],
                                U.ap[:, c0 + k:c0 + k + ncol], k == 0, k == 3, reads=[dg.b, U.b], writes=[self.psb[bk]])
                    if pending is not None:
                        evac(pending)
                    pending = (bk, UCv[:, cc, c0:c0 + ncol], ucb_l[cc][blk], UCbv[:, cc, c0:c0 + ncol],
                               ucbb_l[cc][blk], j, ncol)
                    yield
            if pending is not None:
                evac(pending)

        def run_all(g):
            for _ in g:
                pass

        import itertools
        run_all(conv_gen(0))
        run_all(conv_gen(1))
        fin_gen = iter(())
        for idx in range(len(items)):
            if True:
                h, (sname, col0, n) = items[idx]
                nblk = (n + 511) // 512
                UCv, UCbv, ucb_l, ucbb_l = bufsel(sname)
                if sname == "ctx":
                    cgen = iter(())
                elif sname == "oth":
                    cgen = conv_gen(idx + 1)
                else:
                    run_all(fin_gen)
                    cgen = itertools.chain(*[conv_gen(i) for i in (idx + 1, idx + 2) if i < len(items)])
                def chain(co, d, sname=sname, n=n, nblk=nblk, h=h, UCv=UCv, UCbv=UCbv, ucb_l=ucb_l, ucbb_l=ucbb_l):
                    j = 2 * h + co
                    bcol = d * NJ + j
                    cb = cbuf[co]
                    if sname == "ctx":
                        init = (0.0, None)
                    elif sname == "oth":
                        init = (s_ctx[co][d].ap, s_ctx[co][d].b)
                    else:
                        f = fl.ap[:, 1:2] if d == 0 else fl.ap[:, 0:1]
                        st_ = cb["stmp"]
                        self.tt("dve", st_.ap, s_oth[co][d].ap, s_ctx[co][d].ap, ALU.subtract,
                                reads=[s_oth[co][d].b, s_ctx[co][d].b], writes=[st_.b])
                        self.stt(s_ini[co][d].ap, st_.ap, f, s_ctx[co][d].ap, ALU.mult, ALU.add,
                                 reads=[st_.b, fl.b, s_ctx[co][d].b], writes=[s_ini[co][d].b])
                        init = (s_ini[co][d].ap, s_ini[co][d].b)
                    order = list(range(nblk)) if d == 0 else list(range(nblk - 1, -1, -1))
                    for bi, blk in enumerate(order):
                        ncol = min(512, n - blk * 512)
                        cols = slice(blk * 512, blk * 512 + ncol)
                        pr, pi = cb["pairs"].next()
                        for (pb, wt) in ((pr, rwb), (pi, iwb)):
                            for kc in range(2):
                                self.mm(self.bank(pb)[:, 0:ncol], gw(wt, d, h, kc, co), UCbv[:, kc, cols], kc == 0, kc == 1,
                                        reads=[wt.b, ucbb_l[0][blk], ucbb_l[1][blk]], writes=[self.psb[pb]])
                        yield
                        er, ei, a, m = cb["er"].next(), cb["ei"].next(), cb["a"].next(), cb["m"].next()
                        erv, eiv, av, mv = er.ap[:, 0:ncol], ei.ap[:, 0:ncol], a.ap[:, 0:ncol], m.ap[:, 0:ncol]
                        self.act(erv, self.bank(pr)[:, 0:ncol], AF.Exp, reads=[self.psb[pr], self.nrb.b], writes=[er.b],
                                 scale=-1.0, bias=self.nrb.ap[:, bcol:bcol + 1])
                        yield
                        self.act(erv, erv, AF.Ln, reads=[er.b], writes=[er.b], bias=1.0)
                        yield
                        self.act(erv, erv, AF.Exp, reads=[er.b], writes=[er.b], scale=-1.0)
                        yield
                        self.act(av, erv, AF.Exp, reads=[er.b, self.c8.b], writes=[a.b], scale=self.c8.ap[:, bcol:bcol + 1])
                        self.tt("pool", mv, av, av, ALU.mult, reads=[a.b], writes=[m.b])
                        yield
                        self.act(eiv, self.bank(pi)[:, 0:ncol], AF.Exp, reads=[self.psb[pi], self.nib.b], writes=[ei.b],
                                 scale=-1.0, bias=self.nib.ap[:, bcol:bcol + 1])
                        yield
                        self.act(eiv, eiv, AF.Ln, reads=[ei.b], writes=[ei.b], bias=1.0)
                        yield
                        first_ctx = (sname == "ctx" and bi == 0)
                        if first_ctx:
                            c0 = 0 if d == 0 else ncol - 1
                            ig0 = cb["stmp2"]
                            self.act(ig0.ap, ei.ap[:, c0:c0 + 1], AF.Exp, reads=[ei.b], writes=[ig0.b], scale=-1.0)
                        self.act(mv, mv, AF.Ln, reads=[m.b], writes=[m.b], scale=-1.0, bias=1.0)
                        yield
                        self.stt(eiv, mv, 0.5, eiv, ALU.mult, ALU.subtract, reads=[m.b, ei.b], writes=[ei.b])
                        self.act(eiv, eiv, AF.Exp, reads=[ei.b], writes=[ei.b])
                        yield
                        self.tt("dve", mv, eiv, UCv[:, co, cols], ALU.mult, reads=[ei.b, ucb_l[co][blk]], writes=[m.b])
                        if first_ctx:
                            self.tt("dve", m.ap[:, c0:c0 + 1], ig0.ap, UCv[:, co, blk * 512 + c0: blk * 512 + c0 + 1], ALU.mult,
                                    reads=[ig0.b, ucb_l[co][blk], m.b], writes=[m.b])
                        if sname == "own" and d == 0:
                            yap, yb = Yf[co][:, cols], Yf_b[co][blk]
                        else:
                            yd = cb["yd"].next()
                            yap, yb = yd.ap[:, 0:ncol], yd.b
                        ini_ap, ini_b = init
                        rd = [a.b, m.b] + ([ini_b] if ini_b is not None else [])
                        if d == 0:
                            self.P.op("dve", lambda e, o=yap, d0=av, d1=mv, ii=ini_ap: e.tensor_tensor_scan(
                                out=o, data0=d0, data1=d1, initial=ii, op0=ALU.mult, op1=ALU.add), rd, [yb])
                            init = (yap[:, ncol - 1:ncol], yb)
                        else:
                            self.P.op("dve", lambda e, o=yap, d0=av, d1=mv, ii=ini_ap: e.tensor_tensor_scan(
                                out=o[:, ::-1], data0=d0[:, ::-1], data1=d1[:, ::-1], initial=ii, op0=ALU.mult, op1=ALU.add),
                                rd, [yb])
                            init = (yap[:, 0:1], yb)
                        if sname == "own" and d == 1:
                            self.tt("pool", Yf[co][:, cols], Yf[co][:, cols], yap, ALU.add, reads=[Yf_b[co][blk], yb],
                                    writes=[Yf_b[co][blk]])
                        yield
                    if sname == "ctx":
                        self.copy("dve", s_ctx[co][d].ap, init[0], reads=[init[1]], writes=[s_ctx[co][d].b])
                    elif sname == "oth":
                        self.copy("dve", s_oth[co][d].ap, init[0], reads=[init[1]], writes=[s_oth[co][d].b])

                stepn = 0
                for d in range(2):
                    gens = [chain(0, d), chain(1, d)]
                    while gens:
                        for g_ in list(gens):
                            try:
                                next(g_)
                            except StopIteration:
                                gens.remove(g_)
                            stepn += 1
                            if stepn % 14 == 0:
                                next(cgen, None)
                            if stepn % 9 == 0:
                                next(fin_gen, None)
                run_all(cgen)
                if sname == "own":
                    def finalize_gen(h=h):
                        prev = None
                        for co in range(2):
                            j = 2 * h + co
                            for blk in range(8):
                                cols = slice(blk * 512, (blk + 1) * 512)
                                if prev is not None:
                                    pj, pcols, pco, pblk, pggt = prev
                                    ygb = ygb_r.next()
                                    self.tt("pool", ygb.ap, pggt.ap, Yf[pco][:, pcols], ALU.mult, reads=[pggt.b, Yf_b[pco][pblk]], writes=[ygb.b])
                                    key = (pj, pblk)
                                    self.yg_b[key] = Buf(f"yg{key}")
                                    self.dma("sp", self.ygT[pj][:, pcols], ygb.ap, reads=[ygb.b], writes=[self.yg_b[key]])
                                ggt = ggt_r.next()
                                self.dma("sp", ggt.ap, self.gg[j][:, cols], reads=[self.gg_b[blk]], writes=[ggt.b])
                                prev = (j, cols, co, blk, ggt)
                                yield
                        pj, pcols, pco, pblk, pggt = prev
                        ygb = ygb_r.next()
                        self.tt("pool", ygb.ap, pggt.ap, Yf[pco][:, pcols], ALU.mult, reads=[pggt.b, Yf_b[pco][pblk]], writes=[ygb.b])
                        key = (pj, pblk)
                        self.yg_b[key] = Buf(f"yg{key}")
                        self.dma("sp", self.ygT[pj][:, pcols], ygb.ap, reads=[ygb.b], writes=[self.yg_b[key]])
                    fin_gen = finalize_gen()
        run_all(fin_gen)

    def phase_m(self, L, hh):
        I = self.I
        NT = 16
        xacc = self.alloc(NT * D, F32, "xacc")
        xa = [T(xacc[:, t * D:(t + 1) * D], f"xacc{t}") for t in range(NT)]
        hxT = self.alloc(KC * 2048, BF16, "hxT").rearrange("p (kc n) -> p kc n", kc=KC)
        hx_b = [[Buf(f"hx{kc}_{t}") for t in range(NT)] for kc in range(KC)]
        gates = self.alloc(NT * NE, F32, "gates")
        gate_b = [Buf(f"gate{t}") for t in range(NT)]
        G2 = self.tile(D, F32, "G2bc")
        self.dma("sp", G2.ap, self.GBC[2 * L + 1], reads=[self.GBC_b[2 * L + 1]], writes=[G2.b])
        rw = self.tile(KC * NE, F32, "rw")
        self.dma("sp", rw.ap.rearrange("p (kc n) -> p kc n", kc=KC), I["router_w"][L].rearrange("(kc p) n -> p kc n", p=128),
                 writes=[rw.b])
        ot_r = Ring([self.tile(D, F32, f"ot{i}") for i in range(2)])
        ep_junk = self.tile(D, BF16, "ep_junk")
        ep_ss = Ring([self.tile(1, F32, f"ep_ss{i}") for i in range(4)])
        ep_rstd = Ring([self.tile(1, F32, f"ep_rstd{i}") for i in range(4)])
        mark = self.top
        MSKEW = 2
        nb = self.norm_bufs(banks=(0, 2), nxs=MSKEW + 1)
        pendn = []
        hx32_r = Ring([self.tile(KC * 128, F32, f"hx32_{i}") for i in range(2)])
        hx32_b = [[Buf(f"hx32_{i}_{c}") for c in range(KC)] for i in range(2)]
        sm = {n: self.tile(w, F32, n) for n, w in [("th", 64), ("sc", 64), ("sel", 64), ("mx", 64), ("gs", 8), ("g8", 8),
                                                    ("pen", 8), ("selm", 64), ("t8", 8), ("mask", 64), ("den", 1)]}
        obanks = Ring([(4, 5), (6, 7)])
        skipP = 'prologue' in os.environ.get('MSKIP', '')
        if L == 0 and not skipP:
            G1 = self.tile(D, F32, "G1bc")
            self.dma("sp", G1.ap, self.GBC[0], reads=[self.GBC_b[0]], writes=[G1.b])
            woutb = self.tile(NJ * D, BF16, "woutb")
            wst = Ring([self.tile(D, F32, f"wst{i}") for i in range(2)])
            for jc in range(NJ):
                st = wst.next()
                self.dma("sp", st.ap, I["a_w_out"][jc * 128:(jc + 1) * 128, :], writes=[st.b])
                self.tt("pool", woutb.ap[:, jc * D:(jc + 1) * D], st.ap, G1.ap, ALU.mult, reads=[st.b, G1.b], writes=[woutb.b])
            ygt_r = Ring([self.tile(NJ * 128, BF16, f"ygt{i}") for i in range(2)])
        stage_b = lambda t_, xs_: self._m_router(L, t_, xs_, nb, hxT, hx_b, hx32_r, hx32_b, rw, sm, obanks, gates, gate_b)
        for t in range(NT):
            tg = hh * NT + t
            self.dma("sp", xa[t].ap, self.XS[tg * 128:(tg + 1) * 128, :], reads=[self.XS_b[tg]], writes=[xa[t].b])
            if L == 0 and not skipP:
                ygt = ygt_r.next()
                self.dma("sp", ygt.ap.rearrange("p (j n) -> p j n", j=NJ),
                         self.ygT.rearrange("j p n -> p j n")[:, :, tg * 128:(tg + 1) * 128],
                         reads=[self.yg_b[(j, tg // 4)] for j in range(NJ)], writes=[ygt.b])
                ob = obanks.next()
                for n in range(2):
                    for jc in range(NJ):
                        self.mm(self.bank(ob[n]), ygt.ap[:, jc * 128:(jc + 1) * 128],
                                woutb.ap[:, jc * D + n * 512: jc * D + (n + 1) * 512], jc == 0, jc == NJ - 1,
                                reads=[ygt.b, woutb.b], writes=[self.psb[ob[n]]])
                self.tt("dve", xa[t].ap, self.bank(ob[0], 2), xa[t].ap, ALU.add,
                        reads=[self.psb[ob[0]], self.psb[ob[1]], xa[t].b], writes=[xa[t].b])
            xs_ = self.norm_a(xa[t], nb)
            pendn.append((t, xs_))
            if len(pendn) > MSKEW:
                stage_b(*pendn.pop(0))
        while pendn:
            stage_b(*pendn.pop(0))
        self.P.barrier()
        self.top = mark
        self._m_experts(L, hh, xa, hxT, hx_b, gates, gate_b, G2, obanks, ot_r, ep_junk, ep_ss, ep_rstd)

    def _m_router(self, L, t, xs_, nb, hxT, hx_b, hx32_r, hx32_b, rw, sm, obanks, gates, gate_b):
        if True:
            si = t % 2
            h32 = hx32_r.next()
            h32v = h32.ap.rearrange("p (kc n) -> p kc n", kc=KC)
            self.norm_b(xs_, self.A2[L], self.B2[L],
                        lambda c, t=t: (hxT[:, c, t * 128:(t + 1) * 128], hx_b[c][t]), nb,
                        dst32=lambda c, h32v=h32v, si=si: (h32v[:, c, :], hx32_b[si][c]))
            self.copy("act", hxT[:, :, t * 128:(t + 1) * 128], h32v, reads=[hx32_b[si][c] for c in range(KC)],
                      writes=[hx_b[c][t] for c in range(KC)])
            ob = obanks.next()
            lg = self.bank(ob[0])[:, 0:NE]
            for kc in range(KC):
                self.mm(lg, h32v[:, kc, :], rw.ap[:, kc * NE:(kc + 1) * NE], kc == 0, kc == KC - 1,
                        reads=[hx32_b[si][kc], rw.b], writes=[self.psb[ob[0]]])
            th, sc, sel, mx, gs, g8, pen, selm, t8, mask, den = [sm[k] for k in
                                                                 ("th", "sc", "sel", "mx", "gs", "g8", "pen", "selm", "t8", "mask", "den")]
            self.act(th.ap, lg, AF.Tanh, reads=[self.psb[ob[0]]], writes=[th.b], scale=0.5)
            self.ts("dve", sc.ap, th.ap, 0.5, ALU.mult, 0.5, ALU.add, reads=[th.b], writes=[sc.b])
            self.tt("dve", sel.ap, sc.ap, self.rb_bc.ap[:, L * NE:(L + 1) * NE], ALU.add, reads=[sc.b, self.rb_bc.b], writes=[sel.b])
            for g in range(8):
                self.P.op("dve", lambda e, o=mx.ap[:, g * 8:(g + 1) * 8], i_=sel.ap[:, g * 8:(g + 1) * 8]: e.max(out=o, in_=i_),
                          [sel.b], [mx.b])
            mxv = mx.ap.rearrange("p (g k) -> p g k", k=8)
            self.tt("dve", gs.ap, mxv[:, :, 0], mxv[:, :, 1], ALU.add, reads=[mx.b], writes=[gs.b])
            self.P.op("dve", lambda e, o=g8.ap, i_=gs.ap: e.max(out=o, in_=i_), [gs.b], [g8.b])
            self.ts("dve", pen.ap, gs.ap, g8.ap[:, 3:4], ALU.is_ge, reads=[gs.b, g8.b], writes=[pen.b])
            self.ts("dve", pen.ap, pen.ap, -1.0, ALU.add, 1e30, ALU.mult, reads=[pen.b], writes=[pen.b])
            for g in range(8):
                self.ts("dve", selm.ap[:, g * 8:(g + 1) * 8], sel.ap[:, g * 8:(g + 1) * 8], pen.ap[:, g:g + 1], ALU.add,
                        reads=[sel.b, pen.b], writes=[selm.b])
            self.P.op("dve", lambda e, o=t8.ap, i_=selm.ap: e.max(out=o, in_=i_), [selm.b], [t8.b])
            self.ts("dve", mask.ap, selm.ap, t8.ap[:, 7:8], ALU.is_ge, reads=[selm.b, t8.b], writes=[mask.b])
            self.tt("dve", mask.ap, mask.ap, sc.ap, ALU.mult, reads=[mask.b, sc.b], writes=[mask.b])
            self.P.op("dve", lambda e, o=den.ap, i_=mask.ap: e.tensor_reduce(out=o, in_=i_, axis=AX.X, op=ALU.add),
                      [mask.b], [den.b])
            self.P.op("dve", lambda e, o=den.ap: e.reciprocal(out=o, in_=o), [den.b], [den.b])
            self.ts("dve", gates[:, t * NE:(t + 1) * NE], mask.ap, den.ap, ALU.mult, 2.5, ALU.mult,
                    reads=[mask.b, den.b], writes=[gate_b[t]])

    def _m_experts(self, L, hh, xa, hxT, hx_b, gates, gate_b, G2, obanks, ot_r, ep_junk, ep_ss, ep_rstd):
        I = self.I
        NT = 16
        wgu_r = Ring([self.tile(KC * 512, BF16, f"wgu{i}") for i in range(2)])
        wdf_r = Ring([self.tile(2 * D, F32, f"wdf{i}") for i in range(2)])
        wdb_r = Ring([self.tile(2 * D, BF16, f"wdb{i}") for i in range(2)])
        s_r = Ring([self.tile(512, F32, f"s{i}") for i in range(2)])
        actT_r = Ring([[self.tile(512, BF16, f"actT{i}_{f}") for f in range(2)] for i in range(3)])
        gubanks = Ring([(0, 1), (2, 3)])
        elist = list(range(NE + 1)) if 'NE_DBG' not in os.environ else list(range(int(os.environ['NE_DBG']))) + [NE]
        if 'expert' in os.environ.get('MSKIP', ''):
            elist = []
        pend_down = None
        for e in elist:
            wgu, wdf, wdb = wgu_r.next(), wdf_r.next(), wdb_r.next()
            src_gu = I["moe_w_gu"][L, e] if e < NE else I["sh_w_gu"][L]
            src_dn = I["moe_w_down"][L, e] if e < NE else I["sh_w_down"][L]
            wguv = wgu.ap.rearrange("p (kc n) -> p kc n", kc=KC)
            self.dma("q2", wguv, src_gu.rearrange("(kc p) n -> p kc n", p=128), writes=[wgu.b])
            self.dma("sp", wdf.ap.rearrange("p (f n) -> p f n", f=2), src_dn.rearrange("(f p) n -> p f n", p=128), writes=[wdf.b])
            for fh in range(2):
                self.tt("pool", wdb.ap[:, fh * D:(fh + 1) * D], wdf.ap[:, fh * D:(fh + 1) * D], G2.ap, ALU.mult,
                        reads=[wdf.b, G2.b], writes=[wdb.b])
            for blk in range(4):
                cols = slice(blk * 512, (blk + 1) * 512)
                actT = actT_r.next()
                for fh in range(2):
                    bg, bu = gubanks.next()
                    rd = [wgu.b]
                    for kc in range(KC):
                        rdk = rd + [hx_b[kc][blk * 4 + q] for q in range(4)]
                        self.mm(self.bank(bg), wguv[:, kc, fh * 128:(fh + 1) * 128], hxT[:, kc, cols], kc == 0, kc == KC - 1,
                                reads=rdk, writes=[self.psb[bg]])
                    for kc in range(KC):
                        rdk = rd + [hx_b[kc][blk * 4 + q] for q in range(4)]
                        self.mm(self.bank(bu), wguv[:, kc, 256 + fh * 128:256 + (fh + 1) * 128], hxT[:, kc, cols], kc == 0,
                                kc == KC - 1, reads=rdk, writes=[self.psb[bu]])
                    s = s_r.next()
                    self.act(s.ap, self.bank(bg), AF.Silu, reads=[self.psb[bg]], writes=[s.b])
                    self.tt("dve", actT[fh].ap, self.bank(bu), s.ap, ALU.mult, reads=[self.psb[bu], s.b], writes=[actT[fh].b])
                    if pend_down is not None:
                        pend_down(range(2 * fh, 2 * fh + 2))
                def down(qs, blk=blk, actT=actT, wdb=wdb, e=e):
                    for q in qs:
                        t = blk * 4 + q
                        ob = obanks.next()
                        for n in range(2):
                            for fh in range(2):
                                self.mm(self.bank(ob[n]), actT[fh].ap[:, q * 128:(q + 1) * 128],
                                        wdb.ap[:, fh * D + n * 512: fh * D + (n + 1) * 512], fh == 0, fh == 1,
                                        reads=[actT[fh].b, wdb.b], writes=[self.psb[ob[n]]])
                        gsc = gates[:, t * NE + e: t * NE + e + 1] if e < NE else 1.0
                        self.stt(xa[t].ap, self.bank(ob[0], 2), gsc, xa[t].ap, ALU.mult, ALU.add,
                                 reads=[self.psb[ob[0]], self.psb[ob[1]], xa[t].b] + ([gate_b[t]] if e < NE else []),
                                 writes=[xa[t].b])
                pend_down = down
        if pend_down is not None:
            pend_down(range(4))
        for t in range(NT):
            tg = hh * NT + t
            if L == 0:
                self.dma("sp", self.XS[tg * 128:(tg + 1) * 128, :], xa[t].ap, reads=[xa[t].b], writes=[self.XS_b[tg]])
                if self.debug and self.stage == 3:
                    self.dma("sp", self.dbgX[tg * 128:(tg + 1) * 128, :], xa[t].ap, reads=[xa[t].b])
            else:
                ss, rstd = ep_ss.next(), ep_rstd.next()
                self.act(ep_junk.ap, xa[t].ap, AF.Square, reads=[xa[t].b], writes=[ss.b, ep_junk.b], accum_out=ss.ap)
                self.ts("pool", rstd.ap, ss.ap, 1.0 / D, ALU.mult, EPS, ALU.add, reads=[ss.b], writes=[rstd.b])
                self.tt("pool", rstd.ap, rstd.ap, self.neghalf.ap[:, 0:1], ALU.pow, reads=[rstd.b, self.neghalf.b], writes=[rstd.b])
                ot = ot_r.next()
                self.stt(ot.ap, xa[t].ap, rstd.ap, self.fin_bc.ap, ALU.mult, ALU.mult,
                         reads=[xa[t].b, rstd.b, self.fin_bc.b], writes=[ot.b])
                self.outs.append(self.dma("sp", self.out[tg * 128:(tg + 1) * 128, :], ot.ap, reads=[ot.b]))

    def phase_s1(self):
        I = self.I
        NF = 16
        winu = self.tile(KC * 2048, BF16, "winu")
        winv = self.tile(KC * 2048, BF16, "winv")
        winuv = winu.ap.rearrange("p (kc n) -> p kc n", kc=KC)
        winvv = winv.ap.rearrange("p (kc n) -> p kc n", kc=KC)
        src = I["b_w_in"].rearrange("(kc p) n -> p kc n", p=128)
        self.dma("q2", winuv, src[:, :, 0:2048], writes=[winu.b])
        self.dma("q2", winvv, src[:, :, 2048:4096], writes=[winv.b])
        woutb = self.tile(NF * D, BF16, "s_woutb")
        wsTb = self.tile(8 * 128, BF16, "wsTb")
        Cm = self.tile(NF * 128, F32, "Cmat")
        Gbc = self.tile(2048, BF16, "s_Gbc")
        lnG = self.lng_fm.ap[:, 0:16]
        lnB = self.lng_fm.ap[:, 16:32]
        mark = self.top
        G1 = self.tile(D, F32, "s_G1")
        self.dma("sp", G1.ap, self.GBC[2], reads=[self.GBC_b[2]], writes=[G1.b])
        wst = Ring([self.tile(D, F32, f"s_wst{i}") for i in range(2)])
        for fc in range(NF):
            st = wst.next()
            self.dma("sp", st.ap, I["b_w_out"][fc * 128:(fc + 1) * 128, :], writes=[st.b])
            self.tt("pool", woutb.ap[:, fc * D:(fc + 1) * D], st.ap, G1.ap, ALU.mult, reads=[st.b, G1.b], writes=[woutb.b])
        ws32 = self.tile(8 * 128, F32, "ws32")
        self.dma("sp", ws32.ap.rearrange("p (g q) -> p g q", g=8), I["b_w_s"].rearrange("g p q -> p g q"), writes=[ws32.b])
        wsT32 = self.tile(8 * 128, F32, "wsT32")
        rs_bc = self.tile(8 * 128, F32, "rs_bc")
        bs_bc = self.tile(8 * 128, F32, "bs_bc")
        bsrow = self.tile(D, F32, "bsrow")
        self.dma("sp", bsrow.ap[0:1, :], I["b_bs_row"], writes=[bsrow.b])
        for hb in range(2):
            for g4 in range(4):
                g = hb * 4 + g4
                self.tr(self.bank(hb)[:, g4 * 128:(g4 + 1) * 128], ws32.ap[:, g * 128:(g + 1) * 128], self.ident.ap,
                        reads=[ws32.b, self.ident.b], writes=[self.psb[hb]])
            self.copy("dve", wsT32.ap[:, hb * 512:(hb + 1) * 512], self.bank(hb), reads=[self.psb[hb]], writes=[wsT32.b])
        self.copy("pool", wsTb.ap, wsT32.ap, reads=[wsT32.b], writes=[wsTb.b])
        for hb in range(2):
            self.mm(self.bank(2 + hb), self.ones.ap, wsT32.ap[:, hb * 512:(hb + 1) * 512], True, True,
                    reads=[self.ones.b, wsT32.b], writes=[self.psb[2 + hb]])
            self.copy("dve", rs_bc.ap[:, hb * 512:(hb + 1) * 512], self.bank(2 + hb), reads=[self.psb[2 + hb]], writes=[rs_bc.b])
            self.mm(self.bank(4 + hb), self.ones.ap[0:1, :], bsrow.ap[0:1, hb * 512:(hb + 1) * 512], True, True,
                    reads=[self.ones.b, bsrow.b], writes=[self.psb[4 + hb]])
            self.copy("dve", bs_bc.ap[:, hb * 512:(hb + 1) * 512], self.bank(4 + hb), reads=[self.psb[4 + hb]], writes=[bs_bc.b])
        lnrow = self.tile(2048, F32, "lnrow")
        self.dma("sp", lnrow.ap[0:1, :], I["b_ln_row"][:, 0:2048], writes=[lnrow.b])
        for n4 in range(4):
            bk = 6 + n4 % 2
            self.mm(self.bank(bk), self.ones.ap[0:1, :], lnrow.ap[0:1, n4 * 512:(n4 + 1) * 512], True, True,
                    reads=[self.ones.b, lnrow.b], writes=[self.psb[bk]])
            self.copy("dve", Gbc.ap[:, n4 * 512:(n4 + 1) * 512], self.bank(bk), reads=[self.psb[bk]], writes=[Gbc.b])
        for fc in range(NF):
            g = fc // 2
            self.stt(Cm.ap[:, fc * 128:(fc + 1) * 128], rs_bc.ap[:, g * 128:(g + 1) * 128], lnB[:, fc:fc + 1],
                     bs_bc.ap[:, g * 128:(g + 1) * 128], ALU.mult, ALU.add,
                     reads=[rs_bc.b, bs_bc.b, self.lng_fm.b], writes=[Cm.b])
        self.P.barrier()
        self.top = mark
        SK = os.environ.get('SSKIP', '')
        if 'main' in SK:
            return
        xts = Ring([self.tile(D, F32, f"s_xt{i}") for i in range(2)])
        nb = self.norm_bufs(banks=(0,), nxs=1)
        hx_ap = [self.alloc(KC * 512, BF16, f"s_hx{i}").rearrange("p (kc n) -> p kc n", kc=KC) for i in range(2)]
        hx_b = [[[Buf(f"shx{i}_{c}_{q}") for q in range(4)] for c in range(KC)] for i in range(2)]
        uT = self.tile(NF * 512, BF16, "s_uT")
        uTv = uT.ap.rearrange("p (f n) -> p f n", f=NF)
        uT_b = [Buf(f"s_uT{f}") for f in range(NF)]
        v = self.tile(2048, F32, "s_v")
        v_b = [Buf(f"s_v{i}") for i in range(4)]
        vn_r = Ring([self.tile(2048, BF16, f"s_vn{i}") for i in range(2)])
        th_r = Ring([self.tile(1024, F32, f"s_th{i}") for i in range(1)])
        vn0 = self.tile(2048, BF16, "s_vn0")
        zT_r = Ring([self.tile(NF * 128, BF16, f"s_zT{i}") for i in range(2)])
        st_ = {n: self.tile(w, F32, "s_" + n) for n, w in [("s1", 4), ("s2", 2), ("mean", 1), ("ex2", 1), ("msq", 1),
                                                            ("rstd", 1), ("nmr", 1), ("t2", 2)]}
        uvb = Ring([2, 3])
        xres_r = Ring([self.tile(D, F32, f"s_xres{i}") for i in range(2)])

        def stage_n(blk):
            si = blk % 2
            hap = hx_ap[si]
            for q in range(4):
                tg = blk * 4 + q
                xt = xts.next()
                self.dma("sp", xt.ap, self.XS[tg * 128:(tg + 1) * 128, :], reads=[self.XS_b[tg]], writes=[xt.b])
                self.norm_tile(xt, self.A1[1], self.B1[1],
                               lambda c, q=q, hap=hap, si=si: (hap[:, c, q * 128:(q + 1) * 128], hx_b[si][c][q]), nb)

        def stage_u(blk):
            si = blk % 2
            hap = hx_ap[si]
            for fc in range(NF):
                bk = uvb.next()
                for kc in range(KC):
                    self.mm(self.bank(bk), winuv[:, kc, fc * 128:(fc + 1) * 128], hap[:, kc, :], kc == 0, kc == KC - 1,
                            reads=[winu.b] + [hx_b[si][kc][q] for q in range(4)], writes=[self.psb[bk]])
                self.act(uTv[:, fc, :], self.bank(bk), AF.Gelu_apprx_tanh, reads=[self.psb[bk]], writes=[uT_b[fc]])

        def stage_a(blk, q):
            si = blk % 2
            hap = hx_ap[si]
            s1, s2 = st_["s1"], st_["s2"]
            for cg in range(4):
                bk = uvb.next()
                for kc in range(KC):
                    self.mm(self.bank(bk), hap[:, kc, q * 128:(q + 1) * 128], winvv[:, kc, cg * 512:(cg + 1) * 512],
                            kc == 0, kc == KC - 1, reads=[winv.b, hx_b[si][kc][q]], writes=[self.psb[bk]])
                self.act(v.ap[:, cg * 512:(cg + 1) * 512], self.bank(bk), AF.Gelu_apprx_tanh,
                         reads=[self.psb[bk]], writes=[v_b[cg], s1.b], accum_out=s1.ap[:, cg:cg + 1])
            for hv in range(2):
                jk = nb["junkr"].next()
                self.act(jk.ap, v.ap[:, hv * 1024:(hv + 1) * 1024], AF.Square, reads=[v_b[2 * hv], v_b[2 * hv + 1]],
                         writes=[s2.b, jk.b], accum_out=s2.ap[:, hv:hv + 1])
            mean, ex2, msq, rstd, nmr, t2 = [st_[k] for k in ("mean", "ex2", "msq", "rstd", "nmr", "t2")]
            self.tt("pool", t2.ap, s1.ap[:, 0:2], s1.ap[:, 2:4], ALU.add, reads=[s1.b], writes=[t2.b])
            self.tt("pool", mean.ap, t2.ap[:, 0:1], t2.ap[:, 1:2], ALU.add, reads=[t2.b], writes=[mean.b])
            self.tt("pool", ex2.ap, s2.ap[:, 0:1], s2.ap[:, 1:2], ALU.add, reads=[s2.b], writes=[ex2.b])
            self.ts("pool", msq.ap, mean.ap, mean.ap, ALU.mult, 1.0 / (2048.0 * 2048.0), ALU.mult, reads=[mean.b], writes=[msq.b])
            self.ts("pool", ex2.ap, ex2.ap, 1.0 / 2048, ALU.mult, EPS, ALU.add, reads=[ex2.b], writes=[ex2.b])
            self.tt("pool", rstd.ap, ex2.ap, msq.ap, ALU.subtract, reads=[ex2.b, msq.b], writes=[rstd.b])
            self.tt("pool", rstd.ap, rstd.ap, self.neghalf.ap[:, 0:1], ALU.pow, reads=[rstd.b, self.neghalf.b], writes=[rstd.b])
            self.ts("pool", nmr.ap, mean.ap, rstd.ap, ALU.mult, -1.0 / 2048, ALU.mult, reads=[mean.b, rstd.b], writes=[nmr.b])
            vn = vn_r.next()
            self.ts("dve", vn0.ap, v.ap, rstd.ap, ALU.mult, nmr.ap, ALU.add, reads=v_b + [rstd.b, nmr.b], writes=[vn0.b])
            self.tt("dve", vn.ap, vn0.ap, Gbc.ap, ALU.mult, reads=[vn0.b, Gbc.b], writes=[vn.b])
            return vn

        def stage_s(blk, q, vn):
            for fc in range(NF):
                bk = 4 + fc // 4
                self.mm(self.bank(bk)[:, (fc % 4) * 128:(fc % 4 + 1) * 128], vn.ap[:, fc * 128:(fc + 1) * 128],
                        wsTb.ap[:, (fc // 2) * 128:(fc // 2 + 1) * 128], True, True,
                        reads=[vn.b, wsTb.b], writes=[self.psb[bk]])
            zT = zT_r.next()
            for hf in range(2):
                th = th_r.next()
                self.tt("dve", th.ap, self.bank(4 + 2 * hf, 2), Cm.ap[:, hf * 1024:(hf + 1) * 1024], ALU.add,
                        reads=[self.psb[4 + 2 * hf], self.psb[5 + 2 * hf], Cm.b], writes=[th.b])
                self.tt("dve", zT.ap[:, hf * 1024:(hf + 1) * 1024].rearrange("p (f n) -> p f n", f=8),
                        th.ap.rearrange("p (f n) -> p f n", f=8), uTv[:, hf * 8:(hf + 1) * 8, q * 128:(q + 1) * 128], ALU.mult,
                        reads=[th.b] + uT_b[hf * 8:(hf + 1) * 8], writes=[zT.b])
            return zT

        def stage_o(blk, q, zT):
            tg = blk * 4 + q
            xr = xres_r.next()
            self.dma("sp", xr.ap, self.XS[tg * 128:(tg + 1) * 128, :], reads=[self.XS_b[tg]], writes=[xr.b])
            for n in range(2):
                for fc in range(NF):
                    self.mm(self.bank(2 + n), zT.ap[:, fc * 128:(fc + 1) * 128], woutb.ap[:, fc * D + n * 512: fc * D + (n + 1) * 512],
                            fc == 0, fc == NF - 1, reads=[zT.b, woutb.b], writes=[self.psb[2 + n]])
            self.tt("dve", xr.ap, self.bank(2, 2), xr.ap, ALU.add, reads=[self.psb[2], self.psb[3], xr.b], writes=[xr.b])
            self.dma("sp", self.XS[tg * 128:(tg + 1) * 128, :], xr.ap, reads=[xr.b], writes=[self.XS_b[tg]])
            if self.debug and self.stage == 4:
                self.dma("sp", self.dbgX[tg * 128:(tg + 1) * 128, :], xr.ap, reads=[xr.b])

        pendS, pendO = [], []

        def step_s():
            if pendS:
                b_, q_, vn_ = pendS.pop(0)
                pendO.append((b_, q_, stage_s(b_, q_, vn_)))

        def step_o():
            if pendO:
                stage_o(*pendO.pop(0))

        stage_n(0)
        for blk in range(8):
            while pendS:
                step_o()
                step_s()
            stage_u(blk)
            for q in range(4):
                vn = stage_a(blk, q)
                step_o()
                step_s()
                pendS.append((blk, q, vn))
                if q == 1 and blk + 1 < 8:
                    stage_n(blk + 1)
        while pendS or pendO:
            step_o()
            step_s()


def _fm(v, nchunk):
    return np.ascontiguousarray(np.asarray(v, np.float32).reshape(nchunk, 128).T)


def _sincos_table():
    quarter = D // 4
    omega = (1.0 / (np.float32(10000.0) ** (np.arange(quarter, dtype=np.float32) / np.float32(quarter)))).astype(np.float32)

    def emb(n):
        p = np.arange(n, dtype=np.float32)[:, None] * omega[None, :]
        return np.concatenate([np.sin(p), np.cos(p)], axis=-1).astype(np.float32)
    rows = SEQ // 64
    er, ec = emb(rows), emb(64)
    pe = np.concatenate([np.broadcast_to(er[:, None, :], (rows, 64, D // 2)),
                         np.broadcast_to(ec[None, :, :], (rows, 64, D // 2))], axis=-1)
    return np.ascontiguousarray(pe.reshape(SEQ, D).astype(np.float32))


def make_in_maps(inp):
    f = lambda a: np.ascontiguousarray(np.asarray(a, np.float32))
    pe = _sincos_table()
    shared = {}
    shared["ident"] = np.eye(128, dtype=np.float32)
    shared["ada_w"] = f(inp["ada_w"])
    ab = f(inp["ada_b"])
    abf = np.stack([_fm(ab[i], 48) for i in range(2)], 0)
    shared["adab_fm"] = np.ascontiguousarray(np.repeat(abf.transpose(1, 0, 2)[:, :, :, None], 2, axis=3).reshape(128, 192))
    shared["adab_row"] = f(ab.reshape(1, -1))
    shared["ng"] = np.ascontiguousarray(np.concatenate(
        [_fm(inp["mix_norm_g"][0], 8), _fm(inp["ffn_norm_g"][0], 8), _fm(inp["mix_norm_g"][1], 8), _fm(inp["ffn_norm_g"][1], 8)], axis=1))
    shared["fin_row"] = f(inp["final_norm_g"]).reshape(1, D)
    shared["a_w_in"] = f(inp["a_w_in"][0])
    cw = f(inp["a_conv_w"][0])
    cwf = np.stack([_fm(cw[k], 10) for k in range(4)], axis=2).reshape(128, 40)
    parts = [cwf, _fm(inp["a_conv_b"][0], 10)]
    for nm in ("a_gate_r_b", "a_gate_i_b", "a_lambda"):
        v = f(inp[nm][0])
        parts.append(np.concatenate([_fm(v[0], 10), _fm(v[1], 10)], axis=1))
    shared["a_small"] = np.ascontiguousarray(np.concatenate(parts, axis=1))
    shared["a_rw"] = f(inp["a_gate_r_w"][0])
    shared["a_iw"] = f(inp["a_gate_i_w"][0])
    shared["a_w_out"] = f(inp["a_w_out"][0])
    shared["b_w_in"] = f(inp["b_w_in"][0])
    shared["b_ln_row"] = np.ascontiguousarray(np.concatenate([f(inp["b_ln_g"][0]), f(inp["b_ln_b"][0])]).reshape(1, 4096))
    shared["b_lng_fm"] = np.ascontiguousarray(np.concatenate([_fm(inp["b_ln_g"][0], 16), _fm(inp["b_ln_b"][0], 16)], axis=1))
    shared["b_w_s"] = f(inp["b_w_s"][0])
    shared["b_bs_row"] = f(inp["b_b_s"][0]).reshape(1, 1024)
    shared["b_w_out"] = f(inp["b_w_out"][0])
    shared["router_w"] = f(inp["router_w"])
    shared["rb_row"] = f(inp["router_b"]).reshape(1, 2 * NE)
    shared["moe_w_gu"] = f(inp["moe_w_gu"])
    shared["moe_w_down"] = f(inp["moe_w_down"])
    shared["sh_w_gu"] = f(inp["shared_w_gu"])
    shared["sh_w_down"] = f(inp["shared_w_down"])
    x = f(inp["x"])
    ctx = f(inp["ctx"])
    c = f(inp["c"])
    cc = f(inp["c_ctx"])
    maps = []
    for k in range(8):
        b, h = k // 2, k % 2
        m = dict(shared)
        m["x_own"] = np.ascontiguousarray(x[b, h * HALF:(h + 1) * HALF])
        m["x_oth"] = np.ascontiguousarray(x[b, (1 - h) * HALF:(2 - h) * HALF])
        m["pe_own"] = np.ascontiguousarray(pe[h * HALF:(h + 1) * HALF])
        m["pe_oth"] = np.ascontiguousarray(pe[(1 - h) * HALF:(2 - h) * HALF])
        m["ctxb"] = np.ascontiguousarray(ctx[b])
        cv = np.stack([_fm(c[b], 8), _fm(cc, 8)], axis=2).reshape(128, 16)
        m["cvec"] = np.ascontiguousarray(cv)
        fl = np.zeros((128, 2), np.float32)
        fl[:, h] = 1.0
        m["flags"] = fl
        maps.append(m)
    return maps


_NC_CACHE = {}


def kernel(**inputs):
    if "nc" not in _NC_CACHE:
        _NC_CACHE["nc"] = Builder(stage=99).build()
    nc = _NC_CACHE["nc"]
    maps = make_in_maps(inputs)
    res = run_bass_kernel_spmd(nc, maps, core_ids=list(range(8)))
    out = np.empty((4, SEQ, D), np.float32)
    for k in range(8):
        b, h = k // 2, k % 2
        out[b, h * HALF:(h + 1) * HALF] = res.results[k]["out"]
    return out
```
